# Optimizing a Trainium2 kernel written in Bass

```python
import jax, jax.numpy as jnp
from jax import lax
import numpy as np

D_MODEL = 2048
BATCH = 8
SEQ = 4096
DEPTH = 1

N_HEADS = 16
N_KV_HEADS = 4
HEAD_DIM = 64
GROUP = N_HEADS // N_KV_HEADS
ROT_DIM = HEAD_DIM // 4
ROPE_THETA = 500000.0
WINDOW = 128
BLOCK = 128
ATTN_WIDTH = N_HEADS * HEAD_DIM
KV_WIDTH = N_KV_HEADS * HEAD_DIM
POOL_WINDOWS = (2, 4, 8, 16)
N_POOL_GROUPS = len(POOL_WINDOWS)
POOL_WIDTH = D_MODEL // 2
POOL_GROUP_DIM = POOL_WIDTH // N_POOL_GROUPS
MIX_WIDTH = ATTN_WIDTH + POOL_WIDTH
IN_WIDTH = ATTN_WIDTH + 2 * KV_WIDTH + POOL_WIDTH
N_EXPERT_GROUPS = 4
EXPERTS_PER_GROUP = 8
N_EXPERTS = N_EXPERT_GROUPS * EXPERTS_PER_GROUP
TOP_K = 2
D_EXPERT = 512
ROW_BLOCK = 256
EPS = 1e-6

kernel_name = "hymba_swa_sink_pool_hmoe_block"


def rms_norm(t, gain):
    tf = t.astype(jnp.float32)
    tf = tf * lax.rsqrt(jnp.mean(tf * tf, axis=-1, keepdims=True) + EPS)
    return (tf * gain.astype(jnp.float32)).astype(t.dtype)


def partial_rope(t, cos, sin):
    half = ROT_DIM // 2
    x1 = t[..., :half]
    x2 = t[..., half:ROT_DIM]
    c = cos[None, :, None, :].astype(t.dtype)
    s = sin[None, :, None, :].astype(t.dtype)
    return jnp.concatenate([x1 * c - x2 * s, x2 * c + x1 * s, t[..., ROT_DIM:]], axis=-1)


def sliding_window_attention(q, k, v, sinks):
    B, S, _, _ = q.shape
    nb = S // BLOCK
    qb = q.reshape(B, nb, BLOCK, N_KV_HEADS, GROUP, HEAD_DIM)

    def band(t):
        tb = t.reshape(B, nb, BLOCK, N_KV_HEADS, HEAD_DIM)
        prev = jnp.pad(tb, ((0, 0), (1, 0), (0, 0), (0, 0), (0, 0)))[:, :nb]
        return jnp.concatenate([prev, tb], axis=2)

    kb, vb = band(k), band(v)
    scale = HEAD_DIM ** -0.5
    s = jnp.einsum('bnqkgd,bnskd->bnkgqs', qb, kb).astype(jnp.float32) * scale
    qi = jnp.arange(BLOCK)[:, None] + BLOCK
    si = jnp.arange(2 * BLOCK)[None, :]
    diff = qi - si
    local = (diff >= 0) & (diff < WINDOW)
    blk = jnp.arange(nb)[:, None, None]
    valid = local[None] & ((si[None] >= BLOCK) | (blk > 0))
    s = jnp.where(valid[None, :, None, None], s, -1e30)
    sink = sinks.astype(jnp.float32).reshape(N_KV_HEADS, GROUP)[:, :, None]
    m = jnp.maximum(jnp.max(s, axis=-1), sink)
    p = jnp.exp(s - m[..., None])
    denom = jnp.sum(p, axis=-1) + jnp.exp(sink - m)
    probs = (p / denom[..., None]).astype(v.dtype)
    o = jnp.einsum('bnkgqs,bnskd->bnqkgd', probs, vb)
    return o.reshape(B, S, ATTN_WIDTH)


def multiscale_pool(u, w_pool, pool_scale):
    B, S, _ = u.shape
    uf = u.astype(jnp.float32)
    cs = jnp.cumsum(uf, axis=1)
    pos = jnp.arange(S)
    outs = []
    for g, w in enumerate(POOL_WINDOWS):
        sl = slice(g * POOL_GROUP_DIM, (g + 1) * POOL_GROUP_DIM)
        cg = cs[..., sl]
        lag = jnp.pad(cg, ((0, 0), (w, 0), (0, 0)))[:, :S]
        cnt = jnp.minimum(pos + 1, w).astype(jnp.float32)[None, :, None]
        outs.append((cg - lag) / cnt - uf[..., sl])
    d = jnp.stack(outs, axis=2).astype(u.dtype)
    y = jnp.einsum('bsgc,gcd->bsgd', d, w_pool).reshape(B, S, POOL_WIDTH)
    return y * pool_scale


def hierarchical_moe(h, w_coarse, b_coarse, w_fine, b_fine, w_gate, w_up, w_down):
    B, S, D = h.shape
    N = B * S
    hf = h.reshape(N, D)
    coarse = (hf @ w_coarse).astype(jnp.float32) + b_coarse.astype(jnp.float32)
    p_coarse = jax.nn.softmax(coarse, axis=-1)
    grp = jnp.argmax(coarse, axis=-1)
    p_grp = jnp.take_along_axis(p_coarse, grp[:, None], axis=1)[:, 0]
    fine = ((hf @ w_fine).astype(jnp.float32) + b_fine.astype(jnp.float32)).reshape(
        N, N_EXPERT_GROUPS, EXPERTS_PER_GROUP)
    fine_sel = jnp.take_along_axis(fine, grp[:, None, None], axis=1)[:, 0]
    p_fine = jax.nn.softmax(fine_sel, axis=-1)
    top_p, top_i = lax.top_k(p_fine, TOP_K)
    gates = p_grp[:, None] * top_p / jnp.sum(top_p, axis=-1, keepdims=True)

    A = N * TOP_K
    eid = (grp[:, None] * EXPERTS_PER_GROUP + top_i).reshape(A)
    tok = jnp.repeat(jnp.arange(N), TOP_K)
    gate = gates.reshape(A)
    order = jnp.argsort(eid)
    s_eid, s_tok, s_gate = eid[order], tok[order], gate[order]
    counts = jnp.bincount(eid, length=N_EXPERTS)
    padded = ((counts + ROW_BLOCK - 1) // ROW_BLOCK) * ROW_BLOCK
    start = jnp.cumsum(counts) - counts
    pad_end = jnp.cumsum(padded)
    pad_start = pad_end - padded
    dest = pad_start[s_eid] + jnp.arange(A) - start[s_eid]
    n_blocks = -(-A // ROW_BLOCK) + N_EXPERTS
    n_rows = n_blocks * ROW_BLOCK
    row_tok = jnp.full((n_rows,), N, dtype=jnp.int32).at[dest].set(s_tok.astype(jnp.int32))
    row_gate = jnp.zeros((n_rows,), jnp.float32).at[dest].set(s_gate)
    blk_eid = jnp.clip(jnp.searchsorted(pad_end, jnp.arange(n_blocks) * ROW_BLOCK, side='right'),
                       0, N_EXPERTS - 1)
    h_ext = jnp.concatenate([hf, jnp.zeros((1, D), hf.dtype)], axis=0)
    xs = h_ext[row_tok].reshape(n_blocks, ROW_BLOCK, D)

    def expert_block(args):
        xb, e = args
        a = xb @ w_gate[e]
        b = xb @ w_up[e]
        return (jax.nn.silu(a) * b) @ w_down[e]

    ys = lax.map(expert_block, (xs, blk_eid)).reshape(n_rows, D)
    ys = ys * row_gate[:, None].astype(ys.dtype)
    out = jnp.zeros((N, D), ys.dtype).at[row_tok].add(ys, mode='drop')
    return out.reshape(B, S, D)


def setup_inputs(seed: int = 0) -> dict:
    key = jax.random.key(seed)
    ks = jax.random.split(key, 18)
    f32 = jnp.float32
    nrm = lambda k, shape, scale: jax.random.normal(k, shape, f32) * scale
    L = DEPTH
    return {
        "x": jax.random.normal(ks[0], (BATCH, SEQ, D_MODEL), f32),
        "norm_mix": 1.0 + nrm(ks[1], (L, D_MODEL), 0.02),
        "w_in": nrm(ks[2], (L, D_MODEL, IN_WIDTH), D_MODEL ** -0.5),
        "q_norm": 1.0 + nrm(ks[3], (L, HEAD_DIM), 0.02),
        "k_norm": 1.0 + nrm(ks[4], (L, HEAD_DIM), 0.02),
        "sinks": nrm(ks[5], (L, N_HEADS), 1.0),
        "w_pool": nrm(ks[6], (L, N_POOL_GROUPS, POOL_GROUP_DIM, POOL_GROUP_DIM), POOL_GROUP_DIM ** -0.5),
        "pool_scale": 1.0 + nrm(ks[7], (L, POOL_WIDTH), 0.1),
        "w_out": nrm(ks[8], (L, MIX_WIDTH, D_MODEL), MIX_WIDTH ** -0.5),
        "norm_ffn": 1.0 + nrm(ks[9], (L, D_MODEL), 0.02),
        "w_coarse": nrm(ks[10], (L, D_MODEL, N_EXPERT_GROUPS), D_MODEL ** -0.5),
        "b_coarse": nrm(ks[11], (L, N_EXPERT_GROUPS), 0.01),
        "w_fine": nrm(ks[12], (L, D_MODEL, N_EXPERTS), D_MODEL ** -0.5),
        "b_fine": nrm(ks[13], (L, N_EXPERTS), 0.01),
        "w_gate": nrm(ks[14], (L, N_EXPERTS, D_MODEL, D_EXPERT), D_MODEL ** -0.5),
        "w_up": nrm(ks[15], (L, N_EXPERTS, D_MODEL, D_EXPERT), D_MODEL ** -0.5),
        "w_down": nrm(ks[16], (L, N_EXPERTS, D_EXPERT, D_MODEL), D_EXPERT ** -0.5),
    }


def reference(x, norm_mix, w_in, q_norm, k_norm, sinks, w_pool, pool_scale, w_out,
              norm_ffn, w_coarse, b_coarse, w_fine, b_fine, w_gate, w_up, w_down):
    B, S, _ = x.shape
    pos = jnp.arange(S, dtype=jnp.float32)
    inv_freq = ROPE_THETA ** (-jnp.arange(0, ROT_DIM, 2, dtype=jnp.float32) / ROT_DIM)
    ang = pos[:, None] * inv_freq[None, :]
    cos, sin = jnp.cos(ang), jnp.sin(ang)
    h = x
    for l in range(DEPTH):
        hn = rms_norm(h, norm_mix[l])
        proj = hn @ w_in[l]
        q = proj[..., :ATTN_WIDTH].reshape(B, S, N_HEADS, HEAD_DIM)
        k = proj[..., ATTN_WIDTH:ATTN_WIDTH + KV_WIDTH].reshape(B, S, N_KV_HEADS, HEAD_DIM)
        v = proj[..., ATTN_WIDTH + KV_WIDTH:ATTN_WIDTH + 2 * KV_WIDTH].reshape(B, S, N_KV_HEADS, HEAD_DIM)
        u = proj[..., ATTN_WIDTH + 2 * KV_WIDTH:]
        q = partial_rope(rms_norm(q, q_norm[l]), cos, sin)
        k = partial_rope(rms_norm(k, k_norm[l]), cos, sin)
        attn = sliding_window_attention(q, k, v, sinks[l])
        pool = multiscale_pool(u, w_pool[l], pool_scale[l])
        mixed = jnp.concatenate([attn, pool.astype(attn.dtype)], axis=-1)
        h = h + mixed @ w_out[l]
        hn = rms_norm(h, norm_ffn[l])
        h = h + hierarchical_moe(hn, w_coarse[l], b_coarse[l], w_fine[l], b_fine[l],
                                 w_gate[l], w_up[l], w_down[l])
    return h
```

```python
import numpy as np
from contextlib import ExitStack
import concourse.bass as bass
import concourse.mybir as mybir
from concourse.bass_utils import run_bass_kernel_spmd

F32 = mybir.dt.float32
BF16 = mybir.dt.bfloat16
I32 = mybir.dt.int32
AF = mybir.ActivationFunctionType
ALU = mybir.AluOpType
AX = mybir.AxisListType

D = 2048
KD = 16
INW = 2560
NH = 16
NKV = 4
HD = 64
E = 32
DE = 512
EPS = 1e-6
ROPE_THETA = 500000.0
POOL_WINDOWS = (2, 4, 8, 16)
N_CORES = 8

ENGINES = ("pe", "act", "dve", "pool", "sp")


class _Buf:
    __slots__ = ("last_w", "readers")

    def __init__(self):
        self.last_w = None
        self.readers = {}


class _Op:
    __slots__ = ("eng", "fn", "deps", "inc", "semval", "is_dma", "key")

    def __init__(self, eng, fn, is_dma, key):
        self.eng = eng
        self.fn = fn
        self.deps = []
        self.inc = False
        self.semval = None
        self.is_dma = is_dma
        self.key = key


class Sched:
    def __init__(self):
        self.ops = []
        self.bufs = {}
        self.last_eng = {}
        self.last_dma = {}
        self.barrier_deps = []
        self.final_waits = []

    def _b(self, name):
        b = self.bufs.get(name)
        if b is None:
            b = _Buf()
            self.bufs[name] = b
        return b

    def _add(self, eng, fn, reads, writes, is_dma, key):
        op = _Op(eng, fn, is_dma, key)
        deps = {}
        for r in reads:
            b = self._b(r)
            if b.last_w is not None:
                deps[id(b.last_w)] = b.last_w
        for w in writes:
            b = self._b(w)
            p = b.last_w
            if p is not None and (is_dma or p.is_dma or p.eng != eng):
                deps[id(p)] = p
            for p in b.readers.values():
                if is_dma or p.is_dma or p.eng != eng:
                    deps[id(p)] = p
        for p in self.barrier_deps:
            deps[id(p)] = p
        rk = ("d", key) if is_dma else eng
        for r in reads:
            self._b(r).readers[rk] = op
        for w in writes:
            b = self._b(w)
            b.last_w = op
            b.readers = {}
        op.deps = list(deps.values())
        for p in op.deps:
            p.inc = True
        self.ops.append(op)
        if is_dma:
            self.last_dma[key] = op
        else:
            self.last_eng[eng] = op
        return op

    def op(self, eng, fn, reads=(), writes=()):
        return self._add(eng, fn, reads, writes, False, None)

    def dma(self, eng, fn, reads=(), writes=(), key=None):
        op = self._add(eng, fn, reads, writes, True, key)
        op.inc = True
        return op

    def barrier(self):
        self.barrier_deps = list(self.last_eng.values()) + list(self.last_dma.values())
        self.bufs = {}

    def finish(self):
        self.final_waits = list(self.last_dma.values())


def run_schedule(nc, sched, es):
    eng_sem = {e: es.enter_context(nc.semaphore("s_" + e)) for e in ENGINES}
    dma_sem, dma_cnt = {}, {}
    eng_cnt = {e: 0 for e in ENGINES}
    for op in sched.ops:
        if op.is_dma:
            if op.key not in dma_sem:
                dma_sem[op.key] = es.enter_context(nc.semaphore("d_" + op.key))
                dma_cnt[op.key] = 0
            dma_cnt[op.key] += 16
            op.semval = dma_cnt[op.key]
        elif op.inc:
            eng_cnt[op.eng] += 1
            op.semval = eng_cnt[op.eng]
    per_eng = {e: [o for o in sched.ops if o.eng == e] for e in ENGINES}

    def sem_of(p):
        return dma_sem[p.key] if p.is_dma else eng_sem[p.eng]

    def body(ename, eng):
        waited = {}

        def wait(p):
            s = sem_of(p)
            if waited.get(id(s), 0) >= p.semval:
                return
            eng.wait_ge(s, p.semval)
            waited[id(s)] = p.semval

        for op in per_eng[ename]:
            need = {}
            for p in op.deps:
                k = id(sem_of(p))
                if k not in need or need[k].semval < p.semval:
                    need[k] = p
            for p in need.values():
                wait(p)
            ins = op.fn(eng)
            if op.is_dma:
                ins.then_inc(dma_sem[op.key], 16)
            elif op.inc:
                ins.then_inc(eng_sem[ename], 1)
        if ename == "sp":
            for p in sched.final_waits:
                wait(p)

    with nc.Block() as block:
        @block.tensor
        def _(e):
            body("pe", e)

        @block.scalar
        def _(e):
            body("act", e)

        @block.vector
        def _(e):
            body("dve", e)

        @block.gpsimd
        def _(e):
            body("pool", e)

        @block.sync
        def _(e):
            body("sp", e)
    return dict(n_ops=len(sched.ops), n_dma_sems=len(dma_sem), eng_cnt=eng_cnt)


C_ID, C_TRI, C_ONE, C_MPREV, C_MCUR, C_POOL = 0, 128, 256, 384, 512, 640
NCST = 640 + 12 * 128


def build_consts(S, C):
    NT = S // 128
    cst = np.zeros((128, NCST), np.float32)
    idx = np.arange(128)
    cst[:, C_ID:C_ID + 128] = np.eye(128, dtype=np.float32)
    cst[:, C_TRI:C_TRI + 128] = (idx[:, None] < idx[None, :])
    cst[:, C_ONE:C_ONE + 128] = 1.0
    cst[:, C_MPREV:C_MPREV + 128] = (idx[:, None] > idx[None, :])
    cst[:, C_MCUR:C_MCUR + 128] = (idx[:, None] <= idx[None, :])
    tp = idx[:, None]
    t = idx[None, :]
    for g, w in enumerate(POOL_WINDOWS):
        mcur = ((tp <= t) & (tp > t - w)) / float(w) - (tp == t)
        mprev = (tp >= t + 129 - w) / float(w)
        cnt = np.minimum(t + 1, w).astype(np.float32)
        m0 = ((tp <= t) & (tp > t - w)) / cnt - (tp == t)
        cst[:, C_POOL + (0 + g) * 128:C_POOL + (1 + g) * 128] = mcur
        cst[:, C_POOL + (4 + g) * 128:C_POOL + (5 + g) * 128] = mprev
        cst[:, C_POOL + (8 + g) * 128:C_POOL + (9 + g) * 128] = m0
    pos = np.arange(S, dtype=np.float32)
    rot = HD // 4
    inv_freq = (ROPE_THETA ** (-np.arange(0, rot, 2, dtype=np.float32) / rot)).astype(np.float32)
    ang = pos[:, None] * inv_freq[None, :]
    cs = np.zeros((128, NT, 16), np.float32)
    cs[:, :, 0:8] = np.cos(ang).astype(np.float32).reshape(NT, 128, 8).transpose(1, 0, 2)
    cs[:, :, 8:16] = np.sin(ang).astype(np.float32).reshape(NT, 128, 8).transpose(1, 0, 2)
    eoff = np.tile((np.arange(E, dtype=np.float32) * C)[None, :], (128, 1))
    return cst, cs.reshape(128, NT * 16), eoff


def build_program(S, C, phases="ABCD", debug=False):
    NT = S // 128
    CT = (C + 127) // 128

    def rws(r):
        return min(128, C - r * 128)
    nc = bass.Bass("TRN2", target_bir_lowering=False)

    def din(name, shape, dt=F32):
        return nc.dram_tensor(name, shape, dt, kind="ExternalInput").ap()

    x = din("x", [S, D])
    norm_mix = din("norm_mix", [1, D])
    w_in = din("w_in", [D, INW])
    q_norm = din("q_norm", [1, HD])
    k_norm = din("k_norm", [1, HD])
    sinks = din("sinks", [1, NH])
    w_pool = din("w_pool", [4, 256, 256])
    pool_scale = din("pool_scale", [1, 1024])
    w_out = din("w_out", [D, D])
    norm_ffn = din("norm_ffn", [1, D])
    w_r = din("w_r", [D, 36])
    b_r = din("b_r", [1, 36])
    w_gate = din("w_gate", [E, D, DE])
    w_up = din("w_up", [E, D, DE])
    w_down = din("w_down", [E, DE, D])
    cst_d = din("cst", [128, NCST])
    cs_d = din("cs", [128, NT * 16])
    eoff_d = din("eoff", [128, E])
    out = nc.dram_tensor("out", [S, D], F32, kind="ExternalOutput").ap()
    dk = dict(kind="ExternalOutput") if debug else {}
    MIX = nc.dram_tensor("MIX", [S, D], BF16, **dk).ap()
    XS = nc.dram_tensor("XS", [E * C, D], BF16, **dk).ap()
    YS = nc.dram_tensor("YS", [E * C, D], BF16, **dk).ap()
    if debug:
        DBGD = nc.dram_tensor("DBGD", [128, S // 128, 2], I32, kind="ExternalOutput").ap()
        DBGG = nc.dram_tensor("DBGG", [128, S // 128, 2], F32, kind="ExternalOutput").ap()

    s = Sched()
    taps = {}
    regcache = {}

    def bcreg(e):
        if "bc" not in regcache:
            regcache["bc"] = e.to_reg(E * C - 1)
        return regcache["bc"]

    def tap(name, ap, reads, cond=True):
        if not (debug and cond):
            return
        shp = list(ap.shape)
        t = nc.dram_tensor("T_" + name, shp, ap.dtype, kind="ExternalOutput").ap()
        taps[name] = t
        s.dma("sp", lambda e: e.dma_start(out=t, in_=ap), reads=reads, key="tap")

    with ExitStack() as es:
        def sbt(stack, name, shape, dt):
            return stack.enter_context(nc.sbuf_tensor("sb_" + name, shape, dt))

        def pst(stack, name, shape, dt):
            return stack.enter_context(nc.psum_tensor("ps_" + name, shape, dt))

        cst = sbt(es, "cst", [128, NCST], BF16)
        DEST = sbt(es, "DEST", [128, NT, 2], I32)
        GATE = sbt(es, "GATE", [128, NT, 2], F32)
        base = sbt(es, "base", [128, E], F32)
        eoff = sbt(es, "eoff", [128, E], F32)
        for a, b in ((0, 1024), (1024, NCST)):
            s.dma("pool", lambda e, a=a, b=b: e.dma_start(out=cst[:, a:b], in_=cst_d[:, a:b]), writes=["cst_%d" % a], key="cst")
        s.op("pool", lambda e: e.memset(DEST[:], 0), reads=["cst_0", "cst_1024"], writes=["cst"])
        s.dma("sp", lambda e: e.dma_start(out=eoff[:], in_=eoff_d), writes=["eoff"], key="eoff")
        s.op("dve", lambda e: e.memset(base[:], 0.0), writes=["base"])
        epsb = sbt(es, "epsb", [128, 1], F32)
        s.op("dve", lambda e: e.memset(epsb[:], EPS), writes=["epsb"])
        ident = cst[:, C_ID:C_ID + 128]
        tri = cst[:, C_TRI:C_TRI + 128]
        ones = cst[:, C_ONE:C_ONE + 128]
        mask2 = cst[:, C_MPREV:C_MPREV + 256].rearrange("p (a q) -> p a q", a=2)

        def poolm(kind, g):
            o = C_POOL + (kind * 4 + g) * 128
            return cst[:, o:o + 128]

        def phase_A():
            pa = ExitStack()
            w_in_bf = sbt(pa, "w_in_bf", [128, KD, INW], BF16)
            gmix = sbt(pa, "gmix", [128, D], F32)
            qg = sbt(pa, "qg", [128, HD], F32)
            kg = sbt(pa, "kg", [128, HD], F32)
            gqk = sbt(pa, "gqk", [128, 20, HD], F32)
            esink = sbt(pa, "esink", [128, NH], F32)
            pscale = sbt(pa, "pscale", [128, 1024], F32)
            wpool_bf = sbt(pa, "wpool_bf", [128, 4, 2, 256], BF16)
            cs = sbt(pa, "cs", [128, NT, 16], F32)
            xt = [sbt(pa, "xtA%d" % i, [128, D], F32) for i in range(2)]
            junk = sbt(pa, "junkA", [128, D], BF16)
            ss = sbt(pa, "ssA", [128, 1], F32)
            std = sbt(pa, "stdA", [128, 1], F32)
            rstd = sbt(pa, "rstdA", [128, 1], F32)
            hn = [sbt(pa, "hnA%d" % i, [128, D], BF16) for i in range(2)]
            hnT = sbt(pa, "hnTA", [128, KD, 128], BF16)
            qk32 = [sbt(pa, "qk32_%d" % i, [128, 1280], F32) for i in range(2)]
            sq32 = sbt(pa, "sq32", [128, 1280], F32)
            ssq = sbt(pa, "ssq", [128, 20], F32)
            stq = sbt(pa, "stq", [128, 20], F32)
            rq = sbt(pa, "rq", [128, 20], F32)
            qn32 = sbt(pa, "qn32", [128, 20, HD], F32)
            qg32 = sbt(pa, "qg32", [128, 20, HD], F32)
            tr = [sbt(pa, "tr%d" % i, [128, 20, 8], F32) for i in range(4)]
            qkb = sbt(pa, "qkb", [128, 20, HD], BF16)
            kd = sbt(pa, "kd", [128, 4, 2, HD], BF16)
            qT = sbt(pa, "qT", [128, 8, 128], BF16)
            kT = [sbt(pa, "kT%d" % i, [128, 4, 128], BF16) for i in range(2)]
            vv = [sbt(pa, "vv%d" % i, [128, 4, 65], BF16) for i in range(3)]
            uu = [sbt(pa, "uu%d" % i, [128, 1024], BF16) for i in range(3)]
            PT = [[sbt(pa, "PT%d%d" % (a, b), [128, 2, 2, 128], BF16) for b in range(2)] for a in range(2)]
            den = sbt(pa, "den", [128, 8], F32)
            rden = sbt(pa, "rden", [128, 8], F32)
            dTs = sbt(pa, "dTs", [128, 8, 128], BF16)
            mixed = [sbt(pa, "mixed%d" % i, [128, D], BF16) for i in range(2)]
            pj = pst(pa, "pj", [128, 5, 512], F32)
            pt = pst(pa, "ptA", [128, 8, 128], BF16)
            sc = [pst(pa, "sc%d" % i, [128, 2, 2, 128], F32) for i in range(2)]
            pjf = pj[:].rearrange("p a b -> p (a b)")

            for c in range(5):
                s.dma("pool", lambda e, c=c: e.dma_start(
                    out=w_in_bf[:, :, c * 512:(c + 1) * 512],
                    in_=w_in[:, c * 512:(c + 1) * 512].rearrange("(k p) n -> p k n", p=128)),
                    writes=["win%d" % c], key="win%d" % c)
            s.dma("sp", lambda e: e.dma_start(out=gmix[:], in_=norm_mix.partition_broadcast(128)), writes=["gmix"], key="gmix")
            s.dma("sp", lambda e: e.dma_start(out=qg[:], in_=q_norm.partition_broadcast(128)), writes=["qg"], key="qg")
            s.dma("sp", lambda e: e.dma_start(out=kg[:], in_=k_norm.partition_broadcast(128)), writes=["kg"], key="kg")
            s.dma("sp", lambda e: e.dma_start(out=esink[:], in_=sinks.partition_broadcast(128)), writes=["esink"], key="esink")
            s.dma("sp", lambda e: e.dma_start(out=pscale[:], in_=pool_scale.partition_broadcast(128)), writes=["pscale"], key="pscale")
            s.dma("sp", lambda e: e.dma_start(out=cs[:], in_=cs_d.rearrange("p (t c) -> p t c", c=16)), writes=["cs"], key="cs")
            s.dma("pool", lambda e: e.dma_start(out=wpool_bf[:], in_=w_pool.rearrange("g (c p) d -> p g c d", p=128)),
                  writes=["wpool"], key="wpool")
            s.op("dve", lambda e: e.tensor_copy(out=gqk[:, 0:16, :], in_=qg[:].unsqueeze(1).to_broadcast([128, 16, HD])),
                 reads=["qg"], writes=["gqk"])
            s.op("dve", lambda e: e.tensor_copy(out=gqk[:, 16:20, :], in_=kg[:].unsqueeze(1).to_broadcast([128, 4, HD])),
                 reads=["kg", "gqk"], writes=["gqk"])
            s.op("act", lambda e: e.activation(out=esink[:], in_=esink[:], func=AF.Exp), reads=["esink"], writes=["esink"])
            for i in range(3):
                s.op("pool", lambda e, i=i: e.memset(vv[i][:], 1.0), writes=["vv%d" % i])

            def F1(i):
                p = i % 2
                XT, HN = xt[p], hn[p]
                nxt, nhn = "xtA%d" % p, "hnA%d" % p
                s.dma("sp", lambda e: e.dma_start(out=XT[:], in_=x[i * 128:(i + 1) * 128, :]), writes=[nxt], key=nxt)
                s.op("act", lambda e: e.activation(out=junk[:], in_=XT[:], func=AF.Square, accum_out=ss[:]),
                     reads=[nxt], writes=["junk", "ss"])
                s.op("act", lambda e: e.activation(out=std[:], in_=ss[:], func=AF.Ln, scale=1.0 / D, bias=epsb[:, 0:1]),
                     reads=["ss", "epsb"], writes=["std"])
                s.op("act", lambda e: e.activation(out=rstd[:], in_=std[:], func=AF.Exp, scale=-0.5), reads=["std"], writes=["rstd"])
                s.op("dve", lambda e: e.scalar_tensor_tensor(out=HN[:], in0=XT[:], scalar=rstd[:, 0:1], in1=gmix[:],
                                                             op0=ALU.mult, op1=ALU.mult),
                     reads=[nxt, "rstd", "gmix"], writes=[nhn])

            def F2(i, part):
                p = i % 2
                HN = hn[p]
                nhn = "hnA%d" % p
                VV, UU, QK = vv[i % 3], uu[i % 3], qk32[p]
                nvv, nuu, nqk = "vv%d" % (i % 3), "uu%d" % (i % 3), "qk32_%d" % p
                if part == 0:
                    for r in range(2):
                        for b in range(8):
                            s.op("pe", lambda e, r=r, b=b: e.transpose(out=pt[:, b, :], in_=HN[:, (r * 8 + b) * 128:(r * 8 + b + 1) * 128],
                                                                       identity=ident),
                                 reads=[nhn, "cst"], writes=["pt"])
                        s.op("act", lambda e, r=r: e.copy(out=hnT[:, r * 8:(r + 1) * 8, :], in_=pt[:]), reads=["pt"], writes=["hnT%d" % r])
                    return
                def chunk(c):
                    for k in range(KD):
                        s.op("pe", lambda e, k=k: e.matmul(out=pj[:, c, :], lhsT=hnT[:, k, :], rhs=w_in_bf[:, k, c * 512:(c + 1) * 512],
                                                           start=(k == 0), stop=(k == KD - 1)),
                             reads=["hnT%d" % (k // 8), "win%d" % c], writes=["pj%d" % c])

                def evac_qkv():
                    s.op("act", lambda e: e.copy(out=QK[:], in_=pjf[:, 0:1280]), reads=[], writes=["pj0", "pj1", "pj2", nqk])
                    s.op("act", lambda e: e.copy(out=VV[:, :, 0:HD], in_=pjf[:, 1280:1536].rearrange("p (h d) -> p h d", d=HD)),
                         reads=[], writes=["pj2", nvv])

                def evac_u():
                    s.op("act", lambda e: e.copy(out=UU[:], in_=pjf[:, 1536:2560]), reads=[], writes=["pj3", "pj4", nuu])

                return [lambda: chunk(0), lambda: chunk(1), lambda: (chunk(2), evac_qkv()), lambda: (chunk(3), chunk(4), evac_u())]

            def QKC(i):
                p = i % 2
                QK = qk32[p]
                nqk = "qk32_%d" % p
                s.op("act", lambda e: e.activation(out=sq32[:], in_=QK[:], func=AF.Square), reads=[nqk], writes=["sq32"])
                s.op("pool", lambda e: e.tensor_tensor(out=qg32[:], in0=QK[:].rearrange("p (h d) -> p h d", d=HD), in1=gqk[:], op=ALU.mult),
                     reads=[nqk, "gqk"], writes=["qg32"])
                s.op("dve", lambda e: e.tensor_reduce(out=ssq[:], in_=sq32[:].rearrange("p (h d) -> p h d", d=HD), axis=AX.X, op=ALU.add),
                     reads=["sq32"], writes=["ssq"])
                s.op("act", lambda e: e.activation(out=stq[:], in_=ssq[:], func=AF.Ln, scale=1.0 / HD, bias=epsb[:, 0:1]),
                     reads=["ssq", "epsb"], writes=["stq"])
                s.op("act", lambda e: e.activation(out=rq[:], in_=stq[:], func=AF.Exp, scale=-0.5), reads=["stq"], writes=["rq"])
                s.op("dve", lambda e: e.tensor_tensor(out=qn32[:], in0=qg32[:],
                                                       in1=rq[:].unsqueeze(2).to_broadcast([128, 20, HD]), op=ALU.mult),
                     reads=["qg32", "rq"], writes=["qn32"])
                cosb = cs[:, i, 0:8].unsqueeze(1).to_broadcast([128, 20, 8])
                sinb = cs[:, i, 8:16].unsqueeze(1).to_broadcast([128, 20, 8])
                s.op("dve", lambda e: e.tensor_tensor(out=tr[0][:], in0=qn32[:, :, 0:8], in1=cosb, op=ALU.mult),
                     reads=["qn32", "cs"], writes=["tr0"])
                s.op("dve", lambda e: e.tensor_tensor(out=tr[1][:], in0=qn32[:, :, 8:16], in1=sinb, op=ALU.mult),
                     reads=["qn32", "cs"], writes=["tr1"])
                s.op("dve", lambda e: e.tensor_tensor(out=tr[2][:], in0=qn32[:, :, 8:16], in1=cosb, op=ALU.mult),
                     reads=["qn32", "cs"], writes=["tr2"])
                s.op("dve", lambda e: e.tensor_tensor(out=tr[3][:], in0=qn32[:, :, 0:8], in1=sinb, op=ALU.mult),
                     reads=["qn32", "cs"], writes=["tr3"])
                s.op("dve", lambda e: e.tensor_tensor(out=qkb[:, :, 0:8], in0=tr[0][:], in1=tr[1][:], op=ALU.subtract),
                     reads=["tr0", "tr1"], writes=["qkb_a"])
                s.op("dve", lambda e: e.tensor_tensor(out=qkb[:, :, 8:16], in0=tr[2][:], in1=tr[3][:], op=ALU.add),
                     reads=["tr2", "tr3"], writes=["qkb_b"])
                s.op("pool", lambda e: e.tensor_copy(out=qkb[:, :, 16:HD], in_=qn32[:, :, 16:HD]), reads=["qn32"], writes=["qkb_c"])
                s.op("dve", lambda e: e.tensor_copy(out=kd[:], in_=qkb[:, 16:20, :].unsqueeze(2).to_broadcast([128, 4, 2, HD])),
                     reads=["qkb_a", "qkb_b", "qkb_c"], writes=["kd"])

            def BK(i):
                p, pp = i % 2, 1 - (i % 2)
                KT, KTp, MX = kT[p], kT[pp], mixed[p]
                nkt, nktp, nmx = "kT%d" % p, "kT%d" % pp, "mixed%d" % p
                VV, VVp, UU, UUp = vv[i % 3], vv[(i - 1) % 3], uu[i % 3], uu[(i - 1) % 3]
                nvv, nvvp, nuu, nuup = "vv%d" % (i % 3), "vv%d" % ((i - 1) % 3), "uu%d" % (i % 3), "uu%d" % ((i - 1) % 3)
                kbs = [1] if i == 0 else [0, 1]
                nkb = len(kbs)
                qkbf = qkb[:].rearrange("p h d -> p (h d)")
                kdf = kd[:].rearrange("p g t d -> p (g t d)")

                def tq():
                    for b in range(8):
                        s.op("pe", lambda e, b=b: e.transpose(out=pt[:, b, :], in_=qkbf[:, b * 128:(b + 1) * 128], identity=ident),
                             reads=["qkb_a", "qkb_b", "qkb_c", "cst"], writes=["pt"])
                    s.op("dve", lambda e: e.tensor_copy(out=qT[:], in_=pt[:]), reads=["pt"], writes=["qT"])
                    for g in range(4):
                        s.op("pe", lambda e, g=g: e.transpose(out=pt[:, g, :], in_=kdf[:, g * 128:(g + 1) * 128], identity=ident),
                             reads=["kd", "cst"], writes=["pt"])
                    s.op("act", lambda e: e.copy(out=KT[:], in_=pt[:, 0:4, :]), reads=["pt"], writes=[nkt])

                def scores(g):
                    for kb in kbs:
                        ktile, nk = (KTp, nktp) if kb == 0 else (KT, nkt)
                        for hf in range(2):
                            s.op("pe", lambda e, kb=kb, hf=hf, ktile=ktile: e.matmul(
                                out=sc[hf][:, kb, :, :], lhsT=ktile[hf * 64:(hf + 1) * 64, g, :],
                                rhs=qT[hf * 64:(hf + 1) * 64, 2 * g:2 * g + 2, :], start=True, stop=True),
                                reads=[nk, "qT"], writes=["sc%d" % hf])
                    k0 = kbs[0]
                    for hf in range(2):
                        P = PT[g % 2][hf]
                        nP = "PT%d%d" % (g % 2, hf)
                        s.op("act", lambda e, hf=hf, P=P: e.activation(out=P[:, k0:2], in_=sc[hf][:, k0:2], func=AF.Exp, scale=HD ** -0.5),
                             reads=[], writes=["sc%d" % hf, nP])
                        s.op("pool" if hf == 1 else "dve", lambda e, P=P: e.tensor_tensor(
                            out=P[:, k0:2], in0=P[:, k0:2], in1=mask2[:, k0:2].unsqueeze(2).to_broadcast([128, 2 - k0, 2, 128]), op=ALU.mult),
                            reads=[nP, "cst"], writes=[nP])

                PVB = (0, 1, 2)

                def pv(g):
                    for blk in range(2):
                        for hf in range(2):
                            h = 4 * g + 2 * blk + hf
                            bnk, j = PVB[h // 7], h % 7
                            o = pj[:, bnk, j * 65:(j + 1) * 65]
                            P = PT[g % 2][hf]
                            for n, kb in enumerate(kbs):
                                vt, nv = (VVp, nvvp) if kb == 0 else (VV, nvv)
                                s.op("pe", lambda e, o=o, P=P, kb=kb, blk=blk, vt=vt, n=n: e.matmul(
                                    out=o, lhsT=P[:, kb, blk, :], rhs=vt[:, g, :], start=(n == 0), stop=(n == nkb - 1)),
                                    reads=["PT%d%d" % (g % 2, hf), nv], writes=["pj%d" % bnk])

                def pv_evac(rnd):
                    bnk = PVB[rnd]
                    h0 = rnd * 7
                    nh = min(7, NH - h0)
                    v = pj[:, bnk, 0:nh * 65].rearrange("p (h d) -> p h d", d=65)
                    s.op("dve", lambda e: e.tensor_tensor(out=den[:, 0:nh], in0=v[:, :, 64], in1=esink[:, h0:h0 + nh], op=ALU.add),
                         reads=["esink"], writes=["pj%d" % bnk, "den"])
                    s.op("dve", lambda e: e.reciprocal(out=rden[:, 0:nh], in_=den[:, 0:nh]), reads=["den"], writes=["rden"])
                    s.op("dve", lambda e: e.tensor_tensor(
                        out=MX[:, h0 * HD:(h0 + nh) * HD].rearrange("p (h d) -> p h d", d=HD), in0=v[:, :, 0:HD],
                        in1=rden[:, 0:nh].unsqueeze(2).to_broadcast([128, nh, HD]), op=ALU.mult),
                        reads=["rden"], writes=["pj%d" % bnk, nmx + "_a%d" % rnd])

                def pool_dT():
                    for cc in range(8):
                        g = cc // 2
                        o = pj[:, 3 + cc // 4, (cc % 4) * 128:(cc % 4 + 1) * 128]
                        if i > 0:
                            s.op("pe", lambda e, o=o, cc=cc, g=g: e.matmul(out=o, lhsT=UUp[:, cc * 128:(cc + 1) * 128], rhs=poolm(1, g),
                                                                           start=True, stop=False),
                                 reads=[nuup, "cst"], writes=["pj%d" % (3 + cc // 4)])
                        s.op("pe", lambda e, o=o, cc=cc, g=g: e.matmul(out=o, lhsT=UU[:, cc * 128:(cc + 1) * 128],
                                                                       rhs=poolm(2 if i == 0 else 0, g), start=(i == 0), stop=True),
                             reads=[nuu, "cst"], writes=["pj%d" % (3 + cc // 4)])
                    s.op("act", lambda e: e.copy(out=dTs[:].rearrange("p a b -> p (a b)"), in_=pjf[:, 1536:2560]),
                         reads=[], writes=["pj3", "pj4", "dTs"])

                def pool_y():
                    for g in range(4):
                        for c2 in range(2):
                            s.op("pe", lambda e, g=g, c2=c2: e.matmul(out=pjf[:, 1536 + g * 256:1536 + (g + 1) * 256], lhsT=dTs[:, 2 * g + c2, :],
                                                                      rhs=wpool_bf[:, g, c2, :], start=(c2 == 0), stop=(c2 == 1)),
                                 reads=["dTs", "wpool"], writes=["pj%d" % (3 + g // 2)])
                    s.op("dve", lambda e: e.tensor_tensor(out=MX[:, 1024:2048], in0=pjf[:, 1536:2560], in1=pscale[:], op=ALU.mult),
                         reads=["pscale"], writes=["pj3", "pj4", nmx + "_p"])

                def sec0():
                    tq()

                def sec1():
                    scores(0)
                    pool_dT()

                def sec2():
                    scores(1)
                    pool_y()

                def sec3():
                    pv(0)
                    scores(2)
                    pv(1)
                    pv_evac(0)

                def sec4():
                    scores(3)
                    pv(2)
                    pv(3)
                    pv_evac(1)
                    pv_evac(2)

                return [sec0, sec1, sec2, sec3, sec4]

            def storeA(i):
                MX = mixed[i % 2]
                nmx = "mixed%d" % (i % 2)
                s.dma("sp", lambda e: e.dma_start(out=MIX[i * 128:(i + 1) * 128, :], in_=MX[:]),
                      reads=[nmx + "_a0", nmx + "_a1", nmx + "_a2", nmx + "_p"], writes=["MIX%d" % i], key="st_" + nmx)

            F1(0)
            if NT > 1:
                F1(1)
            F2(0, 0)
            for f in F2(0, 1):
                f()
            for st in range(NT):
                if st + 1 < NT:
                    F2(st + 1, 0)
                if st + 2 < NT:
                    F1(st + 2)
                if st >= 1:
                    storeA(st - 1)
                QKC(st)
                if st + 1 < NT:
                    for f in F2(st + 1, 1):
                        f()
                for f in BK(st):
                    f()
            storeA(NT - 1)
            s.barrier()
            pa.close()

        def phase_B():
            pb = ExitStack()
            w_out_bf = sbt(pb, "w_out_bf", [128, KD, D], BF16)
            gffn = sbt(pb, "gffn", [128, D], F32)
            wr_bf = sbt(pb, "wr_bf", [128, KD, 36], BF16)
            brb = sbt(pb, "brb", [128, 36], F32)
            xt = [sbt(pb, "xtB%d" % i, [128, D], F32) for i in range(3)]
            mx = [sbt(pb, "mxB%d" % i, [128, D], BF16) for i in range(3)]
            mxT = sbt(pb, "mxT", [128, KD, 128], BF16)
            junk = sbt(pb, "junkB", [128, D], BF16)
            ss = sbt(pb, "ssB", [128, 1], F32)
            std = sbt(pb, "stdB", [128, 1], F32)
            rstd = sbt(pb, "rstdB", [128, 1], F32)
            hn2 = [sbt(pb, "hn2_%d" % i, [128, D], BF16) for i in range(2)]
            hn2T = sbt(pb, "hn2T", [128, KD, 128], BF16)
            lg = sbt(pb, "lg", [128, 36], F32)
            cmax = sbt(pb, "cmax", [128, 1], F32)
            ncmax = sbt(pb, "ncmax", [128, 1], F32)
            ohg = sbt(pb, "ohg", [128, 4], F32)
            ec = sbt(pb, "ec", [128, 4], F32)
            sumc = sbt(pb, "sumc", [128, 1], F32)
            pgrp = sbt(pb, "pgrp", [128, 1], F32)
            tmp48 = sbt(pb, "tmp48", [128, 4, 8], F32)
            fsel = sbt(pb, "fsel", [128, 8], F32)
            m1 = sbt(pb, "m1", [128, 1], F32)
            oh1 = sbt(pb, "oh1", [128, 8], F32)
            msk = sbt(pb, "msk", [128, 8], F32)
            m2 = sbt(pb, "m2", [128, 1], F32)
            oh2 = sbt(pb, "oh2", [128, 8], F32)
            d21 = sbt(pb, "d21", [128, 1], F32)
            e21 = sbt(pb, "e21", [128, 1], F32)
            rr = sbt(pb, "rr", [128, 1], F32)
            E1 = sbt(pb, "E1", [128, 4, 8], F32)
            E2 = sbt(pb, "E2", [128, 4, 8], F32)
            Abf = sbt(pb, "Abf", [128, E], BF16)
            posv = sbt(pb, "posv", [128, E], F32)
            ov = sbt(pb, "ov", [128, E], F32)
            tmp32 = sbt(pb, "tmp32", [128, E], F32)
            dd = sbt(pb, "dd", [128, 2], F32)
            po = pst(pb, "po", [128, 4, 512], F32)
            ptB = pst(pb, "ptB", [128, 8, 128], BF16)
            ptB2 = pst(pb, "ptB2", [128, 8, 128], BF16)
            plg = pst(pb, "plg", [128, 512], F32)
            prk = pst(pb, "prk", [128, 512], F32)
            pof = po[:].rearrange("p a b -> p (a b)")

            for c in range(4):
                s.dma("pool", lambda e, c=c: e.dma_start(
                    out=w_out_bf[:, :, c * 512:(c + 1) * 512],
                    in_=w_out[:, c * 512:(c + 1) * 512].rearrange("(k p) n -> p k n", p=128)),
                    writes=["wout%d" % c], key="wout%d" % c)
            s.dma("pool", lambda e: e.dma_start(out=wr_bf[:], in_=w_r.rearrange("(k p) n -> p k n", p=128)), writes=["wr"], key="wr")
            s.dma("sp", lambda e: e.dma_start(out=gffn[:], in_=norm_ffn.partition_broadcast(128)), writes=["gffn"], key="gffn")
            s.dma("sp", lambda e: e.dma_start(out=brb[:], in_=b_r.partition_broadcast(128)), writes=["brb"], key="brb")
            if pre:
                s.dma("pool", lambda e: e.dma_start(out=pre["wg"][:], in_=w_gate[0].rearrange("(k p) n -> p k n", p=128)),
                      writes=["wg0"], key="wg0")
                s.dma("pool", lambda e: e.dma_start(out=pre["wu"][:], in_=w_up[0].rearrange("(k p) n -> p k n", p=128)),
                      writes=["wu0"], key="wu0")
                for hh_ in range(2):
                    s.dma("pool", lambda e, hh_=hh_: e.dma_start(
                        out=pre["wd"][:, :, hh_ * 1024:(hh_ + 1) * 1024],
                        in_=w_down[0][:, hh_ * 1024:(hh_ + 1) * 1024].rearrange("(j p) n -> p j n", p=128)),
                        writes=["wd0_%d" % hh_], key="wd0_%d" % hh_)

            ptBs = [ptB, ptB2]
            tcnt = [0]

            def transposeB(src, nsrc, dst, ndst):
                for r in range(2):
                    bk = tcnt[0] % 2
                    tcnt[0] += 1
                    PTb = ptBs[bk]
                    for b in range(8):
                        s.op("pe", lambda e, r=r, b=b, PTb=PTb: e.transpose(out=PTb[:, b, :], in_=src[:, (r * 8 + b) * 128:(r * 8 + b + 1) * 128],
                                                                            identity=ident),
                             reads=[nsrc, "cst"], writes=["ptB%d" % bk])
                    s.op("act", lambda e, r=r, PTb=PTb: e.copy(out=dst[:, r * 8:(r + 1) * 8, :], in_=PTb[:]), reads=["ptB%d" % bk],
                         writes=[ndst + "%d" % r])

            V = lambda fn, r, w: s.op("dve", fn, reads=r, writes=w)
            E1f = E1[:].rearrange("p g e -> p (g e)")
            E2f = E2[:].rearrange("p g e -> p (g e)")

            def loadsB(i):
                p = i % 3
                XT, MXb = xt[p], mx[p]
                nxt, nmx = "xtB%d" % p, "mxB%d" % p
                s.dma("sp", lambda e: e.dma_start(out=XT[:], in_=x[i * 128:(i + 1) * 128, :]), writes=[nxt], key=nxt)
                s.dma("sp", lambda e: e.dma_start(out=MXb[:], in_=MIX[i * 128:(i + 1) * 128, :]), writes=[nmx], key=nmx)

            def storeB(i):
                p = i % 3
                XT = xt[p]
                nxt = "xtB%d" % p
                s.dma("sp", lambda e: e.dma_start(out=out[i * 128:(i + 1) * 128, :], in_=XT[:]), reads=[nxt],
                      writes=["out%d" % i], key="st_" + nxt)

            def G1(i, part):
                p = i % 3
                XT, MXb = xt[p], mx[p]
                nxt, nmx = "xtB%d" % p, "mxB%d" % p
                if part == 0:
                    transposeB(MXb, nmx, mxT, "mxT")
                    return
                for c in range(4):
                    for k in range(KD):
                        s.op("pe", lambda e, c=c, k=k: e.matmul(out=po[:, c, :], lhsT=mxT[:, k, :], rhs=w_out_bf[:, k, c * 512:(c + 1) * 512],
                                                                start=(k == 0), stop=(k == KD - 1)),
                             reads=["mxT%d" % (k // 8), "wout%d" % c], writes=["po%d" % c])
                s.op("dve", lambda e: e.tensor_tensor(out=XT[:], in0=pof, in1=XT[:], op=ALU.add),
                     reads=["po0", "po1", "po2", "po3", nxt], writes=[nxt])

            def G2a(i):
                p = i % 2
                XT, H2 = xt[i % 3], hn2[p]
                nxt, nh2 = "xtB%d" % (i % 3), "hn2_%d" % p
                s.op("act", lambda e: e.activation(out=junk[:], in_=XT[:], func=AF.Square, accum_out=ss[:]),
                     reads=[nxt], writes=["junk", "ss"])
                s.op("act", lambda e: e.activation(out=std[:], in_=ss[:], func=AF.Ln, scale=1.0 / D, bias=epsb[:, 0:1]),
                     reads=["ss", "epsb"], writes=["std"])
                s.op("act", lambda e: e.activation(out=rstd[:], in_=std[:], func=AF.Exp, scale=-0.5), reads=["std"], writes=["rstd"])
                s.op("dve", lambda e: e.scalar_tensor_tensor(out=H2[:], in0=XT[:], scalar=rstd[:, 0:1], in1=gffn[:],
                                                             op0=ALU.mult, op1=ALU.mult),
                     reads=[nxt, "rstd", "gffn"], writes=[nh2])

            def G2b(i):
                p = i % 2
                H2 = hn2[p]
                nh2 = "hn2_%d" % p
                transposeB(H2, nh2, hn2T, "hn2T")
                for k in range(KD):
                    s.op("pe", lambda e, k=k: e.matmul(out=plg[:, 0:36], lhsT=hn2T[:, k, :], rhs=wr_bf[:, k, :],
                                                       start=(k == 0), stop=(k == KD - 1)),
                         reads=["hn2T%d" % (k // 8), "wr"], writes=["plg"])
                V(lambda e: e.tensor_tensor(out=lg[:], in0=plg[:, 0:36], in1=brb[:], op=ALU.add), ["plg", "brb"], ["lg"])

            def G3a(i):
                V(lambda e: e.tensor_reduce(out=cmax[:], in_=lg[:, 0:4], axis=AX.X, op=ALU.max), ["lg"], ["cmax"])
                V(lambda e: e.tensor_scalar(out=ohg[:], in0=lg[:, 0:4], scalar1=cmax[:, 0:1], scalar2=None, op0=ALU.is_ge), ["lg", "cmax"], ["ohg"])
                V(lambda e: e.tensor_scalar(out=ncmax[:], in0=cmax[:], scalar1=-1.0, scalar2=None, op0=ALU.mult), ["cmax"], ["ncmax"])
                s.op("act", lambda e: e.activation(out=ec[:], in_=lg[:, 0:4], func=AF.Exp, bias=ncmax[:, 0:1], accum_out=sumc[:]),
                     reads=["lg", "ncmax"], writes=["ec", "sumc"])
                V(lambda e: e.tensor_tensor(out=tmp48[:], in0=lg[:, 4:36].rearrange("p (g e) -> p g e", g=4),
                                            in1=ohg[:].unsqueeze(2).to_broadcast([128, 4, 8]), op=ALU.mult), ["lg", "ohg"], ["tmp48"])
                V(lambda e: e.tensor_reduce(out=fsel[:], in_=tmp48[:].rearrange("p g e -> p e g"), axis=AX.X, op=ALU.add), ["tmp48"], ["fsel"])
                V(lambda e: e.tensor_reduce(out=m1[:], in_=fsel[:], axis=AX.X, op=ALU.max), ["fsel"], ["m1"])
                V(lambda e: e.tensor_scalar(out=oh1[:], in0=fsel[:], scalar1=m1[:, 0:1], scalar2=None, op0=ALU.is_ge), ["fsel", "m1"], ["oh1"])
                V(lambda e: e.scalar_tensor_tensor(out=msk[:], in0=oh1[:], scalar=-1e30, in1=fsel[:], op0=ALU.mult, op1=ALU.add),
                  ["oh1", "fsel"], ["msk"])
                V(lambda e: e.tensor_reduce(out=m2[:], in_=msk[:], axis=AX.X, op=ALU.max), ["msk"], ["m2"])
                V(lambda e: e.tensor_scalar(out=oh2[:], in0=msk[:], scalar1=m2[:, 0:1], scalar2=None, op0=ALU.is_ge), ["msk", "m2"], ["oh2"])
                V(lambda e: e.tensor_tensor(out=d21[:], in0=m2[:], in1=m1[:], op=ALU.subtract), ["m1", "m2"], ["d21"])
                s.op("act", lambda e: e.activation(out=e21[:], in_=d21[:], func=AF.Exp), reads=["d21"], writes=["e21"])
                V(lambda e: e.tensor_tensor(out=E1[:], in0=ohg[:].unsqueeze(2).to_broadcast([128, 4, 8]),
                                            in1=oh1[:].unsqueeze(1).to_broadcast([128, 4, 8]), op=ALU.mult), ["ohg", "oh1"], ["E1"])
                V(lambda e: e.tensor_tensor(out=E2[:], in0=ohg[:].unsqueeze(2).to_broadcast([128, 4, 8]),
                                            in1=oh2[:].unsqueeze(1).to_broadcast([128, 4, 8]), op=ALU.mult), ["ohg", "oh2"], ["E2"])
                V(lambda e: e.tensor_tensor(out=Abf[:], in0=E1f, in1=E2f, op=ALU.add), ["E1", "E2"], ["Abf"])
                V(lambda e: e.reciprocal(out=pgrp[:], in_=sumc[:]), ["sumc"], ["pgrp"])
                V(lambda e: e.tensor_scalar(out=e21[:], in0=e21[:], scalar1=1.0, scalar2=None, op0=ALU.add), ["e21"], ["e21"])
                V(lambda e: e.reciprocal(out=rr[:], in_=e21[:]), ["e21"], ["rr"])
                V(lambda e: e.tensor_tensor(out=GATE[:, i, 0:1], in0=pgrp[:], in1=rr[:], op=ALU.mult), ["pgrp", "rr"], ["GATE"])
                V(lambda e: e.tensor_tensor(out=GATE[:, i, 1:2], in0=pgrp[:], in1=GATE[:, i, 0:1], op=ALU.subtract), ["pgrp", "GATE"], ["GATE"])

            def G3b(i):
                p = i % 2
                H2 = hn2[p]
                nh2 = "hn2_%d" % p
                s.op("pe", lambda e: e.matmul(out=prk[:, 0:E], lhsT=tri, rhs=Abf[:], start=True, stop=True), reads=["Abf", "cst"], writes=["prk"])
                s.op("pe", lambda e: e.matmul(out=prk[:, E:2 * E], lhsT=ones, rhs=Abf[:], start=True, stop=True), reads=["Abf", "cst"], writes=["prk"])
                V(lambda e: e.tensor_tensor(out=posv[:], in0=prk[:, 0:E], in1=base[:], op=ALU.add), ["prk", "base"], ["posv"])
                V(lambda e: e.tensor_tensor(out=base[:], in0=prk[:, E:2 * E], in1=base[:], op=ALU.add), ["prk", "base"], ["base"])
                V(lambda e: e.tensor_scalar(out=ov[:], in0=posv[:], scalar1=float(C), scalar2=1e9, op0=ALU.is_ge, op1=ALU.mult), ["posv"], ["ov"])
                V(lambda e: e.tensor_tensor(out=posv[:], in0=posv[:], in1=ov[:], op=ALU.add), ["posv", "ov"], ["posv"])
                V(lambda e: e.tensor_tensor(out=posv[:], in0=posv[:], in1=eoff[:], op=ALU.add), ["posv", "eoff"], ["posv"])
                V(lambda e: e.tensor_tensor(out=tmp32[:], in0=E1f, in1=posv[:], op=ALU.mult), ["E1", "posv"], ["tmp32"])
                V(lambda e: e.tensor_reduce(out=dd[:, 0:1], in_=tmp32[:], axis=AX.X, op=ALU.add), ["tmp32"], ["dd"])
                V(lambda e: e.tensor_tensor(out=tmp32[:], in0=E2f, in1=posv[:], op=ALU.mult), ["E2", "posv", "dd"], ["tmp32"])
                V(lambda e: e.tensor_reduce(out=dd[:, 1:2], in_=tmp32[:], axis=AX.X, op=ALU.add), ["tmp32"], ["dd"])
                V(lambda e: e.tensor_copy(out=DEST[:, i, :], in_=dd[:]), ["dd"], ["DEST%d" % i])
                for j in range(2):
                    s.dma("pool", lambda e, j=j: e.indirect_dma_start(
                        out=XS, out_offset=bass.IndirectOffsetOnAxis(ap=DEST[:, i, j:j + 1], axis=0),
                        in_=H2[:], in_offset=None, bounds_check=bcreg(e), oob_is_err=False),
                        reads=[nh2, "DEST%d" % i], writes=["XS_%d_%d" % (i, j)], key="sc_" + nh2)

            loadsB(0)
            for st in range(NT + 2):
                if st + 1 < NT:
                    loadsB(st + 1)
                if 0 <= st - 1 < NT:
                    storeB(st - 1)
                if st < NT:
                    G1(st, 0)
                if 0 <= st - 1 < NT:
                    G2a(st - 1)
                if 0 <= st - 2 < NT:
                    G3a(st - 2)
                if st < NT:
                    G1(st, 1)
                if 0 <= st - 1 < NT:
                    G2b(st - 1)
                if 0 <= st - 2 < NT:
                    G3b(st - 2)
            s.barrier()
            pb.close()

        def phase_C():
            pc = ExitStack()
            if pre:
                wg = [pre["wg"], sbt(pc, "wg1", [128, KD, DE], BF16)]
                wu = [pre["wu"], sbt(pc, "wu1", [128, KD, DE], BF16)]
                wd = [pre["wd"], sbt(pc, "wd1", [128, 4, D], BF16)]
            else:
                wg = [sbt(pc, "wg%d" % i, [128, KD, DE], BF16) for i in range(2)]
                wu = [sbt(pc, "wu%d" % i, [128, KD, DE], BF16) for i in range(2)]
                wd = [sbt(pc, "wd%d" % i, [128, 4, D], BF16) for i in range(2)]
            xr = [sbt(pc, "xr%d" % i, [128, D], BF16) for i in range(3)]
            xT = [sbt(pc, "xTC%d" % i, [128, KD, C], BF16) for i in range(2)]
            hT = [sbt(pc, "hTC%d" % i, [128, 4, C], BF16) for i in range(2)]
            sil = [sbt(pc, "sil%d" % i, [128, C], F32) for i in range(2)]
            ys = [sbt(pc, "ys%d" % i, [128, D], BF16) for i in range(2)]
            pab = [[pst(pc, "pab%d%d" % (a, b), [128, 512], F32) for b in range(2)] for a in range(2)]
            py = [pst(pc, "py%d" % i, [128, 512], F32) for i in range(2)]
            ptC = [pst(pc, "ptC%d" % i, [128, 8, 128], BF16) for i in range(2)]
            cnt = dict(row=0, py=0, half=0, cp=0)

            def load_w(ex):
                p = ex % 2
                WG, WU, WD = wg[p], wu[p], wd[p]
                s.dma("pool", lambda e: e.dma_start(out=WG[:], in_=w_gate[ex].rearrange("(k p) n -> p k n", p=128)),
                      writes=["wg%d" % p], key="wg%d" % p)
                s.dma("pool", lambda e: e.dma_start(out=WU[:], in_=w_up[ex].rearrange("(k p) n -> p k n", p=128)),
                      writes=["wu%d" % p], key="wu%d" % p)
                for hh_ in range(2):
                    s.dma("pool", lambda e, hh_=hh_: e.dma_start(
                        out=WD[:, :, hh_ * 1024:(hh_ + 1) * 1024],
                        in_=w_down[ex][:, hh_ * 1024:(hh_ + 1) * 1024].rearrange("(j p) n -> p j n", p=128)),
                        writes=["wd%d_%d" % (p, hh_)], key="wd%d_%d" % (p, hh_))

            def load_x(ex):
                for r in range(CT):
                    q = (ex * CT + r) % 3
                    XR = xr[q]
                    s.dma("sp", lambda e, XR=XR, r=r: e.dma_start(out=XR[0:rws(r), :], in_=XS[ex * C + r * 128:ex * C + r * 128 + rws(r), :]),
                          writes=["xr%d" % q], key="xr%d" % q)

            def transposes(ex):
                p = ex % 2
                XT = xT[p]
                for r in range(CT):
                    q = (ex * CT + r) % 3
                    XR = xr[q]
                    for rr_ in range(2):
                        hf = cnt["half"] % 2
                        cnt["half"] += 1
                        for b in range(8):
                            kk = rr_ * 8 + b
                            s.op("pe", lambda e, XR=XR, kk=kk, hf=hf, b=b, r=r: e.transpose(
                                out=ptC[hf][:, b, 0:rws(r)], in_=XR[0:rws(r), kk * 128:(kk + 1) * 128], identity=ident[0:rws(r), 0:rws(r)]),
                                 reads=["xr%d" % q, "cst"], writes=["ptC%d" % hf])
                        if hf == 0:
                            s.op("act", lambda e, rr_=rr_, r=r, hf=hf: e.copy(out=XT[:, rr_ * 8:(rr_ + 1) * 8, r * 128:r * 128 + rws(r)],
                                                                             in_=ptC[hf][:, :, 0:rws(r)]),
                                 reads=["ptC%d" % hf], writes=["xT%d" % p])
                        else:
                            s.op("dve", lambda e, rr_=rr_, r=r, hf=hf: e.tensor_copy(out=XT[:, rr_ * 8:(rr_ + 1) * 8, r * 128:r * 128 + rws(r)],
                                                                                    in_=ptC[hf][:, :, 0:rws(r)]),
                                 reads=["ptC%d" % hf], writes=["xT%d" % p])

            def gateup(ex):
                p = ex % 2
                WG, WU, XT, HT = wg[p], wu[p], xT[p], hT[p]
                for j in range(4):
                    PA, PB = pab[j % 2]
                    na, nb = "pab%d0" % (j % 2), "pab%d1" % (j % 2)
                    for k in range(KD):
                        s.op("pe", lambda e, PA=PA, j=j, k=k: e.matmul(out=PA[:, 0:C], lhsT=WG[:, k, j * 128:(j + 1) * 128], rhs=XT[:, k, :],
                                                                       start=(k == 0), stop=(k == KD - 1)),
                             reads=["wg%d" % p, "xT%d" % p], writes=[na])
                    for k in range(KD):
                        s.op("pe", lambda e, PB=PB, j=j, k=k: e.matmul(out=PB[:, 0:C], lhsT=WU[:, k, j * 128:(j + 1) * 128], rhs=XT[:, k, :],
                                                                       start=(k == 0), stop=(k == KD - 1)),
                             reads=["wu%d" % p, "xT%d" % p], writes=[nb])
                    SL = sil[j % 2]
                    s.op("act", lambda e, PA=PA, SL=SL: e.activation(out=SL[:], in_=PA[:, 0:C], func=AF.Silu), reads=[na], writes=["sil%d" % (j % 2)])
                    s.op("dve", lambda e, PB=PB, SL=SL, j=j: e.tensor_tensor(out=HT[:, j, :], in0=PB[:, 0:C], in1=SL[:], op=ALU.mult),
                         reads=[nb, "sil%d" % (j % 2)], writes=["hT%d" % p])

            def down(ex):
                p = ex % 2
                WD, HT = wd[p], hT[p]
                for r in range(CT):
                    q = (ex * CT + r) % 2
                    YSb = ys[q]
                    for c in range(4):
                        PY = py[cnt["py"] % 2]
                        npn = "py%d" % (cnt["py"] % 2)
                        for j in range(4):
                            s.op("pe", lambda e, PY=PY, r=r, c=c, j=j: e.matmul(out=PY[0:rws(r), :], lhsT=HT[:, j, r * 128:r * 128 + rws(r)],
                                                                                rhs=WD[:, j, c * 512:(c + 1) * 512], start=(j == 0), stop=(j == 3)),
                                 reads=["hT%d" % p, "wd%d_%d" % (p, c // 2)], writes=[npn])
                        if cnt["py"] % 2 == 0:
                            s.op("act", lambda e, PY=PY, YSb=YSb, c=c, r=r: e.copy(out=YSb[0:rws(r), c * 512:(c + 1) * 512], in_=PY[0:rws(r), :]),
                                 reads=[npn], writes=["ys%d" % q])
                        else:
                            s.op("dve", lambda e, PY=PY, YSb=YSb, c=c, r=r: e.tensor_copy(out=YSb[0:rws(r), c * 512:(c + 1) * 512], in_=PY[0:rws(r), :]),
                                 reads=[npn], writes=["ys%d" % q])
                        cnt["py"] += 1
                    s.dma("sp", lambda e, YSb=YSb, r=r: e.dma_start(out=YS[ex * C + r * 128:ex * C + r * 128 + rws(r), :], in_=YSb[0:rws(r), :]),
                          reads=["ys%d" % q], writes=["YS_%d_%d" % (ex, r)], key="st_ys%d" % q)

            if not pre:
                load_w(0)
            load_x(0)
            transposes(0)
            for ex in range(E):
                if ex + 1 < E:
                    load_w(ex + 1)
                    load_x(ex + 1)
                gateup(ex)
                if ex + 1 < E:
                    transposes(ex + 1)
                down(ex)
            s.barrier()
            pc.close()

        def phase_D():
            pd = ExitStack()
            NS = 4
            ya = [sbt(pd, "ya%d" % i, [128, D], BF16) for i in range(NS)]
            yb = [sbt(pd, "yb%d" % i, [128, D], BF16) for i in range(NS)]
            hh = [sbt(pd, "hh%d" % i, [128, D], F32) for i in range(NS)]
            for i in range(NS):
                s.op("dve", lambda e, i=i: e.memset(ya[i][:], 0.0), writes=["ya%d" % i])
                s.op("pool", lambda e, i=i: e.memset(yb[i][:], 0.0), writes=["yb%d" % i])

            def loadsD(i):
                p = i % NS
                YA, YB, HH = ya[p], yb[p], hh[p]
                s.dma("pool", lambda e: e.indirect_dma_start(
                    out=YA[:], out_offset=None, in_=YS, in_offset=bass.IndirectOffsetOnAxis(ap=DEST[:, i, 0:1], axis=0),
                    bounds_check=bcreg(e), oob_is_err=False), writes=["ya%d" % p], key="ya%d" % p)
                s.dma("pool", lambda e: e.indirect_dma_start(
                    out=YB[:], out_offset=None, in_=YS, in_offset=bass.IndirectOffsetOnAxis(ap=DEST[:, i, 1:2], axis=0),
                    bounds_check=bcreg(e), oob_is_err=False), writes=["yb%d" % p], key="yb%d" % p)
                s.dma("sp", lambda e: e.dma_start(out=HH[:], in_=out[i * 128:(i + 1) * 128, :]), writes=["hh%d" % p], key="hh%d" % p)

            def combineD(i):
                p = i % NS
                YA, YB, HH = ya[p], yb[p], hh[p]
                s.op("dve", lambda e: e.scalar_tensor_tensor(out=HH[:], in0=YA[:], scalar=GATE[:, i, 0:1], in1=HH[:],
                                                             op0=ALU.mult, op1=ALU.add),
                     reads=["ya%d" % p, "hh%d" % p], writes=["hh%d" % p])
                s.op("dve", lambda e: e.scalar_tensor_tensor(out=HH[:], in0=YB[:], scalar=GATE[:, i, 1:2], in1=HH[:],
                                                             op0=ALU.mult, op1=ALU.add),
                     reads=["yb%d" % p, "hh%d" % p], writes=["hh%d" % p])
                s.dma("sp", lambda e: e.dma_start(out=out[i * 128:(i + 1) * 128, :], in_=HH[:]), reads=["hh%d" % p],
                      writes=["outD%d" % i], key="st_hh%d" % p)

            for i in range(NT + 2):
                if i < NT:
                    loadsD(i)
                if i - 2 >= 0:
                    combineD(i - 2)
            pd.close()

        pre = {}
        for ph, fn in (("A", phase_A), ("B", phase_B), ("C", phase_C), ("D", phase_D)):
            if ph == "B" and "B" in phases and "C" in phases:
                pbc = ExitStack()
                pre["wg"] = sbt(pbc, "wg0", [128, KD, DE], BF16)
                pre["wu"] = sbt(pbc, "wu0", [128, KD, DE], BF16)
                pre["wd"] = sbt(pbc, "wd0", [128, 4, D], BF16)
            if ph in phases:
                fn()
            if ph == "C" and pre:
                pbc.close()
        if debug:
            s.barrier()
            s.dma("sp", lambda e: e.dma_start(out=DBGD, in_=DEST[:]), key="dbgd")
            s.dma("sp", lambda e: e.dma_start(out=DBGG, in_=GATE[:]), key="dbgg")
        s.finish()
        info = run_schedule(nc, s, es)
    return nc, info


CAP = 384
_PROG_CACHE = {}


def make_in_map(xb, prm, consts):
    cst, cs, eoff = consts
    m = dict(prm)
    m["x"] = np.ascontiguousarray(xb)
    m["cst"] = cst
    m["cs"] = cs
    m["eoff"] = eoff
    return m


def prep_params(norm_mix, w_in, q_norm, k_norm, sinks, w_pool, pool_scale, w_out, norm_ffn,
                w_coarse, b_coarse, w_fine, b_fine, w_gate, w_up, w_down):
    f = lambda a: np.ascontiguousarray(np.asarray(a, dtype=np.float32))
    return dict(
        norm_mix=f(norm_mix).reshape(1, D), w_in=f(w_in).reshape(D, INW), q_norm=f(q_norm).reshape(1, HD),
        k_norm=f(k_norm).reshape(1, HD), sinks=f(sinks).reshape(1, NH), w_pool=f(w_pool).reshape(4, 256, 256),
        pool_scale=f(pool_scale).reshape(1, 1024), w_out=f(w_out).reshape(D, D), norm_ffn=f(norm_ffn).reshape(1, D),
        w_r=np.ascontiguousarray(np.concatenate([f(w_coarse).reshape(D, 4), f(w_fine).reshape(D, E)], axis=1)),
        b_r=np.ascontiguousarray(np.concatenate([f(b_coarse).reshape(1, 4), f(b_fine).reshape(1, E)], axis=1)),
        w_gate=f(w_gate).reshape(E, D, DE), w_up=f(w_up).reshape(E, D, DE), w_down=f(w_down).reshape(E, DE, D))


def kernel(x, norm_mix, w_in, q_norm, k_norm, sinks, w_pool, pool_scale, w_out, norm_ffn,
           w_coarse, b_coarse, w_fine, b_fine, w_gate, w_up, w_down):
    x = np.asarray(x, dtype=np.float32)
    B, S, _ = x.shape
    assert B == N_CORES
    prm = prep_params(norm_mix, w_in, q_norm, k_norm, sinks, w_pool, pool_scale, w_out, norm_ffn,
                      w_coarse, b_coarse, w_fine, b_fine, w_gate, w_up, w_down)
    key = (S, CAP)
    if key not in _PROG_CACHE:
        _PROG_CACHE[key] = build_program(S, CAP)[0]
    nc = _PROG_CACHE[key]
    consts = build_consts(S, CAP)
    in_maps = [make_in_map(x[b], prm, consts) for b in range(B)]
    res = run_bass_kernel_spmd(nc, in_maps, core_ids=list(range(B)))
    return np.stack([np.asarray(r["out"], dtype=np.float32) for r in res.results], axis=0)
```

```python
import numpy as np
from contextlib import ExitStack
import concourse.bass as bass
import concourse.mybir as mybir
from concourse.bass_utils import run_bass_kernel_spmd

F32 = mybir.dt.float32
BF16 = mybir.dt.bfloat16
I32 = mybir.dt.int32
AF = mybir.ActivationFunctionType
ALU = mybir.AluOpType
AX = mybir.AxisListType

D = 2048
KD = 16
INW = 2560
NH = 16
NKV = 4
HD = 64
E = 32
DE = 512
EPS = 1e-6
ROPE_THETA = 500000.0
POOL_WINDOWS = (2, 4, 8, 16)
N_CORES = 8

ENGINES = ("pe", "act", "dve", "pool", "sp")


class _Buf:
    __slots__ = ("last_w", "readers")

    def __init__(self):
        self.last_w = None
        self.readers = {}


class _Op:
    __slots__ = ("eng", "fn", "deps", "inc", "semval", "is_dma", "key")

    def __init__(self, eng, fn, is_dma, key):
        self.eng = eng
        self.fn = fn
        self.deps = []
        self.inc = False
        self.semval = None
        self.is_dma = is_dma
        self.key = key


class Sched:
    def __init__(self):
        self.ops = []
        self.bufs = {}
        self.last_eng = {}
        self.last_dma = {}
        self.barrier_deps = []
        self.final_waits = []

    def _b(self, name):
        b = self.bufs.get(name)
        if b is None:
            b = _Buf()
            self.bufs[name] = b
        return b

    def _add(self, eng, fn, reads, writes, is_dma, key):
        op = _Op(eng, fn, is_dma, key)
        deps = {}
        for r in reads:
            b = self._b(r)
            if b.last_w is not None:
                deps[id(b.last_w)] = b.last_w
        for w in writes:
            b = self._b(w)
            p = b.last_w
            if p is not None and (is_dma or p.is_dma or p.eng != eng):
                deps[id(p)] = p
            for p in b.readers.values():
                if is_dma or p.is_dma or p.eng != eng:
                    deps[id(p)] = p
        for p in self.barrier_deps:
            deps[id(p)] = p
        rk = ("d", key) if is_dma else eng
        for r in reads:
            self._b(r).readers[rk] = op
        for w in writes:
            b = self._b(w)
            b.last_w = op
            b.readers = {}
        op.deps = list(deps.values())
        for p in op.deps:
            p.inc = True
        self.ops.append(op)
        if is_dma:
            self.last_dma[key] = op
        else:
            self.last_eng[eng] = op
        return op

    def op(self, eng, fn, reads=(), writes=()):
        return self._add(eng, fn, reads, writes, False, None)

    def dma(self, eng, fn, reads=(), writes=(), key=None):
        op = self._add(eng, fn, reads, writes, True, key)
        op.inc = True
        return op

    def barrier(self):
        self.barrier_deps = list(self.last_eng.values()) + list(self.last_dma.values())
        self.bufs = {}

    def finish(self):
        self.final_waits = list(self.last_dma.values())


def run_schedule(nc, sched, es):
    eng_sem = {e: es.enter_context(nc.semaphore("s_" + e)) for e in ENGINES}
    dma_sem, dma_cnt = {}, {}
    eng_cnt = {e: 0 for e in ENGINES}
    for op in sched.ops:
        if op.is_dma:
            if op.key not in dma_sem:
                dma_sem[op.key] = es.enter_context(nc.semaphore("d_" + op.key))
                dma_cnt[op.key] = 0
            dma_cnt[op.key] += 16
            op.semval = dma_cnt[op.key]
        elif op.inc:
            eng_cnt[op.eng] += 1
            op.semval = eng_cnt[op.eng]
    per_eng = {e: [o for o in sched.ops if o.eng == e] for e in ENGINES}

    def sem_of(p):
        return dma_sem[p.key] if p.is_dma else eng_sem[p.eng]

    def body(ename, eng):
        waited = {}

        def wait(p):
            s = sem_of(p)
            if waited.get(id(s), 0) >= p.semval:
                return
            eng.wait_ge(s, p.semval)
            waited[id(s)] = p.semval

        for op in per_eng[ename]:
            need = {}
            for p in op.deps:
                k = id(sem_of(p))
                if k not in need or need[k].semval < p.semval:
                    need[k] = p
            for p in need.values():
                wait(p)
            ins = op.fn(eng)
            if op.is_dma:
                ins.then_inc(dma_sem[op.key], 16)
            elif op.inc:
                ins.then_inc(eng_sem[ename], 1)
        if ename == "sp":
            for p in sched.final_waits:
                wait(p)

    with nc.Block() as block:
        @block.tensor
        def _(e):
            body("pe", e)

        @block.scalar
        def _(e):
            body("act", e)

        @block.vector
        def _(e):
            body("dve", e)

        @block.gpsimd
        def _(e):
            body("pool", e)

        @block.sync
        def _(e):
            body("sp", e)
    return dict(n_ops=len(sched.ops), n_dma_sems=len(dma_sem), eng_cnt=eng_cnt)


C_ID, C_TRI, C_ONE, C_MPREV, C_MCUR, C_POOL = 0, 128, 256, 384, 512, 640
NCST = 640 + 12 * 128


def build_consts(S, C):
    NT = S // 128
    cst = np.zeros((128, NCST), np.float32)
    idx = np.arange(128)
    cst[:, C_ID:C_ID + 128] = np.eye(128, dtype=np.float32)
    cst[:, C_TRI:C_TRI + 128] = (idx[:, None] < idx[None, :])
    cst[:, C_ONE:C_ONE + 128] = 1.0
    cst[:, C_MPREV:C_MPREV + 128] = (idx[:, None] > idx[None, :])
    cst[:, C_MCUR:C_MCUR + 128] = (idx[:, None] <= idx[None, :])
    tp = idx[:, None]
    t = idx[None, :]
    for g, w in enumerate(POOL_WINDOWS):
        mcur = ((tp <= t) & (tp > t - w)) / float(w) - (tp == t)
        mprev = (tp >= t + 129 - w) / float(w)
        cnt = np.minimum(t + 1, w).astype(np.float32)
        m0 = ((tp <= t) & (tp > t - w)) / cnt - (tp == t)
        cst[:, C_POOL + (0 + g) * 128:C_POOL + (1 + g) * 128] = mcur
        cst[:, C_POOL + (4 + g) * 128:C_POOL + (5 + g) * 128] = mprev
        cst[:, C_POOL + (8 + g) * 128:C_POOL + (9 + g) * 128] = m0
    pos = np.arange(S, dtype=np.float32)
    rot = HD // 4
    inv_freq = (ROPE_THETA ** (-np.arange(0, rot, 2, dtype=np.float32) / rot)).astype(np.float32)
    ang = pos[:, None] * inv_freq[None, :]
    cs = np.zeros((128, NT, 16), np.float32)
    cs[:, :, 0:8] = np.cos(ang).astype(np.float32).reshape(NT, 128, 8).transpose(1, 0, 2)
    cs[:, :, 8:16] = np.sin(ang).astype(np.float32).reshape(NT, 128, 8).transpose(1, 0, 2)
    eoff = np.tile((np.arange(E, dtype=np.float32) * C)[None, :], (128, 1))
    return cst, cs.reshape(128, NT * 16), eoff


def build_program(S, C, phases="ABCD", debug=False):
    NT = S // 128
    CT = (C + 127) // 128

    def rws(r):
        return min(128, C - r * 128)
    nc = bass.Bass("TRN2", target_bir_lowering=False)

    def din(name, shape, dt=F32):
        return nc.dram_tensor(name, shape, dt, kind="ExternalInput").ap()

    x = din("x", [S, D])
    norm_mix = din("norm_mix", [1, D])
    w_in = din("w_in", [D, INW])
    q_norm = din("q_norm", [1, HD])
    k_norm = din("k_norm", [1, HD])
    sinks = din("sinks", [1, NH])
    w_pool = din("w_pool", [4, 256, 256])
    pool_scale = din("pool_scale", [1, 1024])
    w_out = din("w_out", [D, D])
    norm_ffn = din("norm_ffn", [1, D])
    w_r = din("w_r", [D, 36])
    b_r = din("b_r", [1, 36])
    w_gate = din("w_gate", [E, D, DE])
    w_up = din("w_up", [E, D, DE])
    w_down = din("w_down", [E, DE, D])
    cst_d = din("cst", [128, NCST])
    cs_d = din("cs", [128, NT * 16])
    eoff_d = din("eoff", [128, E])
    out = nc.dram_tensor("out", [S, D], F32, kind="ExternalOutput").ap()
    dk = dict(kind="ExternalOutput") if debug else {}
    MIX = nc.dram_tensor("MIX", [S, D], BF16, **dk).ap()
    XS = nc.dram_tensor("XS", [E * C, D], BF16, **dk).ap()
    YS = nc.dram_tensor("YS", [E * C, D], BF16, **dk).ap()
    if debug:
        DBGD = nc.dram_tensor("DBGD", [128, S // 128, 2], I32, kind="ExternalOutput").ap()
        DBGG = nc.dram_tensor("DBGG", [128, S // 128, 2], F32, kind="ExternalOutput").ap()

    s = Sched()
    taps = {}
    regcache = {}

    def bcreg(e):
        if "bc" not in regcache:
            regcache["bc"] = e.to_reg(E * C - 1)
        return regcache["bc"]

    def tap(name, ap, reads, cond=True):
        if not (debug and cond):
            return
        shp = list(ap.shape)
        t = nc.dram_tensor("T_" + name, shp, ap.dtype, kind="ExternalOutput").ap()
        taps[name] = t
        s.dma("sp", lambda e: e.dma_start(out=t, in_=ap), reads=reads, key="tap")

    with ExitStack() as es:
        def sbt(stack, name, shape, dt):
            return stack.enter_context(nc.sbuf_tensor("sb_" + name, shape, dt))

        def pst(stack, name, shape, dt):
            return stack.enter_context(nc.psum_tensor("ps_" + name, shape, dt))

        cst = sbt(es, "cst", [128, NCST], BF16)
        DEST = sbt(es, "DEST", [128, NT, 2], I32)
        GATE = sbt(es, "GATE", [128, NT, 2], F32)
        base = sbt(es, "base", [128, E], F32)
        eoff = sbt(es, "eoff", [128, E], F32)
        for a, b in ((0, 1024), (1024, NCST)):
            s.dma("pool", lambda e, a=a, b=b: e.dma_start(out=cst[:, a:b], in_=cst_d[:, a:b]), writes=["cst_%d" % a], key="cst")
        s.op("pool", lambda e: e.memset(DEST[:], 0), reads=["cst_0", "cst_1024"], writes=["cst"])
        s.dma("sp", lambda e: e.dma_start(out=eoff[:], in_=eoff_d), writes=["eoff"], key="eoff")
        s.op("dve", lambda e: e.memset(base[:], 0.0), writes=["base"])
        epsb = sbt(es, "epsb", [128, 1], F32)
        s.op("dve", lambda e: e.memset(epsb[:], EPS), writes=["epsb"])
        ident = cst[:, C_ID:C_ID + 128]
        tri = cst[:, C_TRI:C_TRI + 128]
        ones = cst[:, C_ONE:C_ONE + 128]
        mask2 = cst[:, C_MPREV:C_MPREV + 256].rearrange("p (a q) -> p a q", a=2)

        def poolm(kind, g):
            o = C_POOL + (kind * 4 + g) * 128
            return cst[:, o:o + 128]

        def phase_A():
            pa = ExitStack()
            w_in_bf = sbt(pa, "w_in_bf", [128, KD, INW], BF16)
            gmix = sbt(pa, "gmix", [128, D], F32)
            qg = sbt(pa, "qg", [128, HD], F32)
            kg = sbt(pa, "kg", [128, HD], F32)
            gqk = sbt(pa, "gqk", [128, 20, HD], F32)
            esink = sbt(pa, "esink", [128, NH], F32)
            pscale = sbt(pa, "pscale", [128, 1024], F32)
            wpool_bf = sbt(pa, "wpool_bf", [128, 4, 2, 256], BF16)
            cs = sbt(pa, "cs", [128, NT, 16], F32)
            xt = [sbt(pa, "xtA%d" % i, [128, D], F32) for i in range(2)]
            junk = sbt(pa, "junkA", [128, D], BF16)
            ss = sbt(pa, "ssA", [128, 1], F32)
            std = sbt(pa, "stdA", [128, 1], F32)
            rstd = sbt(pa, "rstdA", [128, 1], F32)
            hn = [sbt(pa, "hnA%d" % i, [128, D], BF16) for i in range(2)]
            hnT = sbt(pa, "hnTA", [128, KD, 128], BF16)
            qk32 = [sbt(pa, "qk32_%d" % i, [128, 1280], F32) for i in range(2)]
            sq32 = sbt(pa, "sq32", [128, 1280], F32)
            ssq = sbt(pa, "ssq", [128, 20], F32)
            stq = sbt(pa, "stq", [128, 20], F32)
            rq = sbt(pa, "rq", [128, 20], F32)
            qn32 = sbt(pa, "qn32", [128, 20, HD], F32)
            qg32 = sbt(pa, "qg32", [128, 20, HD], F32)
            tr = [sbt(pa, "tr%d" % i, [128, 20, 8], F32) for i in range(4)]
            qkb = sbt(pa, "qkb", [128, 20, HD], BF16)
            kd = sbt(pa, "kd", [128, 4, 2, HD], BF16)
            qT = sbt(pa, "qT", [128, 8, 128], BF16)
            kT = [sbt(pa, "kT%d" % i, [128, 4, 128], BF16) for i in range(2)]
            vv = [sbt(pa, "vv%d" % i, [128, 4, 65], BF16) for i in range(3)]
            uu = [sbt(pa, "uu%d" % i, [128, 1024], BF16) for i in range(3)]
            PT = [[sbt(pa, "PT%d%d" % (a, b), [128, 2, 2, 128], BF16) for b in range(2)] for a in range(2)]
            den = sbt(pa, "den", [128, 8], F32)
            rden = sbt(pa, "rden", [128, 8], F32)
            dTs = sbt(pa, "dTs", [128, 8, 128], BF16)
            mixed = [sbt(pa, "mixed%d" % i, [128, D], BF16) for i in range(2)]
            pj = pst(pa, "pj", [128, 5, 512], F32)
            pt = pst(pa, "ptA", [128, 8, 128], BF16)
            sc = [pst(pa, "sc%d" % i, [128, 2, 2, 128], F32) for i in range(2)]
            pjf = pj[:].rearrange("p a b -> p (a b)")

            for c in range(5):
                s.dma("pool", lambda e, c=c: e.dma_start(
                    out=w_in_bf[:, :, c * 512:(c + 1) * 512],
                    in_=w_in[:, c * 512:(c + 1) * 512].rearrange("(k p) n -> p k n", p=128)),
                    writes=["win%d" % c], key="win%d" % c)
            s.dma("sp", lambda e: e.dma_start(out=gmix[:], in_=norm_mix.partition_broadcast(128)), writes=["gmix"], key="gmix")
            s.dma("sp", lambda e: e.dma_start(out=qg[:], in_=q_norm.partition_broadcast(128)), writes=["qg"], key="qg")
            s.dma("sp", lambda e: e.dma_start(out=kg[:], in_=k_norm.partition_broadcast(128)), writes=["kg"], key="kg")
            s.dma("sp", lambda e: e.dma_start(out=esink[:], in_=sinks.partition_broadcast(128)), writes=["esink"], key="esink")
            s.dma("sp", lambda e: e.dma_start(out=pscale[:], in_=pool_scale.partition_broadcast(128)), writes=["pscale"], key="pscale")
            s.dma("sp", lambda e: e.dma_start(out=cs[:], in_=cs_d.rearrange("p (t c) -> p t c", c=16)), writes=["cs"], key="cs")
            s.dma("pool", lambda e: e.dma_start(out=wpool_bf[:], in_=w_pool.rearrange("g (c p) d -> p g c d", p=128)),
                  writes=["wpool"], key="wpool")
            s.op("dve", lambda e: e.tensor_copy(out=gqk[:, 0:16, :], in_=qg[:].unsqueeze(1).to_broadcast([128, 16, HD])),
                 reads=["qg"], writes=["gqk"])
            s.op("dve", lambda e: e.tensor_copy(out=gqk[:, 16:20, :], in_=kg[:].unsqueeze(1).to_broadcast([128, 4, HD])),
                 reads=["kg", "gqk"], writes=["gqk"])
            s.op("act", lambda e: e.activation(out=esink[:], in_=esink[:], func=AF.Exp), reads=["esink"], writes=["esink"])
            for i in range(3):
                s.op("pool", lambda e, i=i: e.memset(vv[i][:], 1.0), writes=["vv%d" % i])

            def F1(i):
                p = i % 2
                XT, HN = xt[p], hn[p]
                nxt, nhn = "xtA%d" % p, "hnA%d" % p
                s.dma("sp", lambda e: e.dma_start(out=XT[:], in_=x[i * 128:(i + 1) * 128, :]), writes=[nxt], key=nxt)
                s.op("act", lambda e: e.activation(out=junk[:], in_=XT[:], func=AF.Square, accum_out=ss[:]),
                     reads=[nxt], writes=["junk", "ss"])
                s.op("act", lambda e: e.activation(out=std[:], in_=ss[:], func=AF.Ln, scale=1.0 / D, bias=epsb[:, 0:1]),
                     reads=["ss", "epsb"], writes=["std"])
                s.op("act", lambda e: e.activation(out=rstd[:], in_=std[:], func=AF.Exp, scale=-0.5), reads=["std"], writes=["rstd"])
                s.op("dve", lambda e: e.scalar_tensor_tensor(out=HN[:], in0=XT[:], scalar=rstd[:, 0:1], in1=gmix[:],
                                                             op0=ALU.mult, op1=ALU.mult),
                     reads=[nxt, "rstd", "gmix"], writes=[nhn])

            def F2(i, part):
                p = i % 2
                HN = hn[p]
                nhn = "hnA%d" % p
                VV, UU, QK = vv[i % 3], uu[i % 3], qk32[p]
                nvv, nuu, nqk = "vv%d" % (i % 3), "uu%d" % (i % 3), "qk32_%d" % p
                if part == 0:
                    for r in range(2):
                        for b in range(8):
                            s.op("pe", lambda e, r=r, b=b: e.transpose(out=pt[:, b, :], in_=HN[:, (r * 8 + b) * 128:(r * 8 + b + 1) * 128],
                                                                       identity=ident),
                                 reads=[nhn, "cst"], writes=["pt"])
                        s.op("act", lambda e, r=r: e.copy(out=hnT[:, r * 8:(r + 1) * 8, :], in_=pt[:]), reads=["pt"], writes=["hnT%d" % r])
                    return
                def chunk(c):
                    for k in range(KD):
                        s.op("pe", lambda e, k=k: e.matmul(out=pj[:, c, :], lhsT=hnT[:, k, :], rhs=w_in_bf[:, k, c * 512:(c + 1) * 512],
                                                           start=(k == 0), stop=(k == KD - 1)),
                             reads=["hnT%d" % (k // 8), "win%d" % c], writes=["pj%d" % c])

                def evac_qkv():
                    s.op("act", lambda e: e.copy(out=QK[:], in_=pjf[:, 0:1280]), reads=[], writes=["pj0", "pj1", "pj2", nqk])
                    s.op("act", lambda e: e.copy(out=VV[:, :, 0:HD], in_=pjf[:, 1280:1536].rearrange("p (h d) -> p h d", d=HD)),
                         reads=[], writes=["pj2", nvv])

                def evac_u():
                    s.op("act", lambda e: e.copy(out=UU[:], in_=pjf[:, 1536:2560]), reads=[], writes=["pj3", "pj4", nuu])

                return [lambda: chunk(0), lambda: chunk(1), lambda: (chunk(2), evac_qkv()), lambda: (chunk(3), chunk(4), evac_u())]

            def QKC(i):
                p = i % 2
                QK = qk32[p]
                nqk = "qk32_%d" % p
                s.op("act", lambda e: e.activation(out=sq32[:], in_=QK[:], func=AF.Square), reads=[nqk], writes=["sq32"])
                s.op("pool", lambda e: e.tensor_tensor(out=qg32[:], in0=QK[:].rearrange("p (h d) -> p h d", d=HD), in1=gqk[:], op=ALU.mult),
                     reads=[nqk, "gqk"], writes=["qg32"])
                s.op("dve", lambda e: e.tensor_reduce(out=ssq[:], in_=sq32[:].rearrange("p (h d) -> p h d", d=HD), axis=AX.X, op=ALU.add),
                     reads=["sq32"], writes=["ssq"])
                s.op("act", lambda e: e.activation(out=stq[:], in_=ssq[:], func=AF.Ln, scale=1.0 / HD, bias=epsb[:, 0:1]),
                     reads=["ssq", "epsb"], writes=["stq"])
                s.op("act", lambda e: e.activation(out=rq[:], in_=stq[:], func=AF.Exp, scale=-0.5), reads=["stq"], writes=["rq"])
                s.op("dve", lambda e: e.tensor_tensor(out=qn32[:], in0=qg32[:],
                                                       in1=rq[:].unsqueeze(2).to_broadcast([128, 20, HD]), op=ALU.mult),
                     reads=["qg32", "rq"], writes=["qn32"])
                cosb = cs[:, i, 0:8].unsqueeze(1).to_broadcast([128, 20, 8])
                sinb = cs[:, i, 8:16].unsqueeze(1).to_broadcast([128, 20, 8])
                s.op("dve", lambda e: e.tensor_tensor(out=tr[0][:], in0=qn32[:, :, 0:8], in1=cosb, op=ALU.mult),
                     reads=["qn32", "cs"], writes=["tr0"])
                s.op("dve", lambda e: e.tensor_tensor(out=tr[1][:], in0=qn32[:, :, 8:16], in1=sinb, op=ALU.mult),
                     reads=["qn32", "cs"], writes=["tr1"])
                s.op("dve", lambda e: e.tensor_tensor(out=tr[2][:], in0=qn32[:, :, 8:16], in1=cosb, op=ALU.mult),
                     reads=["qn32", "cs"], writes=["tr2"])
                s.op("dve", lambda e: e.tensor_tensor(out=tr[3][:], in0=qn32[:, :, 0:8], in1=sinb, op=ALU.mult),
                     reads=["qn32", "cs"], writes=["tr3"])
                s.op("dve", lambda e: e.tensor_tensor(out=qkb[:, :, 0:8], in0=tr[0][:], in1=tr[1][:], op=ALU.subtract),
                     reads=["tr0", "tr1"], writes=["qkb_a"])
                s.op("dve", lambda e: e.tensor_tensor(out=qkb[:, :, 8:16], in0=tr[2][:], in1=tr[3][:], op=ALU.add),
                     reads=["tr2", "tr3"], writes=["qkb_b"])
                s.op("pool", lambda e: e.tensor_copy(out=qkb[:, :, 16:HD], in_=qn32[:, :, 16:HD]), reads=["qn32"], writes=["qkb_c"])
                s.op("dve", lambda e: e.tensor_copy(out=kd[:], in_=qkb[:, 16:20, :].unsqueeze(2).to_broadcast([128, 4, 2, HD])),
                     reads=["qkb_a", "qkb_b", "qkb_c"], writes=["kd"])

            def BK(i):
                p, pp = i % 2, 1 - (i % 2)
                KT, KTp, MX = kT[p], kT[pp], mixed[p]
                nkt, nktp, nmx = "kT%d" % p, "kT%d" % pp, "mixed%d" % p
                VV, VVp, UU, UUp = vv[i % 3], vv[(i - 1) % 3], uu[i % 3], uu[(i - 1) % 3]
                nvv, nvvp, nuu, nuup = "vv%d" % (i % 3), "vv%d" % ((i - 1) % 3), "uu%d" % (i % 3), "uu%d" % ((i - 1) % 3)
                kbs = [1] if i == 0 else [0, 1]
                nkb = len(kbs)
                qkbf = qkb[:].rearrange("p h d -> p (h d)")
                kdf = kd[:].rearrange("p g t d -> p (g t d)")

                def tq():
                    for b in range(8):
                        s.op("pe", lambda e, b=b: e.transpose(out=pt[:, b, :], in_=qkbf[:, b * 128:(b + 1) * 128], identity=ident),
                             reads=["qkb_a", "qkb_b", "qkb_c", "cst"], writes=["pt"])
                    s.op("dve", lambda e: e.tensor_copy(out=qT[:], in_=pt[:]), reads=["pt"], writes=["qT"])
                    for g in range(4):
                        s.op("pe", lambda e, g=g: e.transpose(out=pt[:, g, :], in_=kdf[:, g * 128:(g + 1) * 128], identity=ident),
                             reads=["kd", "cst"], writes=["pt"])
                    s.op("act", lambda e: e.copy(out=KT[:], in_=pt[:, 0:4, :]), reads=["pt"], writes=[nkt])

                def scores(g):
                    for kb in kbs:
                        ktile, nk = (KTp, nktp) if kb == 0 else (KT, nkt)
                        for hf in range(2):
                            s.op("pe", lambda e, kb=kb, hf=hf, ktile=ktile: e.matmul(
                                out=sc[hf][:, kb, :, :], lhsT=ktile[hf * 64:(hf + 1) * 64, g, :],
                                rhs=qT[hf * 64:(hf + 1) * 64, 2 * g:2 * g + 2, :], start=True, stop=True),
                                reads=[nk, "qT"], writes=["sc%d" % hf])
                    k0 = kbs[0]
                    for hf in range(2):
                        P = PT[g % 2][hf]
                        nP = "PT%d%d" % (g % 2, hf)
                        s.op("act", lambda e, hf=hf, P=P: e.activation(out=P[:, k0:2], in_=sc[hf][:, k0:2], func=AF.Exp, scale=HD ** -0.5),
                             reads=[], writes=["sc%d" % hf, nP])
                        s.op("pool" if hf == 1 else "dve", lambda e, P=P: e.tensor_tensor(
                            out=P[:, k0:2], in0=P[:, k0:2], in1=mask2[:, k0:2].unsqueeze(2).to_broadcast([128, 2 - k0, 2, 128]), op=ALU.mult),
                            reads=[nP, "cst"], writes=[nP])

                PVB = (0, 1, 2)

                def pv(g):
                    for blk in range(2):
                        for hf in range(2):
                            h = 4 * g + 2 * blk + hf
                            bnk, j = PVB[h // 7], h % 7
                            o = pj[:, bnk, j * 65:(j + 1) * 65]
                            P = PT[g % 2][hf]
                            for n, kb in enumerate(kbs):
                                vt, nv = (VVp, nvvp) if kb == 0 else (VV, nvv)
                                s.op("pe", lambda e, o=o, P=P, kb=kb, blk=blk, vt=vt, n=n: e.matmul(
                                    out=o, lhsT=P[:, kb, blk, :], rhs=vt[:, g, :], start=(n == 0), stop=(n == nkb - 1)),
                                    reads=["PT%d%d" % (g % 2, hf), nv], writes=["pj%d" % bnk])

                def pv_evac(rnd):
                    bnk = PVB[rnd]
                    h0 = rnd * 7
                    nh = min(7, NH - h0)
                    v = pj[:, bnk, 0:nh * 65].rearrange("p (h d) -> p h d", d=65)
                    s.op("dve", lambda e: e.tensor_tensor(out=den[:, 0:nh], in0=v[:, :, 64], in1=esink[:, h0:h0 + nh], op=ALU.add),
                         reads=["esink"], writes=["pj%d" % bnk, "den"])
                    s.op("dve", lambda e: e.reciprocal(out=rden[:, 0:nh], in_=den[:, 0:nh]), reads=["den"], writes=["rden"])
                    s.op("dve", lambda e: e.tensor_tensor(
                        out=MX[:, h0 * HD:(h0 + nh) * HD].rearrange("p (h d) -> p h d", d=HD), in0=v[:, :, 0:HD],
                        in1=rden[:, 0:nh].unsqueeze(2).to_broadcast([128, nh, HD]), op=ALU.mult),
                        reads=["rden"], writes=["pj%d" % bnk, nmx + "_a%d" % rnd])

                def pool_dT():
                    for cc in range(8):
                        g = cc // 2
                        o = pj[:, 3 + cc // 4, (cc % 4) * 128:(cc % 4 + 1) * 128]
                        if i > 0:
                            s.op("pe", lambda e, o=o, cc=cc, g=g: e.matmul(out=o, lhsT=UUp[:, cc * 128:(cc + 1) * 128], rhs=poolm(1, g),
                                                                           start=True, stop=False),
                                 reads=[nuup, "cst"], writes=["pj%d" % (3 + cc // 4)])
                        s.op("pe", lambda e, o=o, cc=cc, g=g: e.matmul(out=o, lhsT=UU[:, cc * 128:(cc + 1) * 128],
                                                                       rhs=poolm(2 if i == 0 else 0, g), start=(i == 0), stop=True),
                             reads=[nuu, "cst"], writes=["pj%d" % (3 + cc // 4)])
                    s.op("act", lambda e: e.copy(out=dTs[:].rearrange("p a b -> p (a b)"), in_=pjf[:, 1536:2560]),
                         reads=[], writes=["pj3", "pj4", "dTs"])

                def pool_y():
                    for g in range(4):
                        for c2 in range(2):
                            s.op("pe", lambda e, g=g, c2=c2: e.matmul(out=pjf[:, 1536 + g * 256:1536 + (g + 1) * 256], lhsT=dTs[:, 2 * g + c2, :],
                                                                      rhs=wpool_bf[:, g, c2, :], start=(c2 == 0), stop=(c2 == 1)),
                                 reads=["dTs", "wpool"], writes=["pj%d" % (3 + g // 2)])
                    s.op("dve", lambda e: e.tensor_tensor(out=MX[:, 1024:2048], in0=pjf[:, 1536:2560], in1=pscale[:], op=ALU.mult),
                         reads=["pscale"], writes=["pj3", "pj4", nmx + "_p"])

                def sec0():
                    tq()

                def sec1():
                    scores(0)
                    pool_dT()

                def sec2():
                    scores(1)
                    pool_y()

                def sec3():
                    pv(0)
                    scores(2)
                    pv(1)
                    pv_evac(0)

                def sec4():
                    scores(3)
                    pv(2)
                    pv(3)
                    pv_evac(1)
                    pv_evac(2)

                return [sec0, sec1, sec2, sec3, sec4]

            def storeA(i):
                MX = mixed[i % 2]
                nmx = "mixed%d" % (i % 2)
                s.dma("sp", lambda e: e.dma_start(out=MIX[i * 128:(i + 1) * 128, :], in_=MX[:]),
                      reads=[nmx + "_a0", nmx + "_a1", nmx + "_a2", nmx + "_p"], writes=["MIX%d" % i], key="st_" + nmx)

            F1(0)
            if NT > 1:
                F1(1)
            F2(0, 0)
            for f in F2(0, 1):
                f()
            for st in range(NT):
                if st + 1 < NT:
                    F2(st + 1, 0)
                if st + 2 < NT:
                    F1(st + 2)
                if st >= 1:
                    storeA(st - 1)
                QKC(st)
                if st + 1 < NT:
                    for f in F2(st + 1, 1):
                        f()
                for f in BK(st):
                    f()
            storeA(NT - 1)
            s.barrier()
            pa.close()

        def phase_B():
            pb = ExitStack()
            w_out_bf = sbt(pb, "w_out_bf", [128, KD, D], BF16)
            gffn = sbt(pb, "gffn", [128, D], F32)
            wr_bf = sbt(pb, "wr_bf", [128, KD, 36], BF16)
            brb = sbt(pb, "brb", [128, 36], F32)
            xt = [sbt(pb, "xtB%d" % i, [128, D], F32) for i in range(3)]
            mx = [sbt(pb, "mxB%d" % i, [128, D], BF16) for i in range(3)]
            mxT = sbt(pb, "mxT", [128, KD, 128], BF16)
            junk = sbt(pb, "junkB", [128, D], BF16)
            ss = sbt(pb, "ssB", [128, 1], F32)
            std = sbt(pb, "stdB", [128, 1], F32)
            rstd = sbt(pb, "rstdB", [128, 1], F32)
            hn2 = [sbt(pb, "hn2_%d" % i, [128, D], BF16) for i in range(2)]
            hn2T = sbt(pb, "hn2T", [128, KD, 128], BF16)
            lg = sbt(pb, "lg", [128, 36], F32)
            cmax = sbt(pb, "cmax", [128, 1], F32)
            ncmax = sbt(pb, "ncmax", [128, 1], F32)
            ohg = sbt(pb, "ohg", [128, 4], F32)
            ec = sbt(pb, "ec", [128, 4], F32)
            sumc = sbt(pb, "sumc", [128, 1], F32)
            pgrp = sbt(pb, "pgrp", [128, 1], F32)
            tmp48 = sbt(pb, "tmp48", [128, 4, 8], F32)
            fsel = sbt(pb, "fsel", [128, 8], F32)
            m1 = sbt(pb, "m1", [128, 1], F32)
            oh1 = sbt(pb, "oh1", [128, 8], F32)
            msk = sbt(pb, "msk", [128, 8], F32)
            m2 = sbt(pb, "m2", [128, 1], F32)
            oh2 = sbt(pb, "oh2", [128, 8], F32)
            d21 = sbt(pb, "d21", [128, 1], F32)
            e21 = sbt(pb, "e21", [128, 1], F32)
            rr = sbt(pb, "rr", [128, 1], F32)
            E1 = sbt(pb, "E1", [128, 4, 8], F32)
            E2 = sbt(pb, "E2", [128, 4, 8], F32)
            Abf = sbt(pb, "Abf", [128, E], BF16)
            posv = sbt(pb, "posv", [128, E], F32)
            ov = sbt(pb, "ov", [128, E], F32)
            tmp32 = sbt(pb, "tmp32", [128, E], F32)
            dd = sbt(pb, "dd", [128, 2], F32)
            po = pst(pb, "po", [128, 4, 512], F32)
            ptB = pst(pb, "ptB", [128, 8, 128], BF16)
            ptB2 = pst(pb, "ptB2", [128, 8, 128], BF16)
            plg = pst(pb, "plg", [128, 512], F32)
            prk = pst(pb, "prk", [128, 512], F32)
            pof = po[:].rearrange("p a b -> p (a b)")

            for c in range(4):
                s.dma("pool", lambda e, c=c: e.dma_start(
                    out=w_out_bf[:, :, c * 512:(c + 1) * 512],
                    in_=w_out[:, c * 512:(c + 1) * 512].rearrange("(k p) n -> p k n", p=128)),
                    writes=["wout%d" % c], key="wout%d" % c)
            s.dma("pool", lambda e: e.dma_start(out=wr_bf[:], in_=w_r.rearrange("(k p) n -> p k n", p=128)), writes=["wr"], key="wr")
            s.dma("sp", lambda e: e.dma_start(out=gffn[:], in_=norm_ffn.partition_broadcast(128)), writes=["gffn"], key="gffn")
            s.dma("sp", lambda e: e.dma_start(out=brb[:], in_=b_r.partition_broadcast(128)), writes=["brb"], key="brb")
            if pre:
                s.dma("pool", lambda e: e.dma_start(out=pre["wg"][:], in_=w_gate[0].rearrange("(k p) n -> p k n", p=128)),
                      writes=["wg0"], key="wg0")
                s.dma("pool", lambda e: e.dma_start(out=pre["wu"][:], in_=w_up[0].rearrange("(k p) n -> p k n", p=128)),
                      writes=["wu0"], key="wu0")
                for hh_ in range(2):
                    s.dma("pool", lambda e, hh_=hh_: e.dma_start(
                        out=pre["wd"][:, :, hh_ * 1024:(hh_ + 1) * 1024],
                        in_=w_down[0][:, hh_ * 1024:(hh_ + 1) * 1024].rearrange("(j p) n -> p j n", p=128)),
                        writes=["wd0_%d" % hh_], key="wd0_%d" % hh_)

            ptBs = [ptB, ptB2]
            tcnt = [0]

            def transposeB(src, nsrc, dst, ndst):
                for r in range(2):
                    bk = tcnt[0] % 2
                    tcnt[0] += 1
                    PTb = ptBs[bk]
                    for b in range(8):
                        s.op("pe", lambda e, r=r, b=b, PTb=PTb: e.transpose(out=PTb[:, b, :], in_=src[:, (r * 8 + b) * 128:(r * 8 + b + 1) * 128],
                                                                            identity=ident),
                             reads=[nsrc, "cst"], writes=["ptB%d" % bk])
                    s.op("act", lambda e, r=r, PTb=PTb: e.copy(out=dst[:, r * 8:(r + 1) * 8, :], in_=PTb[:]), reads=["ptB%d" % bk],
                         writes=[ndst + "%d" % r])

            V = lambda fn, r, w: s.op("dve", fn, reads=r, writes=w)
            E1f = E1[:].rearrange("p g e -> p (g e)")
            E2f = E2[:].rearrange("p g e -> p (g e)")

            def loadsB(i):
                p = i % 3
                XT, MXb = xt[p], mx[p]
                nxt, nmx = "xtB%d" % p, "mxB%d" % p
                s.dma("sp", lambda e: e.dma_start(out=XT[:], in_=x[i * 128:(i + 1) * 128, :]), writes=[nxt], key=nxt)
                s.dma("sp", lambda e: e.dma_start(out=MXb[:], in_=MIX[i * 128:(i + 1) * 128, :]), writes=[nmx], key=nmx)

            def storeB(i):
                p = i % 3
                XT = xt[p]
                nxt = "xtB%d" % p
                s.dma("sp", lambda e: e.dma_start(out=out[i * 128:(i + 1) * 128, :], in_=XT[:]), reads=[nxt],
                      writes=["out%d" % i], key="st_" + nxt)

            def G1(i, part):
                p = i % 3
                XT, MXb = xt[p], mx[p]
                nxt, nmx = "xtB%d" % p, "mxB%d" % p
                if part == 0:
                    transposeB(MXb, nmx, mxT, "mxT")
                    return
                for c in range(4):
                    for k in range(KD):
                        s.op("pe", lambda e, c=c, k=k: e.matmul(out=po[:, c, :], lhsT=mxT[:, k, :], rhs=w_out_bf[:, k, c * 512:(c + 1) * 512],
                                                                start=(k == 0), stop=(k == KD - 1)),
                             reads=["mxT%d" % (k // 8), "wout%d" % c], writes=["po%d" % c])
                s.op("dve", lambda e: e.tensor_tensor(out=XT[:], in0=pof, in1=XT[:], op=ALU.add),
                     reads=["po0", "po1", "po2", "po3", nxt], writes=[nxt])

            def G2a(i):
                p = i % 2
                XT, H2 = xt[i % 3], hn2[p]
                nxt, nh2 = "xtB%d" % (i % 3), "hn2_%d" % p
                s.op("act", lambda e: e.activation(out=junk[:], in_=XT[:], func=AF.Square, accum_out=ss[:]),
                     reads=[nxt], writes=["junk", "ss"])
                s.op("act", lambda e: e.activation(out=std[:], in_=ss[:], func=AF.Ln, scale=1.0 / D, bias=epsb[:, 0:1]),
                     reads=["ss", "epsb"], writes=["std"])
                s.op("act", lambda e: e.activation(out=rstd[:], in_=std[:], func=AF.Exp, scale=-0.5), reads=["std"], writes=["rstd"])
                s.op("dve", lambda e: e.scalar_tensor_tensor(out=H2[:], in0=XT[:], scalar=rstd[:, 0:1], in1=gffn[:],
                                                             op0=ALU.mult, op1=ALU.mult),
                     reads=[nxt, "rstd", "gffn"], writes=[nh2])

            def G2b(i):
                p = i % 2
                H2 = hn2[p]
                nh2 = "hn2_%d" % p
                transposeB(H2, nh2, hn2T, "hn2T")
                for k in range(KD):
                    s.op("pe", lambda e, k=k: e.matmul(out=plg[:, 0:36], lhsT=hn2T[:, k, :], rhs=wr_bf[:, k, :],
                                                       start=(k == 0), stop=(k == KD - 1)),
                         reads=["hn2T%d" % (k // 8), "wr"], writes=["plg"])
                V(lambda e: e.tensor_tensor(out=lg[:], in0=plg[:, 0:36], in1=brb[:], op=ALU.add), ["plg", "brb"], ["lg"])

            def G3a(i):
                V(lambda e: e.tensor_reduce(out=cmax[:], in_=lg[:, 0:4], axis=AX.X, op=ALU.max), ["lg"], ["cmax"])
                V(lambda e: e.tensor_scalar(out=ohg[:], in0=lg[:, 0:4], scalar1=cmax[:, 0:1], scalar2=None, op0=ALU.is_ge), ["lg", "cmax"], ["ohg"])
                V(lambda e: e.tensor_scalar(out=ncmax[:], in0=cmax[:], scalar1=-1.0, scalar2=None, op0=ALU.mult), ["cmax"], ["ncmax"])
                s.op("act", lambda e: e.activation(out=ec[:], in_=lg[:, 0:4], func=AF.Exp, bias=ncmax[:, 0:1], accum_out=sumc[:]),
                     reads=["lg", "ncmax"], writes=["ec", "sumc"])
                V(lambda e: e.tensor_tensor(out=tmp48[:], in0=lg[:, 4:36].rearrange("p (g e) -> p g e", g=4),
                                            in1=ohg[:].unsqueeze(2).to_broadcast([128, 4, 8]), op=ALU.mult), ["lg", "ohg"], ["tmp48"])
                V(lambda e: e.tensor_reduce(out=fsel[:], in_=tmp48[:].rearrange("p g e -> p e g"), axis=AX.X, op=ALU.add), ["tmp48"], ["fsel"])
                V(lambda e: e.tensor_reduce(out=m1[:], in_=fsel[:], axis=AX.X, op=ALU.max), ["fsel"], ["m1"])
                V(lambda e: e.tensor_scalar(out=oh1[:], in0=fsel[:], scalar1=m1[:, 0:1], scalar2=None, op0=ALU.is_ge), ["fsel", "m1"], ["oh1"])
                V(lambda e: e.scalar_tensor_tensor(out=msk[:], in0=oh1[:], scalar=-1e30, in1=fsel[:], op0=ALU.mult, op1=ALU.add),
                  ["oh1", "fsel"], ["msk"])
                V(lambda e: e.tensor_reduce(out=m2[:], in_=msk[:], axis=AX.X, op=ALU.max), ["msk"], ["m2"])
                V(lambda e: e.tensor_scalar(out=oh2[:], in0=msk[:], scalar1=m2[:, 0:1], scalar2=None, op0=ALU.is_ge), ["msk", "m2"], ["oh2"])
                V(lambda e: e.tensor_tensor(out=d21[:], in0=m2[:], in1=m1[:], op=ALU.subtract), ["m1", "m2"], ["d21"])
                s.op("act", lambda e: e.activation(out=e21[:], in_=d21[:], func=AF.Exp), reads=["d21"], writes=["e21"])
                V(lambda e: e.tensor_tensor(out=E1[:], in0=ohg[:].unsqueeze(2).to_broadcast([128, 4, 8]),
                                            in1=oh1[:].unsqueeze(1).to_broadcast([128, 4, 8]), op=ALU.mult), ["ohg", "oh1"], ["E1"])
                V(lambda e: e.tensor_tensor(out=E2[:], in0=ohg[:].unsqueeze(2).to_broadcast([128, 4, 8]),
                                            in1=oh2[:].unsqueeze(1).to_broadcast([128, 4, 8]), op=ALU.mult), ["ohg", "oh2"], ["E2"])
                V(lambda e: e.tensor_tensor(out=Abf[:], in0=E1f, in1=E2f, op=ALU.add), ["E1", "E2"], ["Abf"])
                V(lambda e: e.reciprocal(out=pgrp[:], in_=sumc[:]), ["sumc"], ["pgrp"])
                V(lambda e: e.tensor_scalar(out=e21[:], in0=e21[:], scalar1=1.0, scalar2=None, op0=ALU.add), ["e21"], ["e21"])
                V(lambda e: e.reciprocal(out=rr[:], in_=e21[:]), ["e21"], ["rr"])
                V(lambda e: e.tensor_tensor(out=GATE[:, i, 0:1], in0=pgrp[:], in1=rr[:], op=ALU.mult), ["pgrp", "rr"], ["GATE"])
                V(lambda e: e.tensor_tensor(out=GATE[:, i, 1:2], in0=pgrp[:], in1=GATE[:, i, 0:1], op=ALU.subtract), ["pgrp", "GATE"], ["GATE"])

            def G3b(i):
                p = i % 2
                H2 = hn2[p]
                nh2 = "hn2_%d" % p
                s.op("pe", lambda e: e.matmul(out=prk[:, 0:E], lhsT=tri, rhs=Abf[:], start=True, stop=True), reads=["Abf", "cst"], writes=["prk"])
                s.op("pe", lambda e: e.matmul(out=prk[:, E:2 * E], lhsT=ones, rhs=Abf[:], start=True, stop=True), reads=["Abf", "cst"], writes=["prk"])
                V(lambda e: e.tensor_tensor(out=posv[:], in0=prk[:, 0:E], in1=base[:], op=ALU.add), ["prk", "base"], ["posv"])
                V(lambda e: e.tensor_tensor(out=base[:], in0=prk[:, E:2 * E], in1=base[:], op=ALU.add), ["prk", "base"], ["base"])
                V(lambda e: e.tensor_scalar(out=ov[:], in0=posv[:], scalar1=float(C), scalar2=1e9, op0=ALU.is_ge, op1=ALU.mult), ["posv"], ["ov"])
                V(lambda e: e.tensor_tensor(out=posv[:], in0=posv[:], in1=ov[:], op=ALU.add), ["posv", "ov"], ["posv"])
                V(lambda e: e.tensor_tensor(out=posv[:], in0=posv[:], in1=eoff[:], op=ALU.add), ["posv", "eoff"], ["posv"])
                V(lambda e: e.tensor_tensor(out=tmp32[:], in0=E1f, in1=posv[:], op=ALU.mult), ["E1", "posv"], ["tmp32"])
                V(lambda e: e.tensor_reduce(out=dd[:, 0:1], in_=tmp32[:], axis=AX.X, op=ALU.add), ["tmp32"], ["dd"])
                V(lambda e: e.tensor_tensor(out=tmp32[:], in0=E2f, in1=posv[:], op=ALU.mult), ["E2", "posv", "dd"], ["tmp32"])
                V(lambda e: e.tensor_reduce(out=dd[:, 1:2], in_=tmp32[:], axis=AX.X, op=ALU.add), ["tmp32"], ["dd"])
                V(lambda e: e.tensor_copy(out=DEST[:, i, :], in_=dd[:]), ["dd"], ["DEST%d" % i])
                for j in range(2):
                    s.dma("pool", lambda e, j=j: e.indirect_dma_start(
                        out=XS, out_offset=bass.IndirectOffsetOnAxis(ap=DEST[:, i, j:j + 1], axis=0),
                        in_=H2[:], in_offset=None, bounds_check=bcreg(e), oob_is_err=False),
                        reads=[nh2, "DEST%d" % i], writes=["XS_%d_%d" % (i, j)], key="sc_" + nh2)

            loadsB(0)
            for st in range(NT + 2):
                if st + 1 < NT:
                    loadsB(st + 1)
                if 0 <= st - 1 < NT:
                    storeB(st - 1)
                if st < NT:
                    G1(st, 0)
                if 0 <= st - 1 < NT:
                    G2a(st - 1)
                if 0 <= st - 2 < NT:
                    G3a(st - 2)
                if st < NT:
                    G1(st, 1)
                if 0 <= st - 1 < NT:
                    G2b(st - 1)
                if 0 <= st - 2 < NT:
                    G3b(st - 2)
            s.barrier()
            pb.close()

        def phase_C():
            pc = ExitStack()
            if pre:
                wg = [pre["wg"], sbt(pc, "wg1", [128, KD, DE], BF16)]
                wu = [pre["wu"], sbt(pc, "wu1", [128, KD, DE], BF16)]
                wd = [pre["wd"], sbt(pc, "wd1", [128, 4, D], BF16)]
            else:
                wg = [sbt(pc, "wg%d" % i, [128, KD, DE], BF16) for i in range(2)]
                wu = [sbt(pc, "wu%d" % i, [128, KD, DE], BF16) for i in range(2)]
                wd = [sbt(pc, "wd%d" % i, [128, 4, D], BF16) for i in range(2)]
            xr = [sbt(pc, "xr%d" % i, [128, D], BF16) for i in range(3)]
            xT = [sbt(pc, "xTC%d" % i, [128, KD, C], BF16) for i in range(2)]
            hT = [sbt(pc, "hTC%d" % i, [128, 4, C], BF16) for i in range(2)]
            sil = [sbt(pc, "sil%d" % i, [128, C], F32) for i in range(2)]
            ys = [sbt(pc, "ys%d" % i, [128, D], BF16) for i in range(4)]
            pab = [[pst(pc, "pab%d%d" % (a, b), [128, 512], F32) for b in range(2)] for a in range(2)]
            py = [pst(pc, "py%d" % i, [128, 512], F32) for i in range(2)]
            ptC = [pst(pc, "ptC%d" % i, [128, 8, 128], BF16) for i in range(2)]
            cnt = dict(row=0, py=0, half=0, cp=0)

            def load_w(ex):
                p = ex % 2
                WG, WU, WD = wg[p], wu[p], wd[p]
                s.dma("pool", lambda e: e.dma_start(out=WG[:], in_=w_gate[ex].rearrange("(k p) n -> p k n", p=128)),
                      writes=["wg%d" % p], key="wg%d" % p)
                s.dma("pool", lambda e: e.dma_start(out=WU[:], in_=w_up[ex].rearrange("(k p) n -> p k n", p=128)),
                      writes=["wu%d" % p], key="wu%d" % p)
                for hh_ in range(2):
                    s.dma("pool", lambda e, hh_=hh_: e.dma_start(
                        out=WD[:, :, hh_ * 1024:(hh_ + 1) * 1024],
                        in_=w_down[ex][:, hh_ * 1024:(hh_ + 1) * 1024].rearrange("(j p) n -> p j n", p=128)),
                        writes=["wd%d_%d" % (p, hh_)], key="wd%d_%d" % (p, hh_))

            def load_x(ex):
                for r in range(CT):
                    q = (ex * CT + r) % 3
                    XR = xr[q]
                    s.dma("sp", lambda e, XR=XR, r=r: e.dma_start(out=XR[0:rws(r), :], in_=XS[ex * C + r * 128:ex * C + r * 128 + rws(r), :]),
                          writes=["xr%d" % q], key="xr%d" % q)

            def transposes(ex):
                p = ex % 2
                XT = xT[p]
                for r in range(CT):
                    q = (ex * CT + r) % 3
                    XR = xr[q]
                    for rr_ in range(2):
                        hf = cnt["half"] % 2
                        cnt["half"] += 1
                        for b in range(8):
                            kk = rr_ * 8 + b
                            s.op("pe", lambda e, XR=XR, kk=kk, hf=hf, b=b, r=r: e.transpose(
                                out=ptC[hf][:, b, 0:rws(r)], in_=XR[0:rws(r), kk * 128:(kk + 1) * 128], identity=ident[0:rws(r), 0:rws(r)]),
                                 reads=["xr%d" % q, "cst"], writes=["ptC%d" % hf])
                        if hf == 0:
                            s.op("act", lambda e, rr_=rr_, r=r, hf=hf: e.copy(out=XT[:, rr_ * 8:(rr_ + 1) * 8, r * 128:r * 128 + rws(r)],
                                                                             in_=ptC[hf][:, :, 0:rws(r)]),
                                 reads=["ptC%d" % hf], writes=["xT%d" % p])
                        else:
                            s.op("dve", lambda e, rr_=rr_, r=r, hf=hf: e.tensor_copy(out=XT[:, rr_ * 8:(rr_ + 1) * 8, r * 128:r * 128 + rws(r)],
                                                                                    in_=ptC[hf][:, :, 0:rws(r)]),
                                 reads=["ptC%d" % hf], writes=["xT%d" % p])

            def gateup(ex):
                p = ex % 2
                WG, WU, XT, HT = wg[p], wu[p], xT[p], hT[p]
                for j in range(4):
                    PA, PB = pab[j % 2]
                    na, nb = "pab%d0" % (j % 2), "pab%d1" % (j % 2)
                    for k in range(KD):
                        s.op("pe", lambda e, PA=PA, j=j, k=k: e.matmul(out=PA[:, 0:C], lhsT=WG[:, k, j * 128:(j + 1) * 128], rhs=XT[:, k, :],
                                                                       start=(k == 0), stop=(k == KD - 1)),
                             reads=["wg%d" % p, "xT%d" % p], writes=[na])
                    for k in range(KD):
                        s.op("pe", lambda e, PB=PB, j=j, k=k: e.matmul(out=PB[:, 0:C], lhsT=WU[:, k, j * 128:(j + 1) * 128], rhs=XT[:, k, :],
                                                                       start=(k == 0), stop=(k == KD - 1)),
                             reads=["wu%d" % p, "xT%d" % p], writes=[nb])
                    SL = sil[j % 2]
                    s.op("act", lambda e, PA=PA, SL=SL: e.activation(out=SL[:], in_=PA[:, 0:C], func=AF.Silu), reads=[na], writes=["sil%d" % (j % 2)])
                    s.op("dve", lambda e, PB=PB, SL=SL, j=j: e.tensor_tensor(out=HT[:, j, :], in0=PB[:, 0:C], in1=SL[:], op=ALU.mult),
                         reads=[nb, "sil%d" % (j % 2)], writes=["hT%d" % p])

            def down(ex):
                p = ex % 2
                WD, HT = wd[p], hT[p]
                for r in range(CT):
                    q = (ex * CT + r) % 4
                    YSb = ys[q]
                    for c in range(4):
                        PY = py[cnt["py"] % 2]
                        npn = "py%d" % (cnt["py"] % 2)
                        for j in range(4):
                            s.op("pe", lambda e, PY=PY, r=r, c=c, j=j: e.matmul(out=PY[0:rws(r), :], lhsT=HT[:, j, r * 128:r * 128 + rws(r)],
                                                                                rhs=WD[:, j, c * 512:(c + 1) * 512], start=(j == 0), stop=(j == 3)),
                                 reads=["hT%d" % p, "wd%d_%d" % (p, c // 2)], writes=[npn])
                        if cnt["py"] % 2 == 0:
                            s.op("act", lambda e, PY=PY, YSb=YSb, c=c, r=r: e.copy(out=YSb[0:rws(r), c * 512:(c + 1) * 512], in_=PY[0:rws(r), :]),
                                 reads=[npn], writes=["ys%d" % q])
                        else:
                            s.op("dve", lambda e, PY=PY, YSb=YSb, c=c, r=r: e.tensor_copy(out=YSb[0:rws(r), c * 512:(c + 1) * 512], in_=PY[0:rws(r), :]),
                                 reads=[npn], writes=["ys%d" % q])
                        cnt["py"] += 1
                    s.dma("sp", lambda e, YSb=YSb, r=r: e.dma_start(out=YS[ex * C + r * 128:ex * C + r * 128 + rws(r), :], in_=YSb[0:rws(r), :]),
                          reads=["ys%d" % q], writes=["YS_%d_%d" % (ex, r)], key="st_ys%d" % q)

            if not pre:
                load_w(0)
            load_x(0)
            transposes(0)
            for ex in range(E):
                if ex + 1 < E:
                    load_w(ex + 1)
                    load_x(ex + 1)
                gateup(ex)
                if ex + 1 < E:
                    transposes(ex + 1)
                down(ex)
            s.barrier()
            pc.close()

        def phase_D():
            pd = ExitStack()
            NS = 4
            ya = [sbt(pd, "ya%d" % i, [128, D], BF16) for i in range(NS)]
            yb = [sbt(pd, "yb%d" % i, [128, D], BF16) for i in range(NS)]
            hh = [sbt(pd, "hh%d" % i, [128, D], F32) for i in range(NS)]
            for i in range(NS):
                s.op("dve", lambda e, i=i: e.memset(ya[i][:], 0.0), writes=["ya%d" % i])
                s.op("pool", lambda e, i=i: e.memset(yb[i][:], 0.0), writes=["yb%d" % i])

            def loadsD(i):
                p = i % NS
                YA, YB, HH = ya[p], yb[p], hh[p]
                s.dma("pool", lambda e: e.indirect_dma_start(
                    out=YA[:], out_offset=None, in_=YS, in_offset=bass.IndirectOffsetOnAxis(ap=DEST[:, i, 0:1], axis=0),
                    bounds_check=bcreg(e), oob_is_err=False), writes=["ya%d" % p], key="ya%d" % p)
                s.dma("pool", lambda e: e.indirect_dma_start(
                    out=YB[:], out_offset=None, in_=YS, in_offset=bass.IndirectOffsetOnAxis(ap=DEST[:, i, 1:2], axis=0),
                    bounds_check=bcreg(e), oob_is_err=False), writes=["yb%d" % p], key="yb%d" % p)
                s.dma("sp", lambda e: e.dma_start(out=HH[:], in_=out[i * 128:(i + 1) * 128, :]), writes=["hh%d" % p], key="hh%d" % p)

            def combineD(i):
                p = i % NS
                YA, YB, HH = ya[p], yb[p], hh[p]
                s.op("dve", lambda e: e.scalar_tensor_tensor(out=HH[:], in0=YA[:], scalar=GATE[:, i, 0:1], in1=HH[:],
                                                             op0=ALU.mult, op1=ALU.add),
                     reads=["ya%d" % p, "hh%d" % p], writes=["hh%d" % p])
                s.op("dve", lambda e: e.scalar_tensor_tensor(out=HH[:], in0=YB[:], scalar=GATE[:, i, 1:2], in1=HH[:],
                                                             op0=ALU.mult, op1=ALU.add),
                     reads=["yb%d" % p, "hh%d" % p], writes=["hh%d" % p])
                s.dma("sp", lambda e: e.dma_start(out=out[i * 128:(i + 1) * 128, :], in_=HH[:]), reads=["hh%d" % p],
                      writes=["outD%d" % i], key="st_hh%d" % p)

            for i in range(NT + 2):
                if i < NT:
                    loadsD(i)
                if i - 2 >= 0:
                    combineD(i - 2)
            pd.close()

        pre = {}
        for ph, fn in (("A", phase_A), ("B", phase_B), ("C", phase_C), ("D", phase_D)):
            if ph == "B" and "B" in phases and "C" in phases:
                pbc = ExitStack()
                pre["wg"] = sbt(pbc, "wg0", [128, KD, DE], BF16)
                pre["wu"] = sbt(pbc, "wu0", [128, KD, DE], BF16)
                pre["wd"] = sbt(pbc, "wd0", [128, 4, D], BF16)
            if ph in phases:
                fn()
            if ph == "C" and pre:
                pbc.close()
        if debug:
            s.barrier()
            s.dma("sp", lambda e: e.dma_start(out=DBGD, in_=DEST[:]), key="dbgd")
            s.dma("sp", lambda e: e.dma_start(out=DBGG, in_=GATE[:]), key="dbgg")
        s.finish()
        info = run_schedule(nc, s, es)
    return nc, info


CAP = 384
_PROG_CACHE = {}


def make_in_map(xb, prm, consts):
    cst, cs, eoff = consts
    m = dict(prm)
    m["x"] = np.ascontiguousarray(xb)
    m["cst"] = cst
    m["cs"] = cs
    m["eoff"] = eoff
    return m


def prep_params(norm_mix, w_in, q_norm, k_norm, sinks, w_pool, pool_scale, w_out, norm_ffn,
                w_coarse, b_coarse, w_fine, b_fine, w_gate, w_up, w_down):
    f = lambda a: np.ascontiguousarray(np.asarray(a, dtype=np.float32))
    return dict(
        norm_mix=f(norm_mix).reshape(1, D), w_in=f(w_in).reshape(D, INW), q_norm=f(q_norm).reshape(1, HD),
        k_norm=f(k_norm).reshape(1, HD), sinks=f(sinks).reshape(1, NH), w_pool=f(w_pool).reshape(4, 256, 256),
        pool_scale=f(pool_scale).reshape(1, 1024), w_out=f(w_out).reshape(D, D), norm_ffn=f(norm_ffn).reshape(1, D),
        w_r=np.ascontiguousarray(np.concatenate([f(w_coarse).reshape(D, 4), f(w_fine).reshape(D, E)], axis=1)),
        b_r=np.ascontiguousarray(np.concatenate([f(b_coarse).reshape(1, 4), f(b_fine).reshape(1, E)], axis=1)),
        w_gate=f(w_gate).reshape(E, D, DE), w_up=f(w_up).reshape(E, D, DE), w_down=f(w_down).reshape(E, DE, D))


def kernel(x, norm_mix, w_in, q_norm, k_norm, sinks, w_pool, pool_scale, w_out, norm_ffn,
           w_coarse, b_coarse, w_fine, b_fine, w_gate, w_up, w_down):
    x = np.asarray(x, dtype=np.float32)
    B, S, _ = x.shape
    assert B == N_CORES
    prm = prep_params(norm_mix, w_in, q_norm, k_norm, sinks, w_pool, pool_scale, w_out, norm_ffn,
                      w_coarse, b_coarse, w_fine, b_fine, w_gate, w_up, w_down)
    key = (S, CAP)
    if key not in _PROG_CACHE:
        _PROG_CACHE[key] = build_program(S, CAP)[0]
    nc = _PROG_CACHE[key]
    consts = build_consts(S, CAP)
    in_maps = [make_in_map(x[b], prm, consts) for b in range(B)]
    res = run_bass_kernel_spmd(nc, in_maps, core_ids=list(range(B)))
    return np.stack([np.asarray(r["out"], dtype=np.float32) for r in res.results], axis=0)
```

```python
import numpy as np
from contextlib import ExitStack
import concourse.bass as bass
import concourse.mybir as mybir
from concourse.bass_utils import run_bass_kernel_spmd

F32 = mybir.dt.float32
BF16 = mybir.dt.bfloat16
I32 = mybir.dt.int32
AF = mybir.ActivationFunctionType
ALU = mybir.AluOpType
AX = mybir.AxisListType

D = 2048
KD = 16
INW = 2560
NH = 16
NKV = 4
HD = 64
E = 32
DE = 512
EPS = 1e-6
ROPE_THETA = 500000.0
POOL_WINDOWS = (2, 4, 8, 16)
N_CORES = 8

ENGINES = ("pe", "act", "dve", "pool", "sp")


class _Buf:
    __slots__ = ("last_w", "readers")

    def __init__(self):
        self.last_w = None
        self.readers = {}


class _Op:
    __slots__ = ("eng", "fn", "deps", "inc", "semval", "is_dma", "key")

    def __init__(self, eng, fn, is_dma, key):
        self.eng = eng
        self.fn = fn
        self.deps = []
        self.inc = False
        self.semval = None
        self.is_dma = is_dma
        self.key = key


class Sched:
    def __init__(self):
        self.ops = []
        self.bufs = {}
        self.last_eng = {}
        self.last_dma = {}
        self.barrier_deps = []
        self.final_waits = []

    def _b(self, name):
        b = self.bufs.get(name)
        if b is None:
            b = _Buf()
            self.bufs[name] = b
        return b

    def _add(self, eng, fn, reads, writes, is_dma, key):
        op = _Op(eng, fn, is_dma, key)
        deps = {}
        for r in reads:
            b = self._b(r)
            if b.last_w is not None:
                deps[id(b.last_w)] = b.last_w
        for w in writes:
            b = self._b(w)
            p = b.last_w
            if p is not None and (is_dma or p.is_dma or p.eng != eng):
                deps[id(p)] = p
            for p in b.readers.values():
                if is_dma or p.is_dma or p.eng != eng:
                    deps[id(p)] = p
        for p in self.barrier_deps:
            deps[id(p)] = p
        rk = ("d", key) if is_dma else eng
        for r in reads:
            self._b(r).readers[rk] = op
        for w in writes:
            b = self._b(w)
            b.last_w = op
            b.readers = {}
        op.deps = list(deps.values())
        for p in op.deps:
            p.inc = True
        self.ops.append(op)
        if is_dma:
            self.last_dma[key] = op
        else:
            self.last_eng[eng] = op
        return op

    def op(self, eng, fn, reads=(), writes=()):
        return self._add(eng, fn, reads, writes, False, None)

    def dma(self, eng, fn, reads=(), writes=(), key=None):
        op = self._add(eng, fn, reads, writes, True, key)
        op.inc = True
        return op

    def barrier(self):
        self.barrier_deps = list(self.last_eng.values()) + list(self.last_dma.values())
        self.bufs = {}

    def finish(self):
        self.final_waits = list(self.last_dma.values())


def run_schedule(nc, sched, es):
    eng_sem = {e: es.enter_context(nc.semaphore("s_" + e)) for e in ENGINES}
    dma_sem, dma_cnt = {}, {}
    eng_cnt = {e: 0 for e in ENGINES}
    for op in sched.ops:
        if op.is_dma:
            if op.key not in dma_sem:
                dma_sem[op.key] = es.enter_context(nc.semaphore("d_" + op.key))
                dma_cnt[op.key] = 0
            dma_cnt[op.key] += 16
            op.semval = dma_cnt[op.key]
        elif op.inc:
            eng_cnt[op.eng] += 1
            op.semval = eng_cnt[op.eng]
    per_eng = {e: [o for o in sched.ops if o.eng == e] for e in ENGINES}

    def sem_of(p):
        return dma_sem[p.key] if p.is_dma else eng_sem[p.eng]

    def body(ename, eng):
        waited = {}

        def wait(p):
            s = sem_of(p)
            if waited.get(id(s), 0) >= p.semval:
                return
            eng.wait_ge(s, p.semval)
            waited[id(s)] = p.semval

        for op in per_eng[ename]:
            need = {}
            for p in op.deps:
                k = id(sem_of(p))
                if k not in need or need[k].semval < p.semval:
                    need[k] = p
            for p in need.values():
                wait(p)
            ins = op.fn(eng)
            if op.is_dma:
                ins.then_inc(dma_sem[op.key], 16)
            elif op.inc:
                ins.then_inc(eng_sem[ename], 1)
        if ename == "sp":
            for p in sched.final_waits:
                wait(p)

    with nc.Block() as block:
        @block.tensor
        def _(e):
            body("pe", e)

        @block.scalar
        def _(e):
            body("act", e)

        @block.vector
        def _(e):
            body("dve", e)

        @block.gpsimd
        def _(e):
            body("pool", e)

        @block.sync
        def _(e):
            body("sp", e)
    return dict(n_ops=len(sched.ops), n_dma_sems=len(dma_sem), eng_cnt=eng_cnt)


C_ID, C_TRI, C_ONE, C_MPREV, C_MCUR, C_POOL = 0, 128, 256, 384, 512, 640
NCST = 640 + 12 * 128


def build_consts(S, C):
    NT = S // 128
    cst = np.zeros((128, NCST), np.float32)
    idx = np.arange(128)
    cst[:, C_ID:C_ID + 128] = np.eye(128, dtype=np.float32)
    cst[:, C_TRI:C_TRI + 128] = (idx[:, None] < idx[None, :])
    cst[:, C_ONE:C_ONE + 128] = 1.0
    cst[:, C_MPREV:C_MPREV + 128] = (idx[:, None] > idx[None, :])
    cst[:, C_MCUR:C_MCUR + 128] = (idx[:, None] <= idx[None, :])
    tp = idx[:, None]
    t = idx[None, :]
    for g, w in enumerate(POOL_WINDOWS):
        mcur = ((tp <= t) & (tp > t - w)) / float(w) - (tp == t)
        mprev = (tp >= t + 129 - w) / float(w)
        cnt = np.minimum(t + 1, w).astype(np.float32)
        m0 = ((tp <= t) & (tp > t - w)) / cnt - (tp == t)
        cst[:, C_POOL + (0 + g) * 128:C_POOL + (1 + g) * 128] = mcur
        cst[:, C_POOL + (4 + g) * 128:C_POOL + (5 + g) * 128] = mprev
        cst[:, C_POOL + (8 + g) * 128:C_POOL + (9 + g) * 128] = m0
    pos = np.arange(S, dtype=np.float32)
    rot = HD // 4
    inv_freq = (ROPE_THETA ** (-np.arange(0, rot, 2, dtype=np.float32) / rot)).astype(np.float32)
    ang = pos[:, None] * inv_freq[None, :]
    cs = np.zeros((128, NT, 16), np.float32)
    cs[:, :, 0:8] = np.cos(ang).astype(np.float32).reshape(NT, 128, 8).transpose(1, 0, 2)
    cs[:, :, 8:16] = np.sin(ang).astype(np.float32).reshape(NT, 128, 8).transpose(1, 0, 2)
    eoff = np.tile((np.arange(E, dtype=np.float32) * C)[None, :], (128, 1))
    return cst, cs.reshape(128, NT * 16), eoff


def build_program(S, C, phases="ABCD", debug=False):
    NT = S // 128
    CT = (C + 127) // 128

    def rws(r):
        return min(128, C - r * 128)
    nc = bass.Bass("TRN2", target_bir_lowering=False)

    def din(name, shape, dt=F32):
        return nc.dram_tensor(name, shape, dt, kind="ExternalInput").ap()

    x = din("x", [S, D])
    norm_mix = din("norm_mix", [1, D])
    w_in = din("w_in", [D, INW])
    q_norm = din("q_norm", [1, HD])
    k_norm = din("k_norm", [1, HD])
    sinks = din("sinks", [1, NH])
    w_pool = din("w_pool", [4, 256, 256])
    pool_scale = din("pool_scale", [1, 1024])
    w_out = din("w_out", [D, D])
    norm_ffn = din("norm_ffn", [1, D])
    w_r = din("w_r", [D, 36])
    b_r = din("b_r", [1, 36])
    w_gate = din("w_gate", [E, D, DE])
    w_up = din("w_up", [E, D, DE])
    w_down = din("w_down", [E, DE, D])
    cst_d = din("cst", [128, NCST])
    cs_d = din("cs", [128, NT * 16])
    eoff_d = din("eoff", [128, E])
    out = nc.dram_tensor("out", [S, D], F32, kind="ExternalOutput").ap()
    dk = dict(kind="ExternalOutput") if debug else {}
    MIX = nc.dram_tensor("MIX", [S, D], BF16, **dk).ap()
    XS = nc.dram_tensor("XS", [E * C, D], BF16, **dk).ap()
    YS = nc.dram_tensor("YS", [E * C, D], BF16, **dk).ap()
    if debug:
        DBGD = nc.dram_tensor("DBGD", [128, S // 128, 2], I32, kind="ExternalOutput").ap()
        DBGG = nc.dram_tensor("DBGG", [128, S // 128, 2], F32, kind="ExternalOutput").ap()

    s = Sched()
    taps = {}
    regcache = {}

    def bcreg(e):
        if "bc" not in regcache:
            regcache["bc"] = e.to_reg(E * C - 1)
        return regcache["bc"]

    def tap(name, ap, reads, cond=True):
        if not (debug and cond):
            return
        shp = list(ap.shape)
        t = nc.dram_tensor("T_" + name, shp, ap.dtype, kind="ExternalOutput").ap()
        taps[name] = t
        s.dma("sp", lambda e: e.dma_start(out=t, in_=ap), reads=reads, key="tap")

    with ExitStack() as es:
        def sbt(stack, name, shape, dt):
            return stack.enter_context(nc.sbuf_tensor("sb_" + name, shape, dt))

        def pst(stack, name, shape, dt):
            return stack.enter_context(nc.psum_tensor("ps_" + name, shape, dt))

        cst = sbt(es, "cst", [128, NCST], BF16)
        DEST = sbt(es, "DEST", [128, NT, 2], I32)
        GATE = sbt(es, "GATE", [128, NT, 2], F32)
        base = sbt(es, "base", [128, E], F32)
        eoff = sbt(es, "eoff", [128, E], F32)
        for a, b in ((0, 1024), (1024, NCST)):
            s.dma("pool", lambda e, a=a, b=b: e.dma_start(out=cst[:, a:b], in_=cst_d[:, a:b]), writes=["cst_%d" % a], key="cst")
        s.op("pool", lambda e: e.memset(DEST[:], 0), reads=["cst_0", "cst_1024"], writes=["cst"])
        s.dma("sp", lambda e: e.dma_start(out=eoff[:], in_=eoff_d), writes=["eoff"], key="eoff")
        s.op("dve", lambda e: e.memset(base[:], 0.0), writes=["base"])
        epsb = sbt(es, "epsb", [128, 1], F32)
        s.op("dve", lambda e: e.memset(epsb[:], EPS), writes=["epsb"])
        ident = cst[:, C_ID:C_ID + 128]
        tri = cst[:, C_TRI:C_TRI + 128]
        ones = cst[:, C_ONE:C_ONE + 128]
        mask2 = cst[:, C_MPREV:C_MPREV + 256].rearrange("p (a q) -> p a q", a=2)

        def poolm(kind, g):
            o = C_POOL + (kind * 4 + g) * 128
            return cst[:, o:o + 128]

        def phase_A():
            pa = ExitStack()
            w_in_bf = sbt(pa, "w_in_bf", [128, KD, INW], BF16)
            gmix = sbt(pa, "gmix", [128, D], F32)
            qg = sbt(pa, "qg", [128, HD], F32)
            kg = sbt(pa, "kg", [128, HD], F32)
            gqk = sbt(pa, "gqk", [128, 20, HD], F32)
            esink = sbt(pa, "esink", [128, NH], F32)
            pscale = sbt(pa, "pscale", [128, 1024], F32)
            wpool_bf = sbt(pa, "wpool_bf", [128, 4, 2, 256], BF16)
            cs = sbt(pa, "cs", [128, NT, 16], F32)
            xt = [sbt(pa, "xtA%d" % i, [128, D], F32) for i in range(2)]
            junk = sbt(pa, "junkA", [128, D], BF16)
            ss = sbt(pa, "ssA", [128, 1], F32)
            std = sbt(pa, "stdA", [128, 1], F32)
            rstd = sbt(pa, "rstdA", [128, 1], F32)
            hn = [sbt(pa, "hnA%d" % i, [128, D], BF16) for i in range(2)]
            hnT = sbt(pa, "hnTA", [128, KD, 128], BF16)
            qk32 = [sbt(pa, "qk32_%d" % i, [128, 1280], F32) for i in range(2)]
            sq32 = sbt(pa, "sq32", [128, 1280], F32)
            ssq = sbt(pa, "ssq", [128, 20], F32)
            stq = sbt(pa, "stq", [128, 20], F32)
            rq = sbt(pa, "rq", [128, 20], F32)
            qn32 = sbt(pa, "qn32", [128, 20, HD], F32)
            qg32 = sbt(pa, "qg32", [128, 20, HD], F32)
            tr = [sbt(pa, "tr%d" % i, [128, 20, 8], F32) for i in range(4)]
            qkb = sbt(pa, "qkb", [128, 20, HD], BF16)
            kd = sbt(pa, "kd", [128, 4, 2, HD], BF16)
            qT = sbt(pa, "qT", [128, 8, 128], BF16)
            kT = [sbt(pa, "kT%d" % i, [128, 4, 128], BF16) for i in range(2)]
            vv = [sbt(pa, "vv%d" % i, [128, 4, 65], BF16) for i in range(3)]
            uu = [sbt(pa, "uu%d" % i, [128, 1024], BF16) for i in range(3)]
            PT = [[sbt(pa, "PT%d%d" % (a, b), [128, 2, 2, 128], BF16) for b in range(2)] for a in range(2)]
            den = sbt(pa, "den", [128, 8], F32)
            rden = sbt(pa, "rden", [128, 8], F32)
            dTs = sbt(pa, "dTs", [128, 8, 128], BF16)
            mixed = [sbt(pa, "mixed%d" % i, [128, D], BF16) for i in range(2)]
            pj = pst(pa, "pj", [128, 5, 512], F32)
            pt = pst(pa, "ptA", [128, 8, 128], BF16)
            sc = [pst(pa, "sc%d" % i, [128, 2, 2, 128], F32) for i in range(2)]
            pjf = pj[:].rearrange("p a b -> p (a b)")

            for c in range(5):
                s.dma("pool", lambda e, c=c: e.dma_start(
                    out=w_in_bf[:, :, c * 512:(c + 1) * 512],
                    in_=w_in[:, c * 512:(c + 1) * 512].rearrange("(k p) n -> p k n", p=128)),
                    writes=["win%d" % c], key="win%d" % c)
            s.dma("sp", lambda e: e.dma_start(out=gmix[:], in_=norm_mix.partition_broadcast(128)), writes=["gmix"], key="gmix")
            s.dma("sp", lambda e: e.dma_start(out=qg[:], in_=q_norm.partition_broadcast(128)), writes=["qg"], key="qg")
            s.dma("sp", lambda e: e.dma_start(out=kg[:], in_=k_norm.partition_broadcast(128)), writes=["kg"], key="kg")
            s.dma("sp", lambda e: e.dma_start(out=esink[:], in_=sinks.partition_broadcast(128)), writes=["esink"], key="esink")
            s.dma("sp", lambda e: e.dma_start(out=pscale[:], in_=pool_scale.partition_broadcast(128)), writes=["pscale"], key="pscale")
            s.dma("sp", lambda e: e.dma_start(out=cs[:], in_=cs_d.rearrange("p (t c) -> p t c", c=16)), writes=["cs"], key="cs")
            s.dma("pool", lambda e: e.dma_start(out=wpool_bf[:], in_=w_pool.rearrange("g (c p) d -> p g c d", p=128)),
                  writes=["wpool"], key="wpool")
            s.op("dve", lambda e: e.tensor_copy(out=gqk[:, 0:16, :], in_=qg[:].unsqueeze(1).to_broadcast([128, 16, HD])),
                 reads=["qg"], writes=["gqk"])
            s.op("dve", lambda e: e.tensor_copy(out=gqk[:, 16:20, :], in_=kg[:].unsqueeze(1).to_broadcast([128, 4, HD])),
                 reads=["kg", "gqk"], writes=["gqk"])
            s.op("act", lambda e: e.activation(out=esink[:], in_=esink[:], func=AF.Exp), reads=["esink"], writes=["esink"])
            for i in range(3):
                s.op("pool", lambda e, i=i: e.memset(vv[i][:], 1.0), writes=["vv%d" % i])

            def F1(i):
                p = i % 2
                XT, HN = xt[p], hn[p]
                nxt, nhn = "xtA%d" % p, "hnA%d" % p
                s.dma("sp", lambda e: e.dma_start(out=XT[:], in_=x[i * 128:(i + 1) * 128, :]), writes=[nxt], key=nxt)
                s.op("act", lambda e: e.activation(out=junk[:], in_=XT[:], func=AF.Square, accum_out=ss[:]),
                     reads=[nxt], writes=["junk", "ss"])
                s.op("act", lambda e: e.activation(out=std[:], in_=ss[:], func=AF.Ln, scale=1.0 / D, bias=epsb[:, 0:1]),
                     reads=["ss", "epsb"], writes=["std"])
                s.op("act", lambda e: e.activation(out=rstd[:], in_=std[:], func=AF.Exp, scale=-0.5), reads=["std"], writes=["rstd"])
                s.op("dve", lambda e: e.scalar_tensor_tensor(out=HN[:], in0=XT[:], scalar=rstd[:, 0:1], in1=gmix[:],
                                                             op0=ALU.mult, op1=ALU.mult),
                     reads=[nxt, "rstd", "gmix"], writes=[nhn])

            def F2(i, part):
                p = i % 2
                HN = hn[p]
                nhn = "hnA%d" % p
                VV, UU, QK = vv[i % 3], uu[i % 3], qk32[p]
                nvv, nuu, nqk = "vv%d" % (i % 3), "uu%d" % (i % 3), "qk32_%d" % p
                if part == 0:
                    for r in range(2):
                        for b in range(8):
                            s.op("pe", lambda e, r=r, b=b: e.transpose(out=pt[:, b, :], in_=HN[:, (r * 8 + b) * 128:(r * 8 + b + 1) * 128],
                                                                       identity=ident),
                                 reads=[nhn, "cst"], writes=["pt"])
                        s.op("act", lambda e, r=r: e.copy(out=hnT[:, r * 8:(r + 1) * 8, :], in_=pt[:]), reads=["pt"], writes=["hnT%d" % r])
                    return
                def chunk(c):
                    for k in range(KD):
                        s.op("pe", lambda e, k=k: e.matmul(out=pj[:, c, :], lhsT=hnT[:, k, :], rhs=w_in_bf[:, k, c * 512:(c + 1) * 512],
                                                           start=(k == 0), stop=(k == KD - 1)),
                             reads=["hnT%d" % (k // 8), "win%d" % c], writes=["pj%d" % c])

                def evac_qkv():
                    s.op("act", lambda e: e.copy(out=QK[:], in_=pjf[:, 0:1280]), reads=[], writes=["pj0", "pj1", "pj2", nqk])
                    s.op("act", lambda e: e.copy(out=VV[:, :, 0:HD], in_=pjf[:, 1280:1536].rearrange("p (h d) -> p h d", d=HD)),
                         reads=[], writes=["pj2", nvv])

                def evac_u():
                    s.op("act", lambda e: e.copy(out=UU[:], in_=pjf[:, 1536:2560]), reads=[], writes=["pj3", "pj4", nuu])

                return [lambda: chunk(0), lambda: chunk(1), lambda: (chunk(2), evac_qkv()), lambda: (chunk(3), chunk(4), evac_u())]

            def QKC(i):
                p = i % 2
                QK = qk32[p]
                nqk = "qk32_%d" % p
                s.op("act", lambda e: e.activation(out=sq32[:], in_=QK[:], func=AF.Square), reads=[nqk], writes=["sq32"])
                s.op("pool", lambda e: e.tensor_tensor(out=qg32[:], in0=QK[:].rearrange("p (h d) -> p h d", d=HD), in1=gqk[:], op=ALU.mult),
                     reads=[nqk, "gqk"], writes=["qg32"])
                s.op("dve", lambda e: e.tensor_reduce(out=ssq[:], in_=sq32[:].rearrange("p (h d) -> p h d", d=HD), axis=AX.X, op=ALU.add),
                     reads=["sq32"], writes=["ssq"])
                s.op("act", lambda e: e.activation(out=stq[:], in_=ssq[:], func=AF.Ln, scale=1.0 / HD, bias=epsb[:, 0:1]),
                     reads=["ssq", "epsb"], writes=["stq"])
                s.op("act", lambda e: e.activation(out=rq[:], in_=stq[:], func=AF.Exp, scale=-0.5), reads=["stq"], writes=["rq"])
                s.op("dve", lambda e: e.tensor_tensor(out=qn32[:], in0=qg32[:],
                                                       in1=rq[:].unsqueeze(2).to_broadcast([128, 20, HD]), op=ALU.mult),
                     reads=["qg32", "rq"], writes=["qn32"])
                cosb = cs[:, i, 0:8].unsqueeze(1).to_broadcast([128, 20, 8])
                sinb = cs[:, i, 8:16].unsqueeze(1).to_broadcast([128, 20, 8])
                s.op("dve", lambda e: e.tensor_tensor(out=tr[0][:], in0=qn32[:, :, 0:8], in1=cosb, op=ALU.mult),
                     reads=["qn32", "cs"], writes=["tr0"])
                s.op("dve", lambda e: e.tensor_tensor(out=tr[1][:], in0=qn32[:, :, 8:16], in1=sinb, op=ALU.mult),
                     reads=["qn32", "cs"], writes=["tr1"])
                s.op("dve", lambda e: e.tensor_tensor(out=tr[2][:], in0=qn32[:, :, 8:16], in1=cosb, op=ALU.mult),
                     reads=["qn32", "cs"], writes=["tr2"])
                s.op("dve", lambda e: e.tensor_tensor(out=tr[3][:], in0=qn32[:, :, 0:8], in1=sinb, op=ALU.mult),
                     reads=["qn32", "cs"], writes=["tr3"])
                s.op("dve", lambda e: e.tensor_tensor(out=qkb[:, :, 0:8], in0=tr[0][:], in1=tr[1][:], op=ALU.subtract),
                     reads=["tr0", "tr1"], writes=["qkb_a"])
                s.op("dve", lambda e: e.tensor_tensor(out=qkb[:, :, 8:16], in0=tr[2][:], in1=tr[3][:], op=ALU.add),
                     reads=["tr2", "tr3"], writes=["qkb_b"])
                s.op("pool", lambda e: e.tensor_copy(out=qkb[:, :, 16:HD], in_=qn32[:, :, 16:HD]), reads=["qn32"], writes=["qkb_c"])
                s.op("dve", lambda e: e.tensor_copy(out=kd[:], in_=qkb[:, 16:20, :].unsqueeze(2).to_broadcast([128, 4, 2, HD])),
                     reads=["qkb_a", "qkb_b", "qkb_c"], writes=["kd"])

            def BK(i):
                p, pp = i % 2, 1 - (i % 2)
                KT, KTp, MX = kT[p], kT[pp], mixed[p]
                nkt, nktp, nmx = "kT%d" % p, "kT%d" % pp, "mixed%d" % p
                VV, VVp, UU, UUp = vv[i % 3], vv[(i - 1) % 3], uu[i % 3], uu[(i - 1) % 3]
                nvv, nvvp, nuu, nuup = "vv%d" % (i % 3), "vv%d" % ((i - 1) % 3), "uu%d" % (i % 3), "uu%d" % ((i - 1) % 3)
                kbs = [1] if i == 0 else [0, 1]
                nkb = len(kbs)
                qkbf = qkb[:].rearrange("p h d -> p (h d)")
                kdf = kd[:].rearrange("p g t d -> p (g t d)")

                def tq():
                    for b in range(8):
                        s.op("pe", lambda e, b=b: e.transpose(out=pt[:, b, :], in_=qkbf[:, b * 128:(b + 1) * 128], identity=ident),
                             reads=["qkb_a", "qkb_b", "qkb_c", "cst"], writes=["pt"])
                    s.op("dve", lambda e: e.tensor_copy(out=qT[:], in_=pt[:]), reads=["pt"], writes=["qT"])
                    for g in range(4):
                        s.op("pe", lambda e, g=g: e.transpose(out=pt[:, g, :], in_=kdf[:, g * 128:(g + 1) * 128], identity=ident),
                             reads=["kd", "cst"], writes=["pt"])
                    s.op("act", lambda e: e.copy(out=KT[:], in_=pt[:, 0:4, :]), reads=["pt"], writes=[nkt])

                def scores(g):
                    for kb in kbs:
                        ktile, nk = (KTp, nktp) if kb == 0 else (KT, nkt)
                        for hf in range(2):
                            s.op("pe", lambda e, kb=kb, hf=hf, ktile=ktile: e.matmul(
                                out=sc[hf][:, kb, :, :], lhsT=ktile[hf * 64:(hf + 1) * 64, g, :],
                                rhs=qT[hf * 64:(hf + 1) * 64, 2 * g:2 * g + 2, :], start=True, stop=True),
                                reads=[nk, "qT"], writes=["sc%d" % hf])
                    k0 = kbs[0]
                    for hf in range(2):
                        P = PT[g % 2][hf]
                        nP = "PT%d%d" % (g % 2, hf)
                        s.op("act", lambda e, hf=hf, P=P: e.activation(out=P[:, k0:2], in_=sc[hf][:, k0:2], func=AF.Exp, scale=HD ** -0.5),
                             reads=[], writes=["sc%d" % hf, nP])
                        s.op("pool" if hf == 1 else "dve", lambda e, P=P: e.tensor_tensor(
                            out=P[:, k0:2], in0=P[:, k0:2], in1=mask2[:, k0:2].unsqueeze(2).to_broadcast([128, 2 - k0, 2, 128]), op=ALU.mult),
                            reads=[nP, "cst"], writes=[nP])

                PVB = (0, 1, 2)

                def pv(g):
                    for blk in range(2):
                        for hf in range(2):
                            h = 4 * g + 2 * blk + hf
                            bnk, j = PVB[h // 7], h % 7
                            o = pj[:, bnk, j * 65:(j + 1) * 65]
                            P = PT[g % 2][hf]
                            for n, kb in enumerate(kbs):
                                vt, nv = (VVp, nvvp) if kb == 0 else (VV, nvv)
                                s.op("pe", lambda e, o=o, P=P, kb=kb, blk=blk, vt=vt, n=n: e.matmul(
                                    out=o, lhsT=P[:, kb, blk, :], rhs=vt[:, g, :], start=(n == 0), stop=(n == nkb - 1)),
                                    reads=["PT%d%d" % (g % 2, hf), nv], writes=["pj%d" % bnk])

                def pv_evac(rnd):
                    bnk = PVB[rnd]
                    h0 = rnd * 7
                    nh = min(7, NH - h0)
                    v = pj[:, bnk, 0:nh * 65].rearrange("p (h d) -> p h d", d=65)
                    s.op("dve", lambda e: e.tensor_tensor(out=den[:, 0:nh], in0=v[:, :, 64], in1=esink[:, h0:h0 + nh], op=ALU.add),
                         reads=["esink"], writes=["pj%d" % bnk, "den"])
                    s.op("dve", lambda e: e.reciprocal(out=rden[:, 0:nh], in_=den[:, 0:nh]), reads=["den"], writes=["rden"])
                    s.op("dve", lambda e: e.tensor_tensor(
                        out=MX[:, h0 * HD:(h0 + nh) * HD].rearrange("p (h d) -> p h d", d=HD), in0=v[:, :, 0:HD],
                        in1=rden[:, 0:nh].unsqueeze(2).to_broadcast([128, nh, HD]), op=ALU.mult),
                        reads=["rden"], writes=["pj%d" % bnk, nmx + "_a%d" % rnd])

                def pool_dT():
                    for cc in range(8):
                        g = cc // 2
                        o = pj[:, 3 + cc // 4, (cc % 4) * 128:(cc % 4 + 1) * 128]
                        if i > 0:
                            s.op("pe", lambda e, o=o, cc=cc, g=g: e.matmul(out=o, lhsT=UUp[:, cc * 128:(cc + 1) * 128], rhs=poolm(1, g),
                                                                           start=True, stop=False),
                                 reads=[nuup, "cst"], writes=["pj%d" % (3 + cc // 4)])
                        s.op("pe", lambda e, o=o, cc=cc, g=g: e.matmul(out=o, lhsT=UU[:, cc * 128:(cc + 1) * 128],
                                                                       rhs=poolm(2 if i == 0 else 0, g), start=(i == 0), stop=True),
                             reads=[nuu, "cst"], writes=["pj%d" % (3 + cc // 4)])
                    s.op("act", lambda e: e.copy(out=dTs[:].rearrange("p a b -> p (a b)"), in_=pjf[:, 1536:2560]),
                         reads=[], writes=["pj3", "pj4", "dTs"])

                def pool_y():
                    for g in range(4):
                        for c2 in range(2):
                            s.op("pe", lambda e, g=g, c2=c2: e.matmul(out=pjf[:, 1536 + g * 256:1536 + (g + 1) * 256], lhsT=dTs[:, 2 * g + c2, :],
                                                                      rhs=wpool_bf[:, g, c2, :], start=(c2 == 0), stop=(c2 == 1)),
                                 reads=["dTs", "wpool"], writes=["pj%d" % (3 + g // 2)])
                    s.op("dve", lambda e: e.tensor_tensor(out=MX[:, 1024:2048], in0=pjf[:, 1536:2560], in1=pscale[:], op=ALU.mult),
                         reads=["pscale"], writes=["pj3", "pj4", nmx + "_p"])

                def sec0():
                    tq()

                def sec1():
                    scores(0)
                    pool_dT()

                def sec2():
                    scores(1)
                    pool_y()

                def sec3():
                    pv(0)
                    scores(2)
                    pv(1)
                    pv_evac(0)

                def sec4():
                    scores(3)
                    pv(2)
                    pv(3)
                    pv_evac(1)
                    pv_evac(2)

                return [sec0, sec1, sec2, sec3, sec4]

            def storeA(i):
                MX = mixed[i % 2]
                nmx = "mixed%d" % (i % 2)
                s.dma("sp", lambda e: e.dma_start(out=MIX[i * 128:(i + 1) * 128, :], in_=MX[:]),
                      reads=[nmx + "_a0", nmx + "_a1", nmx + "_a2", nmx + "_p"], writes=["MIX%d" % i], key="st_" + nmx)

            F1(0)
            if NT > 1:
                F1(1)
            F2(0, 0)
            for f in F2(0, 1):
                f()
            for st in range(NT):
                if st + 1 < NT:
                    F2(st + 1, 0)
                if st + 2 < NT:
                    F1(st + 2)
                if st >= 1:
                    storeA(st - 1)
                QKC(st)
                if st + 1 < NT:
                    for f in F2(st + 1, 1):
                        f()
                for f in BK(st):
                    f()
            storeA(NT - 1)
            s.barrier()
            pa.close()

        def phase_B():
            pb = ExitStack()
            w_out_bf = sbt(pb, "w_out_bf", [128, KD, D], BF16)
            gffn = sbt(pb, "gffn", [128, D], F32)
            wr_bf = sbt(pb, "wr_bf", [128, KD, 36], BF16)
            brb = sbt(pb, "brb", [128, 36], F32)
            xt = [sbt(pb, "xtB%d" % i, [128, D], F32) for i in range(3)]
            mx = [sbt(pb, "mxB%d" % i, [128, D], BF16) for i in range(3)]
            mxT = sbt(pb, "mxT", [128, KD, 128], BF16)
            junk = sbt(pb, "junkB", [128, D], BF16)
            ss = sbt(pb, "ssB", [128, 1], F32)
            std = sbt(pb, "stdB", [128, 1], F32)
            rstd = sbt(pb, "rstdB", [128, 1], F32)
            hn2 = [sbt(pb, "hn2_%d" % i, [128, D], BF16) for i in range(3)]
            hn2T = sbt(pb, "hn2T", [128, KD, 128], BF16)
            lg = sbt(pb, "lg", [128, 36], F32)
            cmax = sbt(pb, "cmax", [128, 1], F32)
            ncmax = sbt(pb, "ncmax", [128, 1], F32)
            ohg = sbt(pb, "ohg", [128, 4], F32)
            ec = sbt(pb, "ec", [128, 4], F32)
            sumc = sbt(pb, "sumc", [128, 1], F32)
            pgrp = sbt(pb, "pgrp", [128, 1], F32)
            tmp48 = sbt(pb, "tmp48", [128, 4, 8], F32)
            fsel = sbt(pb, "fsel", [128, 8], F32)
            m1 = sbt(pb, "m1", [128, 1], F32)
            oh1 = sbt(pb, "oh1", [128, 8], F32)
            msk = sbt(pb, "msk", [128, 8], F32)
            m2 = sbt(pb, "m2", [128, 1], F32)
            oh2 = sbt(pb, "oh2", [128, 8], F32)
            d21 = sbt(pb, "d21", [128, 1], F32)
            e21 = sbt(pb, "e21", [128, 1], F32)
            rr = sbt(pb, "rr", [128, 1], F32)
            E1 = sbt(pb, "E1", [128, 4, 8], F32)
            E2 = sbt(pb, "E2", [128, 4, 8], F32)
            Abf = sbt(pb, "Abf", [128, E], BF16)
            posv = sbt(pb, "posv", [128, E], F32)
            ov = sbt(pb, "ov", [128, E], F32)
            tmp32 = sbt(pb, "tmp32", [128, E], F32)
            dd = sbt(pb, "dd", [128, 2], F32)
            po = pst(pb, "po", [128, 4, 512], F32)
            ptB = pst(pb, "ptB", [128, 8, 128], BF16)
            ptB2 = pst(pb, "ptB2", [128, 8, 128], BF16)
            plg = pst(pb, "plg", [128, 512], F32)
            prk = pst(pb, "prk", [128, 512], F32)
            pof = po[:].rearrange("p a b -> p (a b)")

            for c in range(4):
                s.dma("pool", lambda e, c=c: e.dma_start(
                    out=w_out_bf[:, :, c * 512:(c + 1) * 512],
                    in_=w_out[:, c * 512:(c + 1) * 512].rearrange("(k p) n -> p k n", p=128)),
                    writes=["wout%d" % c], key="wout%d" % c)
            s.dma("pool", lambda e: e.dma_start(out=wr_bf[:], in_=w_r.rearrange("(k p) n -> p k n", p=128)), writes=["wr"], key="wr")
            s.dma("sp", lambda e: e.dma_start(out=gffn[:], in_=norm_ffn.partition_broadcast(128)), writes=["gffn"], key="gffn")
            s.dma("sp", lambda e: e.dma_start(out=brb[:], in_=b_r.partition_broadcast(128)), writes=["brb"], key="brb")
            if pre:
                s.dma("pool", lambda e: e.dma_start(out=pre["wg"][:], in_=w_gate[0].rearrange("(k p) n -> p k n", p=128)),
                      writes=["wg0"], key="wg0")
                s.dma("pool", lambda e: e.dma_start(out=pre["wu"][:], in_=w_up[0].rearrange("(k p) n -> p k n", p=128)),
                      writes=["wu0"], key="wu0")
                for hh_ in range(2):
                    s.dma("pool", lambda e, hh_=hh_: e.dma_start(
                        out=pre["wd"][:, :, hh_ * 1024:(hh_ + 1) * 1024],
                        in_=w_down[0][:, hh_ * 1024:(hh_ + 1) * 1024].rearrange("(j p) n -> p j n", p=128)),
                        writes=["wd0_%d" % hh_], key="wd0_%d" % hh_)

            ptBs = [ptB, ptB2]
            tcnt = [0]

            def transposeB(src, nsrc, dst, ndst):
                for r in range(2):
                    bk = tcnt[0] % 2
                    tcnt[0] += 1
                    PTb = ptBs[bk]
                    for b in range(8):
                        s.op("pe", lambda e, r=r, b=b, PTb=PTb: e.transpose(out=PTb[:, b, :], in_=src[:, (r * 8 + b) * 128:(r * 8 + b + 1) * 128],
                                                                            identity=ident),
                             reads=[nsrc, "cst"], writes=["ptB%d" % bk])
                    s.op("act", lambda e, r=r, PTb=PTb: e.copy(out=dst[:, r * 8:(r + 1) * 8, :], in_=PTb[:]), reads=["ptB%d" % bk],
                         writes=[ndst + "%d" % r])

            V = lambda fn, r, w: s.op("dve", fn, reads=r, writes=w)
            E1f = E1[:].rearrange("p g e -> p (g e)")
            E2f = E2[:].rearrange("p g e -> p (g e)")

            def loadsB(i):
                p = i % 3
                XT, MXb = xt[p], mx[p]
                nxt, nmx = "xtB%d" % p, "mxB%d" % p
                s.dma("sp", lambda e: e.dma_start(out=XT[:], in_=x[i * 128:(i + 1) * 128, :]), writes=[nxt], key=nxt)
                s.dma("sp", lambda e: e.dma_start(out=MXb[:], in_=MIX[i * 128:(i + 1) * 128, :]), writes=[nmx], key=nmx)

            def storeB(i):
                p = i % 3
                XT = xt[p]
                nxt = "xtB%d" % p
                s.dma("sp", lambda e: e.dma_start(out=out[i * 128:(i + 1) * 128, :], in_=XT[:]), reads=[nxt],
                      writes=["out%d" % i], key="st_" + nxt)

            def G1(i, part):
                p = i % 3
                XT, MXb = xt[p], mx[p]
                nxt, nmx = "xtB%d" % p, "mxB%d" % p
                if part == 0:
                    transposeB(MXb, nmx, mxT, "mxT")
                    return
                for c in range(4):
                    for k in range(KD):
                        s.op("pe", lambda e, c=c, k=k: e.matmul(out=po[:, c, :], lhsT=mxT[:, k, :], rhs=w_out_bf[:, k, c * 512:(c + 1) * 512],
                                                                start=(k == 0), stop=(k == KD - 1)),
                             reads=["mxT%d" % (k // 8), "wout%d" % c], writes=["po%d" % c])
                s.op("dve", lambda e: e.tensor_tensor(out=XT[:], in0=pof, in1=XT[:], op=ALU.add),
                     reads=["po0", "po1", "po2", "po3", nxt], writes=[nxt])

            def G2a(i):
                p = i % 3
                XT, H2 = xt[i % 3], hn2[p]
                nxt, nh2 = "xtB%d" % (i % 3), "hn2_%d" % p
                s.op("act", lambda e: e.activation(out=junk[:], in_=XT[:], func=AF.Square, accum_out=ss[:]),
                     reads=[nxt], writes=["junk", "ss"])
                s.op("act", lambda e: e.activation(out=std[:], in_=ss[:], func=AF.Ln, scale=1.0 / D, bias=epsb[:, 0:1]),
                     reads=["ss", "epsb"], writes=["std"])
                s.op("act", lambda e: e.activation(out=rstd[:], in_=std[:], func=AF.Exp, scale=-0.5), reads=["std"], writes=["rstd"])
                s.op("dve", lambda e: e.scalar_tensor_tensor(out=H2[:], in0=XT[:], scalar=rstd[:, 0:1], in1=gffn[:],
                                                             op0=ALU.mult, op1=ALU.mult),
                     reads=[nxt, "rstd", "gffn"], writes=[nh2])

            def G2b(i):
                p = i % 3
                H2 = hn2[p]
                nh2 = "hn2_%d" % p
                transposeB(H2, nh2, hn2T, "hn2T")
                for k in range(KD):
                    s.op("pe", lambda e, k=k: e.matmul(out=plg[:, 0:36], lhsT=hn2T[:, k, :], rhs=wr_bf[:, k, :],
                                                       start=(k == 0), stop=(k == KD - 1)),
                         reads=["hn2T%d" % (k // 8), "wr"], writes=["plg"])
                V(lambda e: e.tensor_tensor(out=lg[:], in0=plg[:, 0:36], in1=brb[:], op=ALU.add), ["plg", "brb"], ["lg"])

            def G3a(i):
                V(lambda e: e.tensor_reduce(out=cmax[:], in_=lg[:, 0:4], axis=AX.X, op=ALU.max), ["lg"], ["cmax"])
                V(lambda e: e.tensor_scalar(out=ohg[:], in0=lg[:, 0:4], scalar1=cmax[:, 0:1], scalar2=None, op0=ALU.is_ge), ["lg", "cmax"], ["ohg"])
                V(lambda e: e.tensor_scalar(out=ncmax[:], in0=cmax[:], scalar1=-1.0, scalar2=None, op0=ALU.mult), ["cmax"], ["ncmax"])
                s.op("act", lambda e: e.activation(out=ec[:], in_=lg[:, 0:4], func=AF.Exp, bias=ncmax[:, 0:1], accum_out=sumc[:]),
                     reads=["lg", "ncmax"], writes=["ec", "sumc"])
                V(lambda e: e.tensor_tensor(out=tmp48[:], in0=lg[:, 4:36].rearrange("p (g e) -> p g e", g=4),
                                            in1=ohg[:].unsqueeze(2).to_broadcast([128, 4, 8]), op=ALU.mult), ["lg", "ohg"], ["tmp48"])
                V(lambda e: e.tensor_reduce(out=fsel[:], in_=tmp48[:].rearrange("p g e -> p e g"), axis=AX.X, op=ALU.add), ["tmp48"], ["fsel"])
                V(lambda e: e.tensor_reduce(out=m1[:], in_=fsel[:], axis=AX.X, op=ALU.max), ["fsel"], ["m1"])
                V(lambda e: e.tensor_scalar(out=oh1[:], in0=fsel[:], scalar1=m1[:, 0:1], scalar2=None, op0=ALU.is_ge), ["fsel", "m1"], ["oh1"])
                V(lambda e: e.scalar_tensor_tensor(out=msk[:], in0=oh1[:], scalar=-1e30, in1=fsel[:], op0=ALU.mult, op1=ALU.add),
                  ["oh1", "fsel"], ["msk"])
                V(lambda e: e.tensor_reduce(out=m2[:], in_=msk[:], axis=AX.X, op=ALU.max), ["msk"], ["m2"])
                V(lambda e: e.tensor_scalar(out=oh2[:], in0=msk[:], scalar1=m2[:, 0:1], scalar2=None, op0=ALU.is_ge), ["msk", "m2"], ["oh2"])
                V(lambda e: e.tensor_tensor(out=d21[:], in0=m2[:], in1=m1[:], op=ALU.subtract), ["m1", "m2"], ["d21"])
                s.op("act", lambda e: e.activation(out=e21[:], in_=d21[:], func=AF.Exp), reads=["d21"], writes=["e21"])
                V(lambda e: e.tensor_tensor(out=E1[:], in0=ohg[:].unsqueeze(2).to_broadcast([128, 4, 8]),
                                            in1=oh1[:].unsqueeze(1).to_broadcast([128, 4, 8]), op=ALU.mult), ["ohg", "oh1"], ["E1"])
                V(lambda e: e.tensor_tensor(out=E2[:], in0=ohg[:].unsqueeze(2).to_broadcast([128, 4, 8]),
                                            in1=oh2[:].unsqueeze(1).to_broadcast([128, 4, 8]), op=ALU.mult), ["ohg", "oh2"], ["E2"])
                V(lambda e: e.tensor_tensor(out=Abf[:], in0=E1f, in1=E2f, op=ALU.add), ["E1", "E2"], ["Abf"])
                V(lambda e: e.reciprocal(out=pgrp[:], in_=sumc[:]), ["sumc"], ["pgrp"])
                V(lambda e: e.tensor_scalar(out=e21[:], in0=e21[:], scalar1=1.0, scalar2=None, op0=ALU.add), ["e21"], ["e21"])
                V(lambda e: e.reciprocal(out=rr[:], in_=e21[:]), ["e21"], ["rr"])
                V(lambda e: e.tensor_tensor(out=GATE[:, i, 0:1], in0=pgrp[:], in1=rr[:], op=ALU.mult), ["pgrp", "rr"], ["GATE"])
                V(lambda e: e.tensor_tensor(out=GATE[:, i, 1:2], in0=pgrp[:], in1=GATE[:, i, 0:1], op=ALU.subtract), ["pgrp", "GATE"], ["GATE"])

            def G3b(i):
                p = i % 3
                H2 = hn2[p]
                nh2 = "hn2_%d" % p
                s.op("pe", lambda e: e.matmul(out=prk[:, 0:E], lhsT=tri, rhs=Abf[:], start=True, stop=True), reads=["Abf", "cst"], writes=["prk"])
                s.op("pe", lambda e: e.matmul(out=prk[:, E:2 * E], lhsT=ones, rhs=Abf[:], start=True, stop=True), reads=["Abf", "cst"], writes=["prk"])
                V(lambda e: e.tensor_tensor(out=posv[:], in0=prk[:, 0:E], in1=base[:], op=ALU.add), ["prk", "base"], ["posv"])
                V(lambda e: e.tensor_tensor(out=base[:], in0=prk[:, E:2 * E], in1=base[:], op=ALU.add), ["prk", "base"], ["base"])
                V(lambda e: e.tensor_scalar(out=ov[:], in0=posv[:], scalar1=float(C), scalar2=1e9, op0=ALU.is_ge, op1=ALU.mult), ["posv"], ["ov"])
                V(lambda e: e.tensor_tensor(out=posv[:], in0=posv[:], in1=ov[:], op=ALU.add), ["posv", "ov"], ["posv"])
                V(lambda e: e.tensor_tensor(out=posv[:], in0=posv[:], in1=eoff[:], op=ALU.add), ["posv", "eoff"], ["posv"])
                V(lambda e: e.tensor_tensor(out=tmp32[:], in0=E1f, in1=posv[:], op=ALU.mult), ["E1", "posv"], ["tmp32"])
                V(lambda e: e.tensor_reduce(out=dd[:, 0:1], in_=tmp32[:], axis=AX.X, op=ALU.add), ["tmp32"], ["dd"])
                V(lambda e: e.tensor_tensor(out=tmp32[:], in0=E2f, in1=posv[:], op=ALU.mult), ["E2", "posv", "dd"], ["tmp32"])
                V(lambda e: e.tensor_reduce(out=dd[:, 1:2], in_=tmp32[:], axis=AX.X, op=ALU.add), ["tmp32"], ["dd"])
                V(lambda e: e.tensor_copy(out=DEST[:, i, :], in_=dd[:]), ["dd"], ["DEST%d" % i])
                for j in range(2):
                    s.dma("pool", lambda e, j=j: e.indirect_dma_start(
                        out=XS, out_offset=bass.IndirectOffsetOnAxis(ap=DEST[:, i, j:j + 1], axis=0),
                        in_=H2[:], in_offset=None, bounds_check=bcreg(e), oob_is_err=False),
                        reads=[nh2, "DEST%d" % i], writes=["XS_%d_%d" % (i, j)], key="sc_" + nh2)

            loadsB(0)
            for st in range(NT + 2):
                if st + 1 < NT:
                    loadsB(st + 1)
                if 0 <= st - 1 < NT:
                    storeB(st - 1)
                if st < NT:
                    G1(st, 0)
                if 0 <= st - 1 < NT:
                    G2a(st - 1)
                if 0 <= st - 2 < NT:
                    G3a(st - 2)
                if st < NT:
                    G1(st, 1)
                if 0 <= st - 1 < NT:
                    G2b(st - 1)
                if 0 <= st - 2 < NT:
                    G3b(st - 2)
            s.barrier()
            pb.close()

        def phase_C():
            pc = ExitStack()
            if pre:
                wg = [pre["wg"], sbt(pc, "wg1", [128, KD, DE], BF16)]
                wu = [pre["wu"], sbt(pc, "wu1", [128, KD, DE], BF16)]
                wd = [pre["wd"], sbt(pc, "wd1", [128, 4, D], BF16)]
            else:
                wg = [sbt(pc, "wg%d" % i, [128, KD, DE], BF16) for i in range(2)]
                wu = [sbt(pc, "wu%d" % i, [128, KD, DE], BF16) for i in range(2)]
                wd = [sbt(pc, "wd%d" % i, [128, 4, D], BF16) for i in range(2)]
            xr = [sbt(pc, "xr%d" % i, [128, D], BF16) for i in range(3)]
            xT = [sbt(pc, "xTC%d" % i, [128, KD, C], BF16) for i in range(2)]
            hT = [sbt(pc, "hTC%d" % i, [128, 4, C], BF16) for i in range(2)]
            sil = [sbt(pc, "sil%d" % i, [128, C], F32) for i in range(2)]
            ys = [sbt(pc, "ys%d" % i, [128, D], BF16) for i in range(4)]
            pab = [[pst(pc, "pab%d%d" % (a, b), [128, 512], F32) for b in range(2)] for a in range(2)]
            py = [pst(pc, "py%d" % i, [128, 512], F32) for i in range(2)]
            ptC = [pst(pc, "ptC%d" % i, [128, 8, 128], BF16) for i in range(2)]
            cnt = dict(row=0, py=0, half=0, cp=0)

            def load_w(ex):
                p = ex % 2
                WG, WU, WD = wg[p], wu[p], wd[p]
                s.dma("pool", lambda e: e.dma_start(out=WG[:], in_=w_gate[ex].rearrange("(k p) n -> p k n", p=128)),
                      writes=["wg%d" % p], key="wg%d" % p)
                s.dma("pool", lambda e: e.dma_start(out=WU[:], in_=w_up[ex].rearrange("(k p) n -> p k n", p=128)),
                      writes=["wu%d" % p], key="wu%d" % p)
                for hh_ in range(2):
                    s.dma("pool", lambda e, hh_=hh_: e.dma_start(
                        out=WD[:, :, hh_ * 1024:(hh_ + 1) * 1024],
                        in_=w_down[ex][:, hh_ * 1024:(hh_ + 1) * 1024].rearrange("(j p) n -> p j n", p=128)),
                        writes=["wd%d_%d" % (p, hh_)], key="wd%d_%d" % (p, hh_))

            def load_x(ex):
                for r in range(CT):
                    q = (ex * CT + r) % 3
                    XR = xr[q]
                    s.dma("sp", lambda e, XR=XR, r=r: e.dma_start(out=XR[0:rws(r), :], in_=XS[ex * C + r * 128:ex * C + r * 128 + rws(r), :]),
                          writes=["xr%d" % q], key="xr%d" % q)

            def transposes(ex):
                p = ex % 2
                XT = xT[p]
                for r in range(CT):
                    q = (ex * CT + r) % 3
                    XR = xr[q]
                    for rr_ in range(2):
                        hf = cnt["half"] % 2
                        cnt["half"] += 1
                        for b in range(8):
                            kk = rr_ * 8 + b
                            s.op("pe", lambda e, XR=XR, kk=kk, hf=hf, b=b, r=r: e.transpose(
                                out=ptC[hf][:, b, 0:rws(r)], in_=XR[0:rws(r), kk * 128:(kk + 1) * 128], identity=ident[0:rws(r), 0:rws(r)]),
                                 reads=["xr%d" % q, "cst"], writes=["ptC%d" % hf])
                        if hf == 0:
                            s.op("act", lambda e, rr_=rr_, r=r, hf=hf: e.copy(out=XT[:, rr_ * 8:(rr_ + 1) * 8, r * 128:r * 128 + rws(r)],
                                                                             in_=ptC[hf][:, :, 0:rws(r)]),
                                 reads=["ptC%d" % hf], writes=["xT%d" % p])
                        else:
                            s.op("dve", lambda e, rr_=rr_, r=r, hf=hf: e.tensor_copy(out=XT[:, rr_ * 8:(rr_ + 1) * 8, r * 128:r * 128 + rws(r)],
                                                                                    in_=ptC[hf][:, :, 0:rws(r)]),
                                 reads=["ptC%d" % hf], writes=["xT%d" % p])

            def gateup(ex):
                p = ex % 2
                WG, WU, XT, HT = wg[p], wu[p], xT[p], hT[p]
                for j in range(4):
                    PA, PB = pab[j % 2]
                    na, nb = "pab%d0" % (j % 2), "pab%d1" % (j % 2)
                    for k in range(KD):
                        s.op("pe", lambda e, PA=PA, j=j, k=k: e.matmul(out=PA[:, 0:C], lhsT=WG[:, k, j * 128:(j + 1) * 128], rhs=XT[:, k, :],
                                                                       start=(k == 0), stop=(k == KD - 1)),
                             reads=["wg%d" % p, "xT%d" % p], writes=[na])
                    for k in range(KD):
                        s.op("pe", lambda e, PB=PB, j=j, k=k: e.matmul(out=PB[:, 0:C], lhsT=WU[:, k, j * 128:(j + 1) * 128], rhs=XT[:, k, :],
                                                                       start=(k == 0), stop=(k == KD - 1)),
                             reads=["wu%d" % p, "xT%d" % p], writes=[nb])
                    SL = sil[j % 2]
                    s.op("act", lambda e, PA=PA, SL=SL: e.activation(out=SL[:], in_=PA[:, 0:C], func=AF.Silu), reads=[na], writes=["sil%d" % (j % 2)])
                    s.op("dve", lambda e, PB=PB, SL=SL, j=j: e.tensor_tensor(out=HT[:, j, :], in0=PB[:, 0:C], in1=SL[:], op=ALU.mult),
                         reads=[nb, "sil%d" % (j % 2)], writes=["hT%d" % p])

            def down(ex):
                p = ex % 2
                WD, HT = wd[p], hT[p]
                for r in range(CT):
                    q = (ex * CT + r) % 4
                    YSb = ys[q]
                    for c in range(4):
                        PY = py[cnt["py"] % 2]
                        npn = "py%d" % (cnt["py"] % 2)
                        for j in range(4):
                            s.op("pe", lambda e, PY=PY, r=r, c=c, j=j: e.matmul(out=PY[0:rws(r), :], lhsT=HT[:, j, r * 128:r * 128 + rws(r)],
                                                                                rhs=WD[:, j, c * 512:(c + 1) * 512], start=(j == 0), stop=(j == 3)),
                                 reads=["hT%d" % p, "wd%d_%d" % (p, c // 2)], writes=[npn])
                        if cnt["py"] % 2 == 0:
                            s.op("act", lambda e, PY=PY, YSb=YSb, c=c, r=r: e.copy(out=YSb[0:rws(r), c * 512:(c + 1) * 512], in_=PY[0:rws(r), :]),
                                 reads=[npn], writes=["ys%d" % q])
                        else:
                            s.op("dve", lambda e, PY=PY, YSb=YSb, c=c, r=r: e.tensor_copy(out=YSb[0:rws(r), c * 512:(c + 1) * 512], in_=PY[0:rws(r), :]),
                                 reads=[npn], writes=["ys%d" % q])
                        cnt["py"] += 1
                    s.dma("sp", lambda e, YSb=YSb, r=r: e.dma_start(out=YS[ex * C + r * 128:ex * C + r * 128 + rws(r), :], in_=YSb[0:rws(r), :]),
                          reads=["ys%d" % q], writes=["YS_%d_%d" % (ex, r)], key="st_ys%d" % q)

            if not pre:
                load_w(0)
            load_x(0)
            transposes(0)
            for ex in range(E):
                if ex + 1 < E:
                    load_w(ex + 1)
                    load_x(ex + 1)
                gateup(ex)
                if ex + 1 < E:
                    transposes(ex + 1)
                down(ex)
            s.barrier()
            pc.close()

        def phase_D():
            pd = ExitStack()
            NS = 4
            ya = [sbt(pd, "ya%d" % i, [128, D], BF16) for i in range(NS)]
            yb = [sbt(pd, "yb%d" % i, [128, D], BF16) for i in range(NS)]
            hh = [sbt(pd, "hh%d" % i, [128, D], F32) for i in range(NS)]
            for i in range(NS):
                s.op("dve", lambda e, i=i: e.memset(ya[i][:], 0.0), writes=["ya%d" % i])
                s.op("pool", lambda e, i=i: e.memset(yb[i][:], 0.0), writes=["yb%d" % i])

            def loadsD(i):
                p = i % NS
                YA, YB, HH = ya[p], yb[p], hh[p]
                s.dma("pool", lambda e: e.indirect_dma_start(
                    out=YA[:], out_offset=None, in_=YS, in_offset=bass.IndirectOffsetOnAxis(ap=DEST[:, i, 0:1], axis=0),
                    bounds_check=bcreg(e), oob_is_err=False), writes=["ya%d" % p], key="ya%d" % p)
                s.dma("pool", lambda e: e.indirect_dma_start(
                    out=YB[:], out_offset=None, in_=YS, in_offset=bass.IndirectOffsetOnAxis(ap=DEST[:, i, 1:2], axis=0),
                    bounds_check=bcreg(e), oob_is_err=False), writes=["yb%d" % p], key="yb%d" % p)
                s.dma("sp", lambda e: e.dma_start(out=HH[:], in_=out[i * 128:(i + 1) * 128, :]), writes=["hh%d" % p], key="hh%d" % p)

            def combineD(i):
                p = i % NS
                YA, YB, HH = ya[p], yb[p], hh[p]
                s.op("dve", lambda e: e.scalar_tensor_tensor(out=HH[:], in0=YA[:], scalar=GATE[:, i, 0:1], in1=HH[:],
                                                             op0=ALU.mult, op1=ALU.add),
                     reads=["ya%d" % p, "hh%d" % p], writes=["hh%d" % p])
                s.op("dve", lambda e: e.scalar_tensor_tensor(out=HH[:], in0=YB[:], scalar=GATE[:, i, 1:2], in1=HH[:],
                                                             op0=ALU.mult, op1=ALU.add),
                     reads=["yb%d" % p, "hh%d" % p], writes=["hh%d" % p])
                s.dma("sp", lambda e: e.dma_start(out=out[i * 128:(i + 1) * 128, :], in_=HH[:]), reads=["hh%d" % p],
                      writes=["outD%d" % i], key="st_hh%d" % p)

            for i in range(NT + 2):
                if i < NT:
                    loadsD(i)
                if i - 2 >= 0:
                    combineD(i - 2)
            pd.close()

        pre = {}
        for ph, fn in (("A", phase_A), ("B", phase_B), ("C", phase_C), ("D", phase_D)):
            if ph == "B" and "B" in phases and "C" in phases:
                pbc = ExitStack()
                pre["wg"] = sbt(pbc, "wg0", [128, KD, DE], BF16)
                pre["wu"] = sbt(pbc, "wu0", [128, KD, DE], BF16)
                pre["wd"] = sbt(pbc, "wd0", [128, 4, D], BF16)
            if ph in phases:
                fn()
            if ph == "C" and pre:
                pbc.close()
        if debug:
            s.barrier()
            s.dma("sp", lambda e: e.dma_start(out=DBGD, in_=DEST[:]), key="dbgd")
            s.dma("sp", lambda e: e.dma_start(out=DBGG, in_=GATE[:]), key="dbgg")
        s.finish()
        info = run_schedule(nc, s, es)
    return nc, info


CAP = 352
_PROG_CACHE = {}


def make_in_map(xb, prm, consts):
    cst, cs, eoff = consts
    m = dict(prm)
    m["x"] = np.ascontiguousarray(xb)
    m["cst"] = cst
    m["cs"] = cs
    m["eoff"] = eoff
    return m


def prep_params(norm_mix, w_in, q_norm, k_norm, sinks, w_pool, pool_scale, w_out, norm_ffn,
                w_coarse, b_coarse, w_fine, b_fine, w_gate, w_up, w_down):
    f = lambda a: np.ascontiguousarray(np.asarray(a, dtype=np.float32))
    return dict(
        norm_mix=f(norm_mix).reshape(1, D), w_in=f(w_in).reshape(D, INW), q_norm=f(q_norm).reshape(1, HD),
        k_norm=f(k_norm).reshape(1, HD), sinks=f(sinks).reshape(1, NH), w_pool=f(w_pool).reshape(4, 256, 256),
        pool_scale=f(pool_scale).reshape(1, 1024), w_out=f(w_out).reshape(D, D), norm_ffn=f(norm_ffn).reshape(1, D),
        w_r=np.ascontiguousarray(np.concatenate([f(w_coarse).reshape(D, 4), f(w_fine).reshape(D, E)], axis=1)),
        b_r=np.ascontiguousarray(np.concatenate([f(b_coarse).reshape(1, 4), f(b_fine).reshape(1, E)], axis=1)),
        w_gate=f(w_gate).reshape(E, D, DE), w_up=f(w_up).reshape(E, D, DE), w_down=f(w_down).reshape(E, DE, D))


def kernel(x, norm_mix, w_in, q_norm, k_norm, sinks, w_pool, pool_scale, w_out, norm_ffn,
           w_coarse, b_coarse, w_fine, b_fine, w_gate, w_up, w_down):
    x = np.asarray(x, dtype=np.float32)
    B, S, _ = x.shape
    assert B == N_CORES
    prm = prep_params(norm_mix, w_in, q_norm, k_norm, sinks, w_pool, pool_scale, w_out, norm_ffn,
                      w_coarse, b_coarse, w_fine, b_fine, w_gate, w_up, w_down)
    key = (S, CAP)
    if key not in _PROG_CACHE:
        _PROG_CACHE[key] = build_program(S, CAP)[0]
    nc = _PROG_CACHE[key]
    consts = build_consts(S, CAP)
    in_maps = [make_in_map(x[b], prm, consts) for b in range(B)]
    res = run_bass_kernel_spmd(nc, in_maps, core_ids=list(range(B)))
    return np.stack([np.asarray(r["out"], dtype=np.float32) for r in res.results], axis=0)
```

```python
import numpy as np
from contextlib import ExitStack
import concourse.bass as bass
import concourse.mybir as mybir
from concourse.bass_utils import run_bass_kernel_spmd

F32 = mybir.dt.float32
BF16 = mybir.dt.bfloat16
I32 = mybir.dt.int32
AF = mybir.ActivationFunctionType
ALU = mybir.AluOpType
AX = mybir.AxisListType

D = 2048
KD = 16
INW = 2560
NH = 16
NKV = 4
HD = 64
E = 32
DE = 512
EPS = 1e-6
ROPE_THETA = 500000.0
POOL_WINDOWS = (2, 4, 8, 16)
N_CORES = 8

ENGINES = ("pe", "act", "dve", "pool", "sp")


class _Buf:
    __slots__ = ("last_w", "readers")

    def __init__(self):
        self.last_w = None
        self.readers = {}


class _Op:
    __slots__ = ("eng", "fn", "deps", "inc", "semval", "is_dma", "key")

    def __init__(self, eng, fn, is_dma, key):
        self.eng = eng
        self.fn = fn
        self.deps = []
        self.inc = False
        self.semval = None
        self.is_dma = is_dma
        self.key = key


class Sched:
    def __init__(self):
        self.ops = []
        self.bufs = {}
        self.last_eng = {}
        self.last_dma = {}
        self.barrier_deps = []
        self.final_waits = []

    def _b(self, name):
        b = self.bufs.get(name)
        if b is None:
            b = _Buf()
            self.bufs[name] = b
        return b

    def _add(self, eng, fn, reads, writes, is_dma, key):
        op = _Op(eng, fn, is_dma, key)
        deps = {}
        for r in reads:
            b = self._b(r)
            if b.last_w is not None:
                deps[id(b.last_w)] = b.last_w
        for w in writes:
            b = self._b(w)
            p = b.last_w
            if p is not None and (is_dma or p.is_dma or p.eng != eng):
                deps[id(p)] = p
            for p in b.readers.values():
                if is_dma or p.is_dma or p.eng != eng:
                    deps[id(p)] = p
        for p in self.barrier_deps:
            deps[id(p)] = p
        rk = ("d", key) if is_dma else eng
        for r in reads:
            self._b(r).readers[rk] = op
        for w in writes:
            b = self._b(w)
            b.last_w = op
            b.readers = {}
        op.deps = list(deps.values())
        for p in op.deps:
            p.inc = True
        self.ops.append(op)
        if is_dma:
            self.last_dma[key] = op
        else:
            self.last_eng[eng] = op
        return op

    def op(self, eng, fn, reads=(), writes=()):
        return self._add(eng, fn, reads, writes, False, None)

    def dma(self, eng, fn, reads=(), writes=(), key=None):
        op = self._add(eng, fn, reads, writes, True, key)
        op.inc = True
        return op

    def barrier(self):
        self.barrier_deps = list(self.last_eng.values()) + list(self.last_dma.values())
        self.bufs = {}

    def finish(self):
        self.final_waits = list(self.last_dma.values())


def run_schedule(nc, sched, es):
    eng_sem = {e: es.enter_context(nc.semaphore("s_" + e)) for e in ENGINES}
    dma_sem, dma_cnt = {}, {}
    eng_cnt = {e: 0 for e in ENGINES}
    for op in sched.ops:
        if op.is_dma:
            if op.key not in dma_sem:
                dma_sem[op.key] = es.enter_context(nc.semaphore("d_" + op.key))
                dma_cnt[op.key] = 0
            dma_cnt[op.key] += 16
            op.semval = dma_cnt[op.key]
        elif op.inc:
            eng_cnt[op.eng] += 1
            op.semval = eng_cnt[op.eng]
    per_eng = {e: [o for o in sched.ops if o.eng == e] for e in ENGINES}

    def sem_of(p):
        return dma_sem[p.key] if p.is_dma else eng_sem[p.eng]

    def body(ename, eng):
        waited = {}

        def wait(p):
            s = sem_of(p)
            if waited.get(id(s), 0) >= p.semval:
                return
            eng.wait_ge(s, p.semval)
            waited[id(s)] = p.semval

        for op in per_eng[ename]:
            need = {}
            for p in op.deps:
                k = id(sem_of(p))
                if k not in need or need[k].semval < p.semval:
                    need[k] = p
            for p in need.values():
                wait(p)
            ins = op.fn(eng)
            if op.is_dma:
                ins.then_inc(dma_sem[op.key], 16)
            elif op.inc:
                ins.then_inc(eng_sem[ename], 1)
        if ename == "sp":
            for p in sched.final_waits:
                wait(p)

    with nc.Block() as block:
        @block.tensor
        def _(e):
            body("pe", e)

        @block.scalar
        def _(e):
            body("act", e)

        @block.vector
        def _(e):
            body("dve", e)

        @block.gpsimd
        def _(e):
            body("pool", e)

        @block.sync
        def _(e):
            body("sp", e)
    return dict(n_ops=len(sched.ops), n_dma_sems=len(dma_sem), eng_cnt=eng_cnt)


C_ID, C_TRI, C_ONE, C_MPREV, C_MCUR, C_POOL = 0, 128, 256, 384, 512, 640
NCST = 640 + 12 * 128


def build_consts(S, C):
    NT = S // 128
    cst = np.zeros((128, NCST), np.float32)
    idx = np.arange(128)
    cst[:, C_ID:C_ID + 128] = np.eye(128, dtype=np.float32)
    cst[:, C_TRI:C_TRI + 128] = (idx[:, None] < idx[None, :])
    cst[:, C_ONE:C_ONE + 128] = 1.0
    cst[:, C_MPREV:C_MPREV + 128] = (idx[:, None] > idx[None, :])
    cst[:, C_MCUR:C_MCUR + 128] = (idx[:, None] <= idx[None, :])
    tp = idx[:, None]
    t = idx[None, :]
    for g, w in enumerate(POOL_WINDOWS):
        mcur = ((tp <= t) & (tp > t - w)) / float(w) - (tp == t)
        mprev = (tp >= t + 129 - w) / float(w)
        cnt = np.minimum(t + 1, w).astype(np.float32)
        m0 = ((tp <= t) & (tp > t - w)) / cnt - (tp == t)
        cst[:, C_POOL + (0 + g) * 128:C_POOL + (1 + g) * 128] = mcur
        cst[:, C_POOL + (4 + g) * 128:C_POOL + (5 + g) * 128] = mprev
        cst[:, C_POOL + (8 + g) * 128:C_POOL + (9 + g) * 128] = m0
    pos = np.arange(S, dtype=np.float32)
    rot = HD // 4
    inv_freq = (ROPE_THETA ** (-np.arange(0, rot, 2, dtype=np.float32) / rot)).astype(np.float32)
    ang = pos[:, None] * inv_freq[None, :]
    cs = np.zeros((128, NT, 16), np.float32)
    cs[:, :, 0:8] = np.cos(ang).astype(np.float32).reshape(NT, 128, 8).transpose(1, 0, 2)
    cs[:, :, 8:16] = np.sin(ang).astype(np.float32).reshape(NT, 128, 8).transpose(1, 0, 2)
    eoff = np.tile((np.arange(E, dtype=np.float32) * C)[None, :], (128, 1))
    return cst, cs.reshape(128, NT * 16), eoff


def build_program(S, C, phases="ABCD", debug=False):
    NT = S // 128
    CT = (C + 127) // 128

    def rws(r):
        return min(128, C - r * 128)
    nc = bass.Bass("TRN2", target_bir_lowering=False)

    def din(name, shape, dt=F32):
        return nc.dram_tensor(name, shape, dt, kind="ExternalInput").ap()

    x = din("x", [S, D])
    norm_mix = din("norm_mix", [1, D])
    w_in = din("w_in", [D, INW])
    q_norm = din("q_norm", [1, HD])
    k_norm = din("k_norm", [1, HD])
    sinks = din("sinks", [1, NH])
    w_pool = din("w_pool", [4, 256, 256])
    pool_scale = din("pool_scale", [1, 1024])
    w_out = din("w_out", [D, D])
    norm_ffn = din("norm_ffn", [1, D])
    w_r = din("w_r", [D, 36])
    b_r = din("b_r", [1, 36])
    w_gate = din("w_gate", [E, D, DE])
    w_up = din("w_up", [E, D, DE])
    w_down = din("w_down", [E, DE, D])
    cst_d = din("cst", [128, NCST])
    cs_d = din("cs", [128, NT * 16])
    eoff_d = din("eoff", [128, E])
    out = nc.dram_tensor("out", [S, D], F32, kind="ExternalOutput").ap()
    dk = dict(kind="ExternalOutput") if debug else {}
    MIX = nc.dram_tensor("MIX", [S, D], BF16, **dk).ap()
    XS = nc.dram_tensor("XS", [E * C, D], BF16, **dk).ap()
    YS = nc.dram_tensor("YS", [E * C, D], BF16, **dk).ap()
    if debug:
        DBGD = nc.dram_tensor("DBGD", [128, S // 128, 2], I32, kind="ExternalOutput").ap()
        DBGG = nc.dram_tensor("DBGG", [128, S // 128, 2], F32, kind="ExternalOutput").ap()

    s = Sched()
    taps = {}
    regcache = {}

    def bcreg(e):
        if "bc" not in regcache:
            regcache["bc"] = e.to_reg(E * C - 1)
        return regcache["bc"]

    def tap(name, ap, reads, cond=True):
        if not (debug and cond):
            return
        shp = list(ap.shape)
        t = nc.dram_tensor("T_" + name, shp, ap.dtype, kind="ExternalOutput").ap()
        taps[name] = t
        s.dma("sp", lambda e: e.dma_start(out=t, in_=ap), reads=reads, key="tap")

    with ExitStack() as es:
        def sbt(stack, name, shape, dt):
            return stack.enter_context(nc.sbuf_tensor("sb_" + name, shape, dt))

        def pst(stack, name, shape, dt):
            return stack.enter_context(nc.psum_tensor("ps_" + name, shape, dt))

        cst = sbt(es, "cst", [128, NCST], BF16)
        DEST = sbt(es, "DEST", [128, NT, 2], I32)
        GATE = sbt(es, "GATE", [128, NT, 2], F32)
        base = sbt(es, "base", [128, E], F32)
        eoff = sbt(es, "eoff", [128, E], F32)
        for a, b in ((0, 1024), (1024, NCST)):
            s.dma("pool", lambda e, a=a, b=b: e.dma_start(out=cst[:, a:b], in_=cst_d[:, a:b]), writes=["cst_%d" % a], key="cst")
        s.op("pool", lambda e: e.memset(DEST[:], 0), reads=["cst_0", "cst_1024"], writes=["cst"])
        s.dma("sp", lambda e: e.dma_start(out=eoff[:], in_=eoff_d), writes=["eoff"], key="eoff")
        s.op("dve", lambda e: e.memset(base[:], 0.0), writes=["base"])
        epsb = sbt(es, "epsb", [128, 1], F32)
        s.op("dve", lambda e: e.memset(epsb[:], EPS), writes=["epsb"])
        ident = cst[:, C_ID:C_ID + 128]
        tri = cst[:, C_TRI:C_TRI + 128]
        ones = cst[:, C_ONE:C_ONE + 128]
        mask2 = cst[:, C_MPREV:C_MPREV + 256].rearrange("p (a q) -> p a q", a=2)

        def poolm(kind, g):
            o = C_POOL + (kind * 4 + g) * 128
            return cst[:, o:o + 128]

        def phase_A():
            pa = ExitStack()
            w_in_bf = sbt(pa, "w_in_bf", [128, KD, INW], BF16)
            gmix = sbt(pa, "gmix", [128, D], F32)
            qg = sbt(pa, "qg", [128, HD], F32)
            kg = sbt(pa, "kg", [128, HD], F32)
            gqk = sbt(pa, "gqk", [128, 20, HD], F32)
            esink = sbt(pa, "esink", [128, NH], F32)
            pscale = sbt(pa, "pscale", [128, 1024], F32)
            wpool_bf = sbt(pa, "wpool_bf", [128, 4, 2, 256], BF16)
            cs = sbt(pa, "cs", [128, NT, 16], F32)
            xt = [sbt(pa, "xtA%d" % i, [128, D], F32) for i in range(2)]
            junk = sbt(pa, "junkA", [128, D], BF16)
            ss = sbt(pa, "ssA", [128, 1], F32)
            std = sbt(pa, "stdA", [128, 1], F32)
            rstd = sbt(pa, "rstdA", [128, 1], F32)
            hn = [sbt(pa, "hnA%d" % i, [128, D], BF16) for i in range(2)]
            hnT = sbt(pa, "hnTA", [128, KD, 128], BF16)
            qk32 = [sbt(pa, "qk32_%d" % i, [128, 1280], F32) for i in range(2)]
            sq32 = sbt(pa, "sq32", [128, 1280], F32)
            ssq = sbt(pa, "ssq", [128, 20], F32)
            stq = sbt(pa, "stq", [128, 20], F32)
            rq = sbt(pa, "rq", [128, 20], F32)
            qn32 = sbt(pa, "qn32", [128, 20, HD], F32)
            qg32 = sbt(pa, "qg32", [128, 20, HD], F32)
            tr = [sbt(pa, "tr%d" % i, [128, 20, 8], F32) for i in range(4)]
            qkb = sbt(pa, "qkb", [128, 20, HD], BF16)
            kd = sbt(pa, "kd", [128, 4, 2, HD], BF16)
            qT = sbt(pa, "qT", [128, 8, 128], BF16)
            kT = [sbt(pa, "kT%d" % i, [128, 4, 128], BF16) for i in range(2)]
            vv = [sbt(pa, "vv%d" % i, [128, 4, 65], BF16) for i in range(3)]
            uu = [sbt(pa, "uu%d" % i, [128, 1024], BF16) for i in range(3)]
            PT = [[sbt(pa, "PT%d%d" % (a, b), [128, 2, 2, 128], BF16) for b in range(2)] for a in range(2)]
            den = sbt(pa, "den", [128, 8], F32)
            rden = sbt(pa, "rden", [128, 8], F32)
            dTs = sbt(pa, "dTs", [128, 8, 128], BF16)
            mixed = [sbt(pa, "mixed%d" % i, [128, D], BF16) for i in range(2)]
            pj = pst(pa, "pj", [128, 5, 512], F32)
            pt = pst(pa, "ptA", [128, 8, 128], BF16)
            sc = [pst(pa, "sc%d" % i, [128, 2, 2, 128], F32) for i in range(2)]
            pjf = pj[:].rearrange("p a b -> p (a b)")
            pt2 = sc[0][:].rearrange("p a b c -> p (a b c)").bitcast(BF16).rearrange("p (b c) -> p b c", c=128)

            for c in range(5):
                s.dma("pool", lambda e, c=c: e.dma_start(
                    out=w_in_bf[:, :, c * 512:(c + 1) * 512],
                    in_=w_in[:, c * 512:(c + 1) * 512].rearrange("(k p) n -> p k n", p=128)),
                    writes=["win%d" % c], key="win%d" % c)
            s.dma("sp", lambda e: e.dma_start(out=gmix[:], in_=norm_mix.partition_broadcast(128)), writes=["gmix"], key="gmix")
            s.dma("sp", lambda e: e.dma_start(out=qg[:], in_=q_norm.partition_broadcast(128)), writes=["qg"], key="qg")
            s.dma("sp", lambda e: e.dma_start(out=kg[:], in_=k_norm.partition_broadcast(128)), writes=["kg"], key="kg")
            s.dma("sp", lambda e: e.dma_start(out=esink[:], in_=sinks.partition_broadcast(128)), writes=["esink"], key="esink")
            s.dma("sp", lambda e: e.dma_start(out=pscale[:], in_=pool_scale.partition_broadcast(128)), writes=["pscale"], key="pscale")
            s.dma("sp", lambda e: e.dma_start(out=cs[:], in_=cs_d.rearrange("p (t c) -> p t c", c=16)), writes=["cs"], key="cs")
            s.dma("pool", lambda e: e.dma_start(out=wpool_bf[:], in_=w_pool.rearrange("g (c p) d -> p g c d", p=128)),
                  writes=["wpool"], key="wpool")
            s.op("dve", lambda e: e.tensor_copy(out=gqk[:, 0:16, :], in_=qg[:].unsqueeze(1).to_broadcast([128, 16, HD])),
                 reads=["qg"], writes=["gqk"])
            s.op("dve", lambda e: e.tensor_copy(out=gqk[:, 16:20, :], in_=kg[:].unsqueeze(1).to_broadcast([128, 4, HD])),
                 reads=["kg", "gqk"], writes=["gqk"])
            s.op("act", lambda e: e.activation(out=esink[:], in_=esink[:], func=AF.Exp), reads=["esink"], writes=["esink"])
            for i in range(3):
                s.op("pool", lambda e, i=i: e.memset(vv[i][:], 1.0), writes=["vv%d" % i])

            def F1(i):
                p = i % 2
                XT, HN = xt[p], hn[p]
                nxt, nhn = "xtA%d" % p, "hnA%d" % p
                s.dma("sp", lambda e: e.dma_start(out=XT[:], in_=x[i * 128:(i + 1) * 128, :]), writes=[nxt], key=nxt)
                s.op("act", lambda e: e.activation(out=junk[:], in_=XT[:], func=AF.Square, accum_out=ss[:]),
                     reads=[nxt], writes=["junk", "ss"])
                s.op("act", lambda e: e.activation(out=std[:], in_=ss[:], func=AF.Ln, scale=1.0 / D, bias=epsb[:, 0:1]),
                     reads=["ss", "epsb"], writes=["std"])
                s.op("act", lambda e: e.activation(out=rstd[:], in_=std[:], func=AF.Exp, scale=-0.5), reads=["std"], writes=["rstd"])
                s.op("dve", lambda e: e.scalar_tensor_tensor(out=HN[:], in0=XT[:], scalar=rstd[:, 0:1], in1=gmix[:],
                                                             op0=ALU.mult, op1=ALU.mult),
                     reads=[nxt, "rstd", "gmix"], writes=[nhn])

            def F2(i, part):
                p = i % 2
                HN = hn[p]
                nhn = "hnA%d" % p
                VV, UU, QK = vv[i % 3], uu[i % 3], qk32[p]
                nvv, nuu, nqk = "vv%d" % (i % 3), "uu%d" % (i % 3), "qk32_%d" % p
                if part == 0:
                    for r in range(2):
                        bank, nb_ = (pt[:], "pt") if r == 0 else (pt2, "sc0")
                        for b in range(8):
                            s.op("pe", lambda e, r=r, b=b, bank=bank: e.transpose(out=bank[:, b, :], in_=HN[:, (r * 8 + b) * 128:(r * 8 + b + 1) * 128],
                                                                                  identity=ident),
                                 reads=[nhn, "cst"], writes=[nb_])
                        s.op("act", lambda e, r=r, bank=bank: e.copy(out=hnT[:, r * 8:(r + 1) * 8, :], in_=bank), reads=[],
                             writes=[nb_, "hnT%d" % r])
                    return
                def chunk(c):
                    for k in range(KD):
                        s.op("pe", lambda e, k=k: e.matmul(out=pj[:, c, :], lhsT=hnT[:, k, :], rhs=w_in_bf[:, k, c * 512:(c + 1) * 512],
                                                           start=(k == 0), stop=(k == KD - 1)),
                             reads=["hnT%d" % (k // 8), "win%d" % c], writes=["pj%d" % c])

                def evac_qkv():
                    s.op("act", lambda e: e.copy(out=QK[:], in_=pjf[:, 0:1280]), reads=[], writes=["pj0", "pj1", "pj2", nqk])
                    s.op("act", lambda e: e.copy(out=VV[:, :, 0:HD], in_=pjf[:, 1280:1536].rearrange("p (h d) -> p h d", d=HD)),
                         reads=[], writes=["pj2", nvv])

                def evac_u():
                    s.op("act", lambda e: e.copy(out=UU[:], in_=pjf[:, 1536:2560]), reads=[], writes=["pj3", "pj4", nuu])

                return [lambda: chunk(0), lambda: chunk(1), lambda: (chunk(2), evac_qkv()), lambda: (chunk(3), chunk(4), evac_u())]

            def QKC(i):
                p = i % 2
                QK = qk32[p]
                nqk = "qk32_%d" % p
                s.op("act", lambda e: e.activation(out=sq32[:], in_=QK[:], func=AF.Square), reads=[nqk], writes=["sq32"])
                s.op("pool", lambda e: e.tensor_tensor(out=qg32[:], in0=QK[:].rearrange("p (h d) -> p h d", d=HD), in1=gqk[:], op=ALU.mult),
                     reads=[nqk, "gqk"], writes=["qg32"])
                s.op("dve", lambda e: e.tensor_reduce(out=ssq[:], in_=sq32[:].rearrange("p (h d) -> p h d", d=HD), axis=AX.X, op=ALU.add),
                     reads=["sq32"], writes=["ssq"])
                s.op("act", lambda e: e.activation(out=stq[:], in_=ssq[:], func=AF.Ln, scale=1.0 / HD, bias=epsb[:, 0:1]),
                     reads=["ssq", "epsb"], writes=["stq"])
                s.op("act", lambda e: e.activation(out=rq[:], in_=stq[:], func=AF.Exp, scale=-0.5), reads=["stq"], writes=["rq"])
                s.op("dve", lambda e: e.tensor_tensor(out=qn32[:], in0=qg32[:],
                                                       in1=rq[:].unsqueeze(2).to_broadcast([128, 20, HD]), op=ALU.mult),
                     reads=["qg32", "rq"], writes=["qn32"])
                cosb = cs[:, i, 0:8].unsqueeze(1).to_broadcast([128, 20, 8])
                sinb = cs[:, i, 8:16].unsqueeze(1).to_broadcast([128, 20, 8])
                s.op("dve", lambda e: e.tensor_tensor(out=tr[0][:], in0=qn32[:, :, 0:8], in1=cosb, op=ALU.mult),
                     reads=["qn32", "cs"], writes=["tr0"])
                s.op("dve", lambda e: e.tensor_tensor(out=tr[1][:], in0=qn32[:, :, 8:16], in1=sinb, op=ALU.mult),
                     reads=["qn32", "cs"], writes=["tr1"])
                s.op("dve", lambda e: e.tensor_tensor(out=tr[2][:], in0=qn32[:, :, 8:16], in1=cosb, op=ALU.mult),
                     reads=["qn32", "cs"], writes=["tr2"])
                s.op("dve", lambda e: e.tensor_tensor(out=tr[3][:], in0=qn32[:, :, 0:8], in1=sinb, op=ALU.mult),
                     reads=["qn32", "cs"], writes=["tr3"])
                s.op("dve", lambda e: e.tensor_tensor(out=qkb[:, :, 0:8], in0=tr[0][:], in1=tr[1][:], op=ALU.subtract),
                     reads=["tr0", "tr1"], writes=["qkb_a"])
                s.op("dve", lambda e: e.tensor_tensor(out=qkb[:, :, 8:16], in0=tr[2][:], in1=tr[3][:], op=ALU.add),
                     reads=["tr2", "tr3"], writes=["qkb_b"])
                s.op("pool", lambda e: e.tensor_copy(out=qkb[:, :, 16:HD], in_=qn32[:, :, 16:HD]), reads=["qn32"], writes=["qkb_c"])
                s.op("dve", lambda e: e.tensor_copy(out=kd[:], in_=qkb[:, 16:20, :].unsqueeze(2).to_broadcast([128, 4, 2, HD])),
                     reads=["qkb_a", "qkb_b", "qkb_c"], writes=["kd"])

            def BK(i):
                p, pp = i % 2, 1 - (i % 2)
                KT, KTp, MX = kT[p], kT[pp], mixed[p]
                nkt, nktp, nmx = "kT%d" % p, "kT%d" % pp, "mixed%d" % p
                VV, VVp, UU, UUp = vv[i % 3], vv[(i - 1) % 3], uu[i % 3], uu[(i - 1) % 3]
                nvv, nvvp, nuu, nuup = "vv%d" % (i % 3), "vv%d" % ((i - 1) % 3), "uu%d" % (i % 3), "uu%d" % ((i - 1) % 3)
                kbs = [1] if i == 0 else [0, 1]
                nkb = len(kbs)
                qkbf = qkb[:].rearrange("p h d -> p (h d)")
                kdf = kd[:].rearrange("p g t d -> p (g t d)")

                def tq():
                    for b in range(8):
                        s.op("pe", lambda e, b=b: e.transpose(out=pt[:, b, :], in_=qkbf[:, b * 128:(b + 1) * 128], identity=ident),
                             reads=["qkb_a", "qkb_b", "qkb_c", "cst"], writes=["pt"])
                    s.op("dve", lambda e: e.tensor_copy(out=qT[:], in_=pt[:]), reads=[], writes=["pt", "qT"])
                    for g in range(4):
                        s.op("pe", lambda e, g=g: e.transpose(out=pt2[:, g, :], in_=kdf[:, g * 128:(g + 1) * 128], identity=ident),
                             reads=["kd", "cst"], writes=["sc0"])
                    s.op("act", lambda e: e.copy(out=KT[:], in_=pt2[:, 0:4, :]), reads=[], writes=["sc0", nkt])

                def scores(g):
                    for kb in kbs:
                        ktile, nk = (KTp, nktp) if kb == 0 else (KT, nkt)
                        for hf in range(2):
                            s.op("pe", lambda e, kb=kb, hf=hf, ktile=ktile: e.matmul(
                                out=sc[hf][:, kb, :, :], lhsT=ktile[hf * 64:(hf + 1) * 64, g, :],
                                rhs=qT[hf * 64:(hf + 1) * 64, 2 * g:2 * g + 2, :], start=True, stop=True),
                                reads=[nk, "qT"], writes=["sc%d" % hf])
                    k0 = kbs[0]
                    for hf in range(2):
                        P = PT[g % 2][hf]
                        nP = "PT%d%d" % (g % 2, hf)
                        s.op("act", lambda e, hf=hf, P=P: e.activation(out=P[:, k0:2], in_=sc[hf][:, k0:2], func=AF.Exp, scale=HD ** -0.5),
                             reads=[], writes=["sc%d" % hf, nP])
                        s.op("pool" if hf == 1 else "dve", lambda e, P=P: e.tensor_tensor(
                            out=P[:, k0:2], in0=P[:, k0:2], in1=mask2[:, k0:2].unsqueeze(2).to_broadcast([128, 2 - k0, 2, 128]), op=ALU.mult),
                            reads=[nP, "cst"], writes=[nP])

                PVB = (0, 1, 2)

                def pv(g):
                    for blk in range(2):
                        for hf in range(2):
                            h = 4 * g + 2 * blk + hf
                            bnk, j = PVB[h // 7], h % 7
                            o = pj[:, bnk, j * 65:(j + 1) * 65]
                            P = PT[g % 2][hf]
                            for n, kb in enumerate(kbs):
                                vt, nv = (VVp, nvvp) if kb == 0 else (VV, nvv)
                                s.op("pe", lambda e, o=o, P=P, kb=kb, blk=blk, vt=vt, n=n: e.matmul(
                                    out=o, lhsT=P[:, kb, blk, :], rhs=vt[:, g, :], start=(n == 0), stop=(n == nkb - 1)),
                                    reads=["PT%d%d" % (g % 2, hf), nv], writes=["pj%d" % bnk])

                def pv_evac(rnd):
                    bnk = PVB[rnd]
                    h0 = rnd * 7
                    nh = min(7, NH - h0)
                    v = pj[:, bnk, 0:nh * 65].rearrange("p (h d) -> p h d", d=65)
                    s.op("dve", lambda e: e.tensor_tensor(out=den[:, 0:nh], in0=v[:, :, 64], in1=esink[:, h0:h0 + nh], op=ALU.add),
                         reads=["esink"], writes=["pj%d" % bnk, "den"])
                    s.op("dve", lambda e: e.reciprocal(out=rden[:, 0:nh], in_=den[:, 0:nh]), reads=["den"], writes=["rden"])
                    s.op("dve", lambda e: e.tensor_tensor(
                        out=MX[:, h0 * HD:(h0 + nh) * HD].rearrange("p (h d) -> p h d", d=HD), in0=v[:, :, 0:HD],
                        in1=rden[:, 0:nh].unsqueeze(2).to_broadcast([128, nh, HD]), op=ALU.mult),
                        reads=["rden"], writes=["pj%d" % bnk, nmx + "_a%d" % rnd])

                def pool_dT():
                    for cc in range(8):
                        g = cc // 2
                        o = pj[:, 3 + cc // 4, (cc % 4) * 128:(cc % 4 + 1) * 128]
                        if i > 0:
                            s.op("pe", lambda e, o=o, cc=cc, g=g: e.matmul(out=o, lhsT=UUp[:, cc * 128:(cc + 1) * 128], rhs=poolm(1, g),
                                                                           start=True, stop=False),
                                 reads=[nuup, "cst"], writes=["pj%d" % (3 + cc // 4)])
                        s.op("pe", lambda e, o=o, cc=cc, g=g: e.matmul(out=o, lhsT=UU[:, cc * 128:(cc + 1) * 128],
                                                                       rhs=poolm(2 if i == 0 else 0, g), start=(i == 0), stop=True),
                             reads=[nuu, "cst"], writes=["pj%d" % (3 + cc // 4)])
                    s.op("act", lambda e: e.copy(out=dTs[:].rearrange("p a b -> p (a b)"), in_=pjf[:, 1536:2560]),
                         reads=[], writes=["pj3", "pj4", "dTs"])

                def pool_y():
                    for g in range(4):
                        for c2 in range(2):
                            s.op("pe", lambda e, g=g, c2=c2: e.matmul(out=pjf[:, 1536 + g * 256:1536 + (g + 1) * 256], lhsT=dTs[:, 2 * g + c2, :],
                                                                      rhs=wpool_bf[:, g, c2, :], start=(c2 == 0), stop=(c2 == 1)),
                                 reads=["dTs", "wpool"], writes=["pj%d" % (3 + g // 2)])
                    s.op("dve", lambda e: e.tensor_tensor(out=MX[:, 1024:2048], in0=pjf[:, 1536:2560], in1=pscale[:], op=ALU.mult),
                         reads=["pscale"], writes=["pj3", "pj4", nmx + "_p"])

                def sec0():
                    tq()

                def sec1():
                    scores(0)
                    pool_dT()

                def sec2():
                    scores(1)
                    pool_y()

                def sec3():
                    pv(0)
                    scores(2)
                    pv(1)
                    pv_evac(0)

                def sec4():
                    scores(3)
                    pv(2)
                    pv(3)
                    pv_evac(1)
                    pv_evac(2)

                return [sec0, sec1, sec2, sec3, sec4]

            def storeA(i):
                MX = mixed[i % 2]
                nmx = "mixed%d" % (i % 2)
                s.dma("sp", lambda e: e.dma_start(out=MIX[i * 128:(i + 1) * 128, :], in_=MX[:]),
                      reads=[nmx + "_a0", nmx + "_a1", nmx + "_a2", nmx + "_p"], writes=["MIX%d" % i], key="st_" + nmx)

            F1(0)
            if NT > 1:
                F1(1)
            F2(0, 0)
            for f in F2(0, 1):
                f()
            for st in range(NT):
                if st + 1 < NT:
                    F2(st + 1, 0)
                if st + 2 < NT:
                    F1(st + 2)
                if st >= 1:
                    storeA(st - 1)
                QKC(st)
                if st + 1 < NT:
                    for f in F2(st + 1, 1):
                        f()
                for f in BK(st):
                    f()
            storeA(NT - 1)
            s.barrier()
            pa.close()

        def phase_B():
            pb = ExitStack()
            w_out_bf = sbt(pb, "w_out_bf", [128, KD, D], BF16)
            gffn = sbt(pb, "gffn", [128, D], F32)
            wr_bf = sbt(pb, "wr_bf", [128, KD, 36], BF16)
            brb = sbt(pb, "brb", [128, 36], F32)
            xt = [sbt(pb, "xtB%d" % i, [128, D], F32) for i in range(3)]
            mx = [sbt(pb, "mxB%d" % i, [128, D], BF16) for i in range(3)]
            mxT = sbt(pb, "mxT", [128, KD, 128], BF16)
            junk = sbt(pb, "junkB", [128, D], BF16)
            ss = sbt(pb, "ssB", [128, 1], F32)
            std = sbt(pb, "stdB", [128, 1], F32)
            rstd = sbt(pb, "rstdB", [128, 1], F32)
            hn2 = [sbt(pb, "hn2_%d" % i, [128, D], BF16) for i in range(3)]
            hn2T = sbt(pb, "hn2T", [128, KD, 128], BF16)
            lg = sbt(pb, "lg", [128, 36], F32)
            cmax = sbt(pb, "cmax", [128, 1], F32)
            ncmax = sbt(pb, "ncmax", [128, 1], F32)
            ohg = sbt(pb, "ohg", [128, 4], F32)
            ec = sbt(pb, "ec", [128, 4], F32)
            sumc = sbt(pb, "sumc", [128, 1], F32)
            pgrp = sbt(pb, "pgrp", [128, 1], F32)
            tmp48 = sbt(pb, "tmp48", [128, 4, 8], F32)
            fsel = sbt(pb, "fsel", [128, 8], F32)
            m1 = sbt(pb, "m1", [128, 1], F32)
            oh1 = sbt(pb, "oh1", [128, 8], F32)
            msk = sbt(pb, "msk", [128, 8], F32)
            m2 = sbt(pb, "m2", [128, 1], F32)
            oh2 = sbt(pb, "oh2", [128, 8], F32)
            d21 = sbt(pb, "d21", [128, 1], F32)
            e21 = sbt(pb, "e21", [128, 1], F32)
            rr = sbt(pb, "rr", [128, 1], F32)
            E1 = sbt(pb, "E1", [128, 4, 8], F32)
            E2 = sbt(pb, "E2", [128, 4, 8], F32)
            Abf = sbt(pb, "Abf", [128, E], BF16)
            posv = sbt(pb, "posv", [128, E], F32)
            ov = sbt(pb, "ov", [128, E], F32)
            tmp32 = sbt(pb, "tmp32", [128, E], F32)
            dd = sbt(pb, "dd", [128, 2], F32)
            po = pst(pb, "po", [128, 4, 512], F32)
            ptB = pst(pb, "ptB", [128, 8, 128], BF16)
            ptB2 = pst(pb, "ptB2", [128, 8, 128], BF16)
            plg = pst(pb, "plg", [128, 512], F32)
            prk = pst(pb, "prk", [128, 512], F32)
            pof = po[:].rearrange("p a b -> p (a b)")

            for c in range(4):
                s.dma("pool", lambda e, c=c: e.dma_start(
                    out=w_out_bf[:, :, c * 512:(c + 1) * 512],
                    in_=w_out[:, c * 512:(c + 1) * 512].rearrange("(k p) n -> p k n", p=128)),
                    writes=["wout%d" % c], key="wout%d" % c)
            s.dma("pool", lambda e: e.dma_start(out=wr_bf[:], in_=w_r.rearrange("(k p) n -> p k n", p=128)), writes=["wr"], key="wr")
            s.dma("sp", lambda e: e.dma_start(out=gffn[:], in_=norm_ffn.partition_broadcast(128)), writes=["gffn"], key="gffn")
            s.dma("sp", lambda e: e.dma_start(out=brb[:], in_=b_r.partition_broadcast(128)), writes=["brb"], key="brb")
            if pre:
                s.dma("pool", lambda e: e.dma_start(out=pre["wg"][:], in_=w_gate[0].rearrange("(k p) n -> p k n", p=128)),
                      writes=["wg0"], key="wg0")
                s.dma("pool", lambda e: e.dma_start(out=pre["wu"][:], in_=w_up[0].rearrange("(k p) n -> p k n", p=128)),
                      writes=["wu0"], key="wu0")
                for hh_ in range(2):
                    s.dma("pool", lambda e, hh_=hh_: e.dma_start(
                        out=pre["wd"][:, :, hh_ * 1024:(hh_ + 1) * 1024],
                        in_=w_down[0][:, hh_ * 1024:(hh_ + 1) * 1024].rearrange("(j p) n -> p j n", p=128)),
                        writes=["wd0_%d" % hh_], key="wd0_%d" % hh_)

            ptBs = [ptB, ptB2]
            tcnt = [0]

            def transposeB(src, nsrc, dst, ndst):
                for r in range(2):
                    bk = tcnt[0] % 2
                    tcnt[0] += 1
                    PTb = ptBs[bk]
                    for b in range(8):
                        s.op("pe", lambda e, r=r, b=b, PTb=PTb: e.transpose(out=PTb[:, b, :], in_=src[:, (r * 8 + b) * 128:(r * 8 + b + 1) * 128],
                                                                            identity=ident),
                             reads=[nsrc, "cst"], writes=["ptB%d" % bk])
                    s.op("act", lambda e, r=r, PTb=PTb: e.copy(out=dst[:, r * 8:(r + 1) * 8, :], in_=PTb[:]), reads=["ptB%d" % bk],
                         writes=[ndst + "%d" % r])

            V = lambda fn, r, w: s.op("dve", fn, reads=r, writes=w)
            E1f = E1[:].rearrange("p g e -> p (g e)")
            E2f = E2[:].rearrange("p g e -> p (g e)")

            def loadsB(i):
                p = i % 3
                XT, MXb = xt[p], mx[p]
                nxt, nmx = "xtB%d" % p, "mxB%d" % p
                s.dma("sp", lambda e: e.dma_start(out=XT[:], in_=x[i * 128:(i + 1) * 128, :]), writes=[nxt], key=nxt)
                s.dma("sp", lambda e: e.dma_start(out=MXb[:], in_=MIX[i * 128:(i + 1) * 128, :]), writes=[nmx], key=nmx)

            def storeB(i):
                p = i % 3
                XT = xt[p]
                nxt = "xtB%d" % p
                s.dma("sp", lambda e: e.dma_start(out=out[i * 128:(i + 1) * 128, :], in_=XT[:]), reads=[nxt],
                      writes=["out%d" % i], key="st_" + nxt)

            def G1(i, part):
                p = i % 3
                XT, MXb = xt[p], mx[p]
                nxt, nmx = "xtB%d" % p, "mxB%d" % p
                if part == 0:
                    transposeB(MXb, nmx, mxT, "mxT")
                    return
                for c in range(4):
                    for k in range(KD):
                        s.op("pe", lambda e, c=c, k=k: e.matmul(out=po[:, c, :], lhsT=mxT[:, k, :], rhs=w_out_bf[:, k, c * 512:(c + 1) * 512],
                                                                start=(k == 0), stop=(k == KD - 1)),
                             reads=["mxT%d" % (k // 8), "wout%d" % c], writes=["po%d" % c])
                s.op("dve", lambda e: e.tensor_tensor(out=XT[:], in0=pof, in1=XT[:], op=ALU.add),
                     reads=["po0", "po1", "po2", "po3", nxt], writes=[nxt])

            def G2a(i):
                p = i % 3
                XT, H2 = xt[i % 3], hn2[p]
                nxt, nh2 = "xtB%d" % (i % 3), "hn2_%d" % p
                s.op("act", lambda e: e.activation(out=junk[:], in_=XT[:], func=AF.Square, accum_out=ss[:]),
                     reads=[nxt], writes=["junk", "ss"])
                s.op("act", lambda e: e.activation(out=std[:], in_=ss[:], func=AF.Ln, scale=1.0 / D, bias=epsb[:, 0:1]),
                     reads=["ss", "epsb"], writes=["std"])
                s.op("act", lambda e: e.activation(out=rstd[:], in_=std[:], func=AF.Exp, scale=-0.5), reads=["std"], writes=["rstd"])
                s.op("dve", lambda e: e.scalar_tensor_tensor(out=H2[:], in0=XT[:], scalar=rstd[:, 0:1], in1=gffn[:],
                                                             op0=ALU.mult, op1=ALU.mult),
                     reads=[nxt, "rstd", "gffn"], writes=[nh2])

            def G2b(i):
                p = i % 3
                H2 = hn2[p]
                nh2 = "hn2_%d" % p
                transposeB(H2, nh2, hn2T, "hn2T")
                for k in range(KD):
                    s.op("pe", lambda e, k=k: e.matmul(out=plg[:, 0:36], lhsT=hn2T[:, k, :], rhs=wr_bf[:, k, :],
                                                       start=(k == 0), stop=(k == KD - 1)),
                         reads=["hn2T%d" % (k // 8), "wr"], writes=["plg"])
                V(lambda e: e.tensor_tensor(out=lg[:], in0=plg[:, 0:36], in1=brb[:], op=ALU.add), ["plg", "brb"], ["lg"])

            def G3a(i):
                V(lambda e: e.tensor_reduce(out=cmax[:], in_=lg[:, 0:4], axis=AX.X, op=ALU.max), ["lg"], ["cmax"])
                V(lambda e: e.tensor_scalar(out=ohg[:], in0=lg[:, 0:4], scalar1=cmax[:, 0:1], scalar2=None, op0=ALU.is_ge), ["lg", "cmax"], ["ohg"])
                V(lambda e: e.tensor_scalar(out=ncmax[:], in0=cmax[:], scalar1=-1.0, scalar2=None, op0=ALU.mult), ["cmax"], ["ncmax"])
                s.op("act", lambda e: e.activation(out=ec[:], in_=lg[:, 0:4], func=AF.Exp, bias=ncmax[:, 0:1], accum_out=sumc[:]),
                     reads=["lg", "ncmax"], writes=["ec", "sumc"])
                V(lambda e: e.tensor_tensor(out=tmp48[:], in0=lg[:, 4:36].rearrange("p (g e) -> p g e", g=4),
                                            in1=ohg[:].unsqueeze(2).to_broadcast([128, 4, 8]), op=ALU.mult), ["lg", "ohg"], ["tmp48"])
                V(lambda e: e.tensor_reduce(out=fsel[:], in_=tmp48[:].rearrange("p g e -> p e g"), axis=AX.X, op=ALU.add), ["tmp48"], ["fsel"])
                V(lambda e: e.tensor_reduce(out=m1[:], in_=fsel[:], axis=AX.X, op=ALU.max), ["fsel"], ["m1"])
                V(lambda e: e.tensor_scalar(out=oh1[:], in0=fsel[:], scalar1=m1[:, 0:1], scalar2=None, op0=ALU.is_ge), ["fsel", "m1"], ["oh1"])
                V(lambda e: e.scalar_tensor_tensor(out=msk[:], in0=oh1[:], scalar=-1e30, in1=fsel[:], op0=ALU.mult, op1=ALU.add),
                  ["oh1", "fsel"], ["msk"])
                V(lambda e: e.tensor_reduce(out=m2[:], in_=msk[:], axis=AX.X, op=ALU.max), ["msk"], ["m2"])
                V(lambda e: e.tensor_scalar(out=oh2[:], in0=msk[:], scalar1=m2[:, 0:1], scalar2=None, op0=ALU.is_ge), ["msk", "m2"], ["oh2"])
                V(lambda e: e.tensor_tensor(out=d21[:], in0=m2[:], in1=m1[:], op=ALU.subtract), ["m1", "m2"], ["d21"])
                s.op("act", lambda e: e.activation(out=e21[:], in_=d21[:], func=AF.Exp), reads=["d21"], writes=["e21"])
                V(lambda e: e.tensor_tensor(out=E1[:], in0=ohg[:].unsqueeze(2).to_broadcast([128, 4, 8]),
                                            in1=oh1[:].unsqueeze(1).to_broadcast([128, 4, 8]), op=ALU.mult), ["ohg", "oh1"], ["E1"])
                V(lambda e: e.tensor_tensor(out=E2[:], in0=ohg[:].unsqueeze(2).to_broadcast([128, 4, 8]),
                                            in1=oh2[:].unsqueeze(1).to_broadcast([128, 4, 8]), op=ALU.mult), ["ohg", "oh2"], ["E2"])
                V(lambda e: e.tensor_tensor(out=Abf[:], in0=E1f, in1=E2f, op=ALU.add), ["E1", "E2"], ["Abf"])
                V(lambda e: e.reciprocal(out=pgrp[:], in_=sumc[:]), ["sumc"], ["pgrp"])
                V(lambda e: e.tensor_scalar(out=e21[:], in0=e21[:], scalar1=1.0, scalar2=None, op0=ALU.add), ["e21"], ["e21"])
                V(lambda e: e.reciprocal(out=rr[:], in_=e21[:]), ["e21"], ["rr"])
                V(lambda e: e.tensor_tensor(out=GATE[:, i, 0:1], in0=pgrp[:], in1=rr[:], op=ALU.mult), ["pgrp", "rr"], ["GATE"])
                V(lambda e: e.tensor_tensor(out=GATE[:, i, 1:2], in0=pgrp[:], in1=GATE[:, i, 0:1], op=ALU.subtract), ["pgrp", "GATE"], ["GATE"])

            def G3b(i):
                p = i % 3
                H2 = hn2[p]
                nh2 = "hn2_%d" % p
                s.op("pe", lambda e: e.matmul(out=prk[:, 0:E], lhsT=tri, rhs=Abf[:], start=True, stop=True), reads=["Abf", "cst"], writes=["prk"])
                s.op("pe", lambda e: e.matmul(out=prk[:, E:2 * E], lhsT=ones, rhs=Abf[:], start=True, stop=True), reads=["Abf", "cst"], writes=["prk"])
                V(lambda e: e.tensor_tensor(out=posv[:], in0=prk[:, 0:E], in1=base[:], op=ALU.add), ["prk", "base"], ["posv"])
                V(lambda e: e.tensor_tensor(out=base[:], in0=prk[:, E:2 * E], in1=base[:], op=ALU.add), ["prk", "base"], ["base"])
                V(lambda e: e.tensor_scalar(out=ov[:], in0=posv[:], scalar1=float(C), scalar2=1e9, op0=ALU.is_ge, op1=ALU.mult), ["posv"], ["ov"])
                V(lambda e: e.tensor_tensor(out=posv[:], in0=posv[:], in1=ov[:], op=ALU.add), ["posv", "ov"], ["posv"])
                V(lambda e: e.tensor_tensor(out=posv[:], in0=posv[:], in1=eoff[:], op=ALU.add), ["posv", "eoff"], ["posv"])
                V(lambda e: e.tensor_tensor(out=tmp32[:], in0=E1f, in1=posv[:], op=ALU.mult), ["E1", "posv"], ["tmp32"])
                V(lambda e: e.tensor_reduce(out=dd[:, 0:1], in_=tmp32[:], axis=AX.X, op=ALU.add), ["tmp32"], ["dd"])
                V(lambda e: e.tensor_tensor(out=tmp32[:], in0=E2f, in1=posv[:], op=ALU.mult), ["E2", "posv", "dd"], ["tmp32"])
                V(lambda e: e.tensor_reduce(out=dd[:, 1:2], in_=tmp32[:], axis=AX.X, op=ALU.add), ["tmp32"], ["dd"])
                V(lambda e: e.tensor_copy(out=DEST[:, i, :], in_=dd[:]), ["dd"], ["DEST%d" % i])
                for j in range(2):
                    s.dma("pool", lambda e, j=j: e.indirect_dma_start(
                        out=XS, out_offset=bass.IndirectOffsetOnAxis(ap=DEST[:, i, j:j + 1], axis=0),
                        in_=H2[:], in_offset=None, bounds_check=bcreg(e), oob_is_err=False),
                        reads=[nh2, "DEST%d" % i], writes=["XS_%d_%d" % (i, j)], key="sc_" + nh2)

            loadsB(0)
            for st in range(NT + 2):
                if st + 1 < NT:
                    loadsB(st + 1)
                if 0 <= st - 1 < NT:
                    storeB(st - 1)
                if st < NT:
                    G1(st, 0)
                if 0 <= st - 1 < NT:
                    G2a(st - 1)
                if 0 <= st - 2 < NT:
                    G3a(st - 2)
                if st < NT:
                    G1(st, 1)
                if 0 <= st - 1 < NT:
                    G2b(st - 1)
                if 0 <= st - 2 < NT:
                    G3b(st - 2)
            s.barrier()
            pb.close()

        def phase_C():
            pc = ExitStack()
            if pre:
                wg = [pre["wg"], sbt(pc, "wg1", [128, KD, DE], BF16)]
                wu = [pre["wu"], sbt(pc, "wu1", [128, KD, DE], BF16)]
                wd = [pre["wd"], sbt(pc, "wd1", [128, 4, D], BF16)]
            else:
                wg = [sbt(pc, "wg%d" % i, [128, KD, DE], BF16) for i in range(2)]
                wu = [sbt(pc, "wu%d" % i, [128, KD, DE], BF16) for i in range(2)]
                wd = [sbt(pc, "wd%d" % i, [128, 4, D], BF16) for i in range(2)]
            xr = [sbt(pc, "xr%d" % i, [128, D], BF16) for i in range(3)]
            xT = [sbt(pc, "xTC%d" % i, [128, KD, C], BF16) for i in range(2)]
            hT = [sbt(pc, "hTC%d" % i, [128, 4, C], BF16) for i in range(2)]
            sil = [sbt(pc, "sil%d" % i, [128, C], F32) for i in range(2)]
            ys = [sbt(pc, "ys%d" % i, [128, D], BF16) for i in range(4)]
            pab = [[pst(pc, "pab%d%d" % (a, b), [128, 512], F32) for b in range(2)] for a in range(2)]
            py = [pst(pc, "py%d" % i, [128, 512], F32) for i in range(2)]
            ptC = [pst(pc, "ptC%d" % i, [128, 8, 128], BF16) for i in range(2)]
            cnt = dict(row=0, py=0, half=0, cp=0)

            def load_w(ex):
                p = ex % 2
                WG, WU, WD = wg[p], wu[p], wd[p]
                s.dma("pool", lambda e: e.dma_start(out=WG[:], in_=w_gate[ex].rearrange("(k p) n -> p k n", p=128)),
                      writes=["wg%d" % p], key="wg%d" % p)
                s.dma("pool", lambda e: e.dma_start(out=WU[:], in_=w_up[ex].rearrange("(k p) n -> p k n", p=128)),
                      writes=["wu%d" % p], key="wu%d" % p)
                for hh_ in range(2):
                    s.dma("pool", lambda e, hh_=hh_: e.dma_start(
                        out=WD[:, :, hh_ * 1024:(hh_ + 1) * 1024],
                        in_=w_down[ex][:, hh_ * 1024:(hh_ + 1) * 1024].rearrange("(j p) n -> p j n", p=128)),
                        writes=["wd%d_%d" % (p, hh_)], key="wd%d_%d" % (p, hh_))

            def load_x(ex):
                for r in range(CT):
                    q = (ex * CT + r) % 3
                    XR = xr[q]
                    s.dma("sp", lambda e, XR=XR, r=r: e.dma_start(out=XR[0:rws(r), :], in_=XS[ex * C + r * 128:ex * C + r * 128 + rws(r), :]),
                          writes=["xr%d" % q], key="xr%d" % q)

            def transposes(ex):
                p = ex % 2
                XT = xT[p]
                for r in range(CT):
                    q = (ex * CT + r) % 3
                    XR = xr[q]
                    for rr_ in range(2):
                        hf = cnt["half"] % 2
                        cnt["half"] += 1
                        for b in range(8):
                            kk = rr_ * 8 + b
                            s.op("pe", lambda e, XR=XR, kk=kk, hf=hf, b=b, r=r: e.transpose(
                                out=ptC[hf][:, b, 0:rws(r)], in_=XR[0:rws(r), kk * 128:(kk + 1) * 128], identity=ident[0:rws(r), 0:rws(r)]),
                                 reads=["xr%d" % q, "cst"], writes=["ptC%d" % hf])
                        if hf == 0:
                            s.op("act", lambda e, rr_=rr_, r=r, hf=hf: e.copy(out=XT[:, rr_ * 8:(rr_ + 1) * 8, r * 128:r * 128 + rws(r)],
                                                                             in_=ptC[hf][:, :, 0:rws(r)]),
                                 reads=["ptC%d" % hf], writes=["xT%d" % p])
                        else:
                            s.op("dve", lambda e, rr_=rr_, r=r, hf=hf: e.tensor_copy(out=XT[:, rr_ * 8:(rr_ + 1) * 8, r * 128:r * 128 + rws(r)],
                                                                                    in_=ptC[hf][:, :, 0:rws(r)]),
                                 reads=["ptC%d" % hf], writes=["xT%d" % p])

            def gateup(ex):
                p = ex % 2
                WG, WU, XT, HT = wg[p], wu[p], xT[p], hT[p]
                for j in range(4):
                    PA, PB = pab[j % 2]
                    na, nb = "pab%d0" % (j % 2), "pab%d1" % (j % 2)
                    for k in range(KD):
                        s.op("pe", lambda e, PA=PA, j=j, k=k: e.matmul(out=PA[:, 0:C], lhsT=WG[:, k, j * 128:(j + 1) * 128], rhs=XT[:, k, :],
                                                                       start=(k == 0), stop=(k == KD - 1)),
                             reads=["wg%d" % p, "xT%d" % p], writes=[na])
                    for k in range(KD):
                        s.op("pe", lambda e, PB=PB, j=j, k=k: e.matmul(out=PB[:, 0:C], lhsT=WU[:, k, j * 128:(j + 1) * 128], rhs=XT[:, k, :],
                                                                       start=(k == 0), stop=(k == KD - 1)),
                             reads=["wu%d" % p, "xT%d" % p], writes=[nb])
                    SL = sil[j % 2]
                    s.op("act", lambda e, PA=PA, SL=SL: e.activation(out=SL[:], in_=PA[:, 0:C], func=AF.Silu), reads=[na], writes=["sil%d" % (j % 2)])
                    s.op("dve", lambda e, PB=PB, SL=SL, j=j: e.tensor_tensor(out=HT[:, j, :], in0=PB[:, 0:C], in1=SL[:], op=ALU.mult),
                         reads=[nb, "sil%d" % (j % 2)], writes=["hT%d" % p])

            def down(ex):
                p = ex % 2
                WD, HT = wd[p], hT[p]
                for r in range(CT):
                    q = (ex * CT + r) % 4
                    YSb = ys[q]
                    for c in range(4):
                        PY = py[cnt["py"] % 2]
                        npn = "py%d" % (cnt["py"] % 2)
                        for j in range(4):
                            s.op("pe", lambda e, PY=PY, r=r, c=c, j=j: e.matmul(out=PY[0:rws(r), :], lhsT=HT[:, j, r * 128:r * 128 + rws(r)],
                                                                                rhs=WD[:, j, c * 512:(c + 1) * 512], start=(j == 0), stop=(j == 3)),
                                 reads=["hT%d" % p, "wd%d_%d" % (p, c // 2)], writes=[npn])
                        if cnt["py"] % 2 == 0:
                            s.op("act", lambda e, PY=PY, YSb=YSb, c=c, r=r: e.copy(out=YSb[0:rws(r), c * 512:(c + 1) * 512], in_=PY[0:rws(r), :]),
                                 reads=[npn], writes=["ys%d" % q])
                        else:
                            s.op("dve", lambda e, PY=PY, YSb=YSb, c=c, r=r: e.tensor_copy(out=YSb[0:rws(r), c * 512:(c + 1) * 512], in_=PY[0:rws(r), :]),
                                 reads=[npn], writes=["ys%d" % q])
                        cnt["py"] += 1
                    s.dma("sp", lambda e, YSb=YSb, r=r: e.dma_start(out=YS[ex * C + r * 128:ex * C + r * 128 + rws(r), :], in_=YSb[0:rws(r), :]),
                          reads=["ys%d" % q], writes=["YS_%d_%d" % (ex, r)], key="st_ys%d" % q)

            if not pre:
                load_w(0)
            load_x(0)
            transposes(0)
            for ex in range(E):
                if ex + 1 < E:
                    load_w(ex + 1)
                    load_x(ex + 1)
                gateup(ex)
                if ex + 1 < E:
                    transposes(ex + 1)
                down(ex)
            s.barrier()
            pc.close()

        def phase_D():
            pd = ExitStack()
            NS = 4
            ya = [sbt(pd, "ya%d" % i, [128, D], BF16) for i in range(NS)]
            yb = [sbt(pd, "yb%d" % i, [128, D], BF16) for i in range(NS)]
            hh = [sbt(pd, "hh%d" % i, [128, D], F32) for i in range(NS)]
            for i in range(NS):
                s.op("dve", lambda e, i=i: e.memset(ya[i][:], 0.0), writes=["ya%d" % i])
                s.op("pool", lambda e, i=i: e.memset(yb[i][:], 0.0), writes=["yb%d" % i])

            def loadsD(i):
                p = i % NS
                YA, YB, HH = ya[p], yb[p], hh[p]
                s.dma("pool", lambda e: e.indirect_dma_start(
                    out=YA[:], out_offset=None, in_=YS, in_offset=bass.IndirectOffsetOnAxis(ap=DEST[:, i, 0:1], axis=0),
                    bounds_check=bcreg(e), oob_is_err=False), writes=["ya%d" % p], key="ya%d" % p)
                s.dma("pool", lambda e: e.indirect_dma_start(
                    out=YB[:], out_offset=None, in_=YS, in_offset=bass.IndirectOffsetOnAxis(ap=DEST[:, i, 1:2], axis=0),
                    bounds_check=bcreg(e), oob_is_err=False), writes=["yb%d" % p], key="yb%d" % p)
                s.dma("sp", lambda e: e.dma_start(out=HH[:], in_=out[i * 128:(i + 1) * 128, :]), writes=["hh%d" % p], key="hh%d" % p)

            def combineD(i):
                p = i % NS
                YA, YB, HH = ya[p], yb[p], hh[p]
                s.op("dve", lambda e: e.scalar_tensor_tensor(out=HH[:], in0=YA[:], scalar=GATE[:, i, 0:1], in1=HH[:],
                                                             op0=ALU.mult, op1=ALU.add),
                     reads=["ya%d" % p, "hh%d" % p], writes=["hh%d" % p])
                s.op("dve", lambda e: e.scalar_tensor_tensor(out=HH[:], in0=YB[:], scalar=GATE[:, i, 1:2], in1=HH[:],
                                                             op0=ALU.mult, op1=ALU.add),
                     reads=["yb%d" % p, "hh%d" % p], writes=["hh%d" % p])
                s.dma("sp", lambda e: e.dma_start(out=out[i * 128:(i + 1) * 128, :], in_=HH[:]), reads=["hh%d" % p],
                      writes=["outD%d" % i], key="st_hh%d" % p)

            for i in range(NT + 2):
                if i < NT:
                    loadsD(i)
                if i - 2 >= 0:
                    combineD(i - 2)
            pd.close()

        pre = {}
        for ph, fn in (("A", phase_A), ("B", phase_B), ("C", phase_C), ("D", phase_D)):
            if ph == "B" and "B" in phases and "C" in phases:
                pbc = ExitStack()
                pre["wg"] = sbt(pbc, "wg0", [128, KD, DE], BF16)
                pre["wu"] = sbt(pbc, "wu0", [128, KD, DE], BF16)
                pre["wd"] = sbt(pbc, "wd0", [128, 4, D], BF16)
            if ph in phases:
                fn()
            if ph == "C" and pre:
                pbc.close()
        if debug:
            s.barrier()
            s.dma("sp", lambda e: e.dma_start(out=DBGD, in_=DEST[:]), key="dbgd")
            s.dma("sp", lambda e: e.dma_start(out=DBGG, in_=GATE[:]), key="dbgg")
        s.finish()
        info = run_schedule(nc, s, es)
    return nc, info


CAP = 352
_PROG_CACHE = {}


def make_in_map(xb, prm, consts):
    cst, cs, eoff = consts
    m = dict(prm)
    m["x"] = np.ascontiguousarray(xb)
    m["cst"] = cst
    m["cs"] = cs
    m["eoff"] = eoff
    return m


def prep_params(norm_mix, w_in, q_norm, k_norm, sinks, w_pool, pool_scale, w_out, norm_ffn,
                w_coarse, b_coarse, w_fine, b_fine, w_gate, w_up, w_down):
    f = lambda a: np.ascontiguousarray(np.asarray(a, dtype=np.float32))
    return dict(
        norm_mix=f(norm_mix).reshape(1, D), w_in=f(w_in).reshape(D, INW), q_norm=f(q_norm).reshape(1, HD),
        k_norm=f(k_norm).reshape(1, HD), sinks=f(sinks).reshape(1, NH), w_pool=f(w_pool).reshape(4, 256, 256),
        pool_scale=f(pool_scale).reshape(1, 1024), w_out=f(w_out).reshape(D, D), norm_ffn=f(norm_ffn).reshape(1, D),
        w_r=np.ascontiguousarray(np.concatenate([f(w_coarse).reshape(D, 4), f(w_fine).reshape(D, E)], axis=1)),
        b_r=np.ascontiguousarray(np.concatenate([f(b_coarse).reshape(1, 4), f(b_fine).reshape(1, E)], axis=1)),
        w_gate=f(w_gate).reshape(E, D, DE), w_up=f(w_up).reshape(E, D, DE), w_down=f(w_down).reshape(E, DE, D))


def kernel(x, norm_mix, w_in, q_norm, k_norm, sinks, w_pool, pool_scale, w_out, norm_ffn,
           w_coarse, b_coarse, w_fine, b_fine, w_gate, w_up, w_down):
    x = np.asarray(x, dtype=np.float32)
    B, S, _ = x.shape
    assert B == N_CORES
    prm = prep_params(norm_mix, w_in, q_norm, k_norm, sinks, w_pool, pool_scale, w_out, norm_ffn,
                      w_coarse, b_coarse, w_fine, b_fine, w_gate, w_up, w_down)
    key = (S, CAP)
    if key not in _PROG_CACHE:
        _PROG_CACHE[key] = build_program(S, CAP)[0]
    nc = _PROG_CACHE[key]
    consts = build_consts(S, CAP)
    in_maps = [make_in_map(x[b], prm, consts) for b in range(B)]
    res = run_bass_kernel_spmd(nc, in_maps, core_ids=list(range(B)))
    return np.stack([np.asarray(r["out"], dtype=np.float32) for r in res.results], axis=0)
```

```python
import numpy as np
from contextlib import ExitStack
import concourse.bass as bass
import concourse.mybir as mybir
from concourse.bass_utils import run_bass_kernel_spmd

F32 = mybir.dt.float32
BF16 = mybir.dt.bfloat16
I32 = mybir.dt.int32
AF = mybir.ActivationFunctionType
ALU = mybir.AluOpType
AX = mybir.AxisListType

D = 2048
KD = 16
INW = 2560
NH = 16
NKV = 4
HD = 64
E = 32
DE = 512
EPS = 1e-6
ROPE_THETA = 500000.0
POOL_WINDOWS = (2, 4, 8, 16)
N_CORES = 8

ENGINES = ("pe", "act", "dve", "pool", "sp")


class _Buf:
    __slots__ = ("last_w", "readers")

    def __init__(self):
        self.last_w = None
        self.readers = {}


class _Op:
    __slots__ = ("eng", "fn", "deps", "inc", "semval", "is_dma", "key")

    def __init__(self, eng, fn, is_dma, key):
        self.eng = eng
        self.fn = fn
        self.deps = []
        self.inc = False
        self.semval = None
        self.is_dma = is_dma
        self.key = key


class Sched:
    def __init__(self):
        self.ops = []
        self.bufs = {}
        self.last_eng = {}
        self.last_dma = {}
        self.barrier_deps = []
        self.final_waits = []

    def _b(self, name):
        b = self.bufs.get(name)
        if b is None:
            b = _Buf()
            self.bufs[name] = b
        return b

    def _add(self, eng, fn, reads, writes, is_dma, key):
        op = _Op(eng, fn, is_dma, key)
        deps = {}
        for r in reads:
            b = self._b(r)
            if b.last_w is not None:
                deps[id(b.last_w)] = b.last_w
        for w in writes:
            b = self._b(w)
            p = b.last_w
            if p is not None and (is_dma or p.is_dma or p.eng != eng):
                deps[id(p)] = p
            for p in b.readers.values():
                if is_dma or p.is_dma or p.eng != eng:
                    deps[id(p)] = p
        for p in self.barrier_deps:
            deps[id(p)] = p
        rk = ("d", key) if is_dma else eng
        for r in reads:
            self._b(r).readers[rk] = op
        for w in writes:
            b = self._b(w)
            b.last_w = op
            b.readers = {}
        op.deps = list(deps.values())
        for p in op.deps:
            p.inc = True
        self.ops.append(op)
        if is_dma:
            self.last_dma[key] = op
        else:
            self.last_eng[eng] = op
        return op

    def op(self, eng, fn, reads=(), writes=()):
        return self._add(eng, fn, reads, writes, False, None)

    def dma(self, eng, fn, reads=(), writes=(), key=None):
        op = self._add(eng, fn, reads, writes, True, key)
        op.inc = True
        return op

    def barrier(self):
        self.barrier_deps = list(self.last_eng.values()) + list(self.last_dma.values())
        self.bufs = {}

    def finish(self):
        self.final_waits = list(self.last_dma.values())


def run_schedule(nc, sched, es):
    eng_sem = {e: es.enter_context(nc.semaphore("s_" + e)) for e in ENGINES}
    dma_sem, dma_cnt = {}, {}
    eng_cnt = {e: 0 for e in ENGINES}
    for op in sched.ops:
        if op.is_dma:
            if op.key not in dma_sem:
                dma_sem[op.key] = es.enter_context(nc.semaphore("d_" + op.key))
                dma_cnt[op.key] = 0
            dma_cnt[op.key] += 16
            op.semval = dma_cnt[op.key]
        elif op.inc:
            eng_cnt[op.eng] += 1
            op.semval = eng_cnt[op.eng]
    per_eng = {e: [o for o in sched.ops if o.eng == e] for e in ENGINES}

    def sem_of(p):
        return dma_sem[p.key] if p.is_dma else eng_sem[p.eng]

    def body(ename, eng):
        waited = {}

        def wait(p):
            s = sem_of(p)
            if waited.get(id(s), 0) >= p.semval:
                return
            eng.wait_ge(s, p.semval)
            waited[id(s)] = p.semval

        for op in per_eng[ename]:
            need = {}
            for p in op.deps:
                k = id(sem_of(p))
                if k not in need or need[k].semval < p.semval:
                    need[k] = p
            for p in need.values():
                wait(p)
            ins = op.fn(eng)
            if op.is_dma:
                ins.then_inc(dma_sem[op.key], 16)
            elif op.inc:
                ins.then_inc(eng_sem[ename], 1)
        if ename == "sp":
            for p in sched.final_waits:
                wait(p)

    with nc.Block() as block:
        @block.tensor
        def _(e):
            body("pe", e)

        @block.scalar
        def _(e):
            body("act", e)

        @block.vector
        def _(e):
            body("dve", e)

        @block.gpsimd
        def _(e):
            body("pool", e)

        @block.sync
        def _(e):
            body("sp", e)
    return dict(n_ops=len(sched.ops), n_dma_sems=len(dma_sem), eng_cnt=eng_cnt)


C_ID, C_TRI, C_ONE, C_MPREV, C_MCUR, C_POOL = 0, 128, 256, 384, 512, 640
NCST = 640 + 12 * 128


def build_consts(S, C):
    NT = S // 128
    cst = np.zeros((128, NCST), np.float32)
    idx = np.arange(128)
    cst[:, C_ID:C_ID + 128] = np.eye(128, dtype=np.float32)
    cst[:, C_TRI:C_TRI + 128] = (idx[:, None] < idx[None, :])
    cst[:, C_ONE:C_ONE + 128] = 1.0
    cst[:, C_MPREV:C_MPREV + 128] = (idx[:, None] > idx[None, :])
    cst[:, C_MCUR:C_MCUR + 128] = (idx[:, None] <= idx[None, :])
    tp = idx[:, None]
    t = idx[None, :]
    for g, w in enumerate(POOL_WINDOWS):
        mcur = ((tp <= t) & (tp > t - w)) / float(w) - (tp == t)
        mprev = (tp >= t + 129 - w) / float(w)
        cnt = np.minimum(t + 1, w).astype(np.float32)
        m0 = ((tp <= t) & (tp > t - w)) / cnt - (tp == t)
        cst[:, C_POOL + (0 + g) * 128:C_POOL + (1 + g) * 128] = mcur
        cst[:, C_POOL + (4 + g) * 128:C_POOL + (5 + g) * 128] = mprev
        cst[:, C_POOL + (8 + g) * 128:C_POOL + (9 + g) * 128] = m0
    pos = np.arange(S, dtype=np.float32)
    rot = HD // 4
    inv_freq = (ROPE_THETA ** (-np.arange(0, rot, 2, dtype=np.float32) / rot)).astype(np.float32)
    ang = pos[:, None] * inv_freq[None, :]
    cs = np.zeros((128, NT, 16), np.float32)
    cs[:, :, 0:8] = np.cos(ang).astype(np.float32).reshape(NT, 128, 8).transpose(1, 0, 2)
    cs[:, :, 8:16] = np.sin(ang).astype(np.float32).reshape(NT, 128, 8).transpose(1, 0, 2)
    eoff = np.tile((np.arange(E, dtype=np.float32) * C)[None, :], (128, 1))
    return cst, cs.reshape(128, NT * 16), eoff


def build_program(S, C, phases="ABCD", debug=False):
    NT = S // 128
    CT = (C + 127) // 128

    def rws(r):
        return min(128, C - r * 128)
    nc = bass.Bass("TRN2", target_bir_lowering=False)

    def din(name, shape, dt=F32):
        return nc.dram_tensor(name, shape, dt, kind="ExternalInput").ap()

    x = din("x", [S, D])
    norm_mix = din("norm_mix", [1, D])
    w_in = din("w_in", [D, INW])
    q_norm = din("q_norm", [1, HD])
    k_norm = din("k_norm", [1, HD])
    sinks = din("sinks", [1, NH])
    w_pool = din("w_pool", [4, 256, 256])
    pool_scale = din("pool_scale", [1, 1024])
    w_out = din("w_out", [D, D])
    norm_ffn = din("norm_ffn", [1, D])
    w_r = din("w_r", [D, 36])
    b_r = din("b_r", [1, 36])
    w_gate = din("w_gate", [E, D, DE])
    w_up = din("w_up", [E, D, DE])
    w_down = din("w_down", [E, DE, D])
    cst_d = din("cst", [128, NCST])
    cs_d = din("cs", [128, NT * 16])
    eoff_d = din("eoff", [128, E])
    out = nc.dram_tensor("out", [S, D], F32, kind="ExternalOutput").ap()
    dk = dict(kind="ExternalOutput") if debug else {}
    MIX = nc.dram_tensor("MIX", [S, D], BF16, **dk).ap()
    XS = nc.dram_tensor("XS", [E * C, D], BF16, **dk).ap()
    YS = nc.dram_tensor("YS", [E * C, D], BF16, **dk).ap()
    if debug:
        DBGD = nc.dram_tensor("DBGD", [128, S // 128, 2], I32, kind="ExternalOutput").ap()
        DBGG = nc.dram_tensor("DBGG", [128, S // 128, 2], F32, kind="ExternalOutput").ap()

    s = Sched()
    taps = {}
    regcache = {}

    def bcreg(e):
        if "bc" not in regcache:
            regcache["bc"] = e.to_reg(E * C - 1)
        return regcache["bc"]

    def tap(name, ap, reads, cond=True):
        if not (debug and cond):
            return
        shp = list(ap.shape)
        t = nc.dram_tensor("T_" + name, shp, ap.dtype, kind="ExternalOutput").ap()
        taps[name] = t
        s.dma("sp", lambda e: e.dma_start(out=t, in_=ap), reads=reads, key="tap")

    with ExitStack() as es:
        def sbt(stack, name, shape, dt):
            return stack.enter_context(nc.sbuf_tensor("sb_" + name, shape, dt))

        def pst(stack, name, shape, dt):
            return stack.enter_context(nc.psum_tensor("ps_" + name, shape, dt))

        cst = sbt(es, "cst", [128, NCST], BF16)
        DEST = sbt(es, "DEST", [128, NT, 2], I32)
        GATE = sbt(es, "GATE", [128, NT, 2], F32)
        base = sbt(es, "base", [128, E], F32)
        eoff = sbt(es, "eoff", [128, E], F32)
        for a, b in ((0, 1024), (1024, NCST)):
            s.dma("pool", lambda e, a=a, b=b: e.dma_start(out=cst[:, a:b], in_=cst_d[:, a:b]), writes=["cst_%d" % a], key="cst")
        s.op("pool", lambda e: e.memset(DEST[:], 0), reads=["cst_0", "cst_1024"], writes=["cst"])
        s.dma("sp", lambda e: e.dma_start(out=eoff[:], in_=eoff_d), writes=["eoff"], key="eoff")
        s.op("dve", lambda e: e.memset(base[:], 0.0), writes=["base"])
        epsb = sbt(es, "epsb", [128, 1], F32)
        s.op("dve", lambda e: e.memset(epsb[:], EPS), writes=["epsb"])
        ident = cst[:, C_ID:C_ID + 128]
        tri = cst[:, C_TRI:C_TRI + 128]
        ones = cst[:, C_ONE:C_ONE + 128]
        mask2 = cst[:, C_MPREV:C_MPREV + 256].rearrange("p (a q) -> p a q", a=2)

        def poolm(kind, g):
            o = C_POOL + (kind * 4 + g) * 128
            return cst[:, o:o + 128]

        def phase_A():
            pa = ExitStack()
            w_in_bf = sbt(pa, "w_in_bf", [128, KD, INW], BF16)
            gmix = sbt(pa, "gmix", [128, D], F32)
            qg = sbt(pa, "qg", [128, HD], F32)
            kg = sbt(pa, "kg", [128, HD], F32)
            gqk = sbt(pa, "gqk", [128, 20, HD], F32)
            esink = sbt(pa, "esink", [128, NH], F32)
            pscale = sbt(pa, "pscale", [128, 1024], F32)
            wpool_bf = sbt(pa, "wpool_bf", [128, 4, 2, 256], BF16)
            cs = sbt(pa, "cs", [128, NT, 16], F32)
            xt = [sbt(pa, "xtA%d" % i, [128, D], F32) for i in range(2)]
            junk = sbt(pa, "junkA", [128, D], BF16)
            ss = sbt(pa, "ssA", [128, 1], F32)
            std = sbt(pa, "stdA", [128, 1], F32)
            rstd = sbt(pa, "rstdA", [128, 1], F32)
            hn = [sbt(pa, "hnA%d" % i, [128, D], BF16) for i in range(2)]
            hnT = sbt(pa, "hnTA", [128, KD, 128], BF16)
            qk32 = [sbt(pa, "qk32_%d" % i, [128, 1280], F32) for i in range(2)]
            sq32 = sbt(pa, "sq32", [128, 1280], F32)
            ssq = sbt(pa, "ssq", [128, 20], F32)
            stq = sbt(pa, "stq", [128, 20], F32)
            rq = sbt(pa, "rq", [128, 20], F32)
            qn32 = sbt(pa, "qn32", [128, 20, HD], F32)
            qg32 = sbt(pa, "qg32", [128, 20, HD], F32)
            tr = [sbt(pa, "tr%d" % i, [128, 20, 8], F32) for i in range(4)]
            qkb = sbt(pa, "qkb", [128, 20, HD], BF16)
            kd = sbt(pa, "kd", [128, 4, 2, HD], BF16)
            qT = sbt(pa, "qT", [128, 8, 128], BF16)
            kT = [sbt(pa, "kT%d" % i, [128, 4, 128], BF16) for i in range(2)]
            vv = [sbt(pa, "vv%d" % i, [128, 4, 65], BF16) for i in range(3)]
            uu = [sbt(pa, "uu%d" % i, [128, 1024], BF16) for i in range(3)]
            PT = [[sbt(pa, "PT%d%d" % (a, b), [128, 2, 2, 128], BF16) for b in range(2)] for a in range(2)]
            den = sbt(pa, "den", [128, 8], F32)
            rden = sbt(pa, "rden", [128, 8], F32)
            dTs = sbt(pa, "dTs", [128, 8, 128], BF16)
            mixed = [sbt(pa, "mixed%d" % i, [128, D], BF16) for i in range(2)]
            pj = pst(pa, "pj", [128, 5, 512], F32)
            pt = pst(pa, "ptA", [128, 8, 128], BF16)
            sc = [pst(pa, "sc%d" % i, [128, 2, 2, 128], F32) for i in range(2)]
            pjf = pj[:].rearrange("p a b -> p (a b)")
            pt2 = sc[0][:].rearrange("p a b c -> p (a b c)").bitcast(BF16).rearrange("p (b c) -> p b c", c=128)

            for c in range(5):
                s.dma("pool", lambda e, c=c: e.dma_start(
                    out=w_in_bf[:, :, c * 512:(c + 1) * 512],
                    in_=w_in[:, c * 512:(c + 1) * 512].rearrange("(k p) n -> p k n", p=128)),
                    writes=["win%d" % c], key="win%d" % c)
            s.dma("sp", lambda e: e.dma_start(out=gmix[:], in_=norm_mix.partition_broadcast(128)), writes=["gmix"], key="gmix")
            s.dma("sp", lambda e: e.dma_start(out=qg[:], in_=q_norm.partition_broadcast(128)), writes=["qg"], key="qg")
            s.dma("sp", lambda e: e.dma_start(out=kg[:], in_=k_norm.partition_broadcast(128)), writes=["kg"], key="kg")
            s.dma("sp", lambda e: e.dma_start(out=esink[:], in_=sinks.partition_broadcast(128)), writes=["esink"], key="esink")
            s.dma("sp", lambda e: e.dma_start(out=pscale[:], in_=pool_scale.partition_broadcast(128)), writes=["pscale"], key="pscale")
            s.dma("sp", lambda e: e.dma_start(out=cs[:], in_=cs_d.rearrange("p (t c) -> p t c", c=16)), writes=["cs"], key="cs")
            s.dma("pool", lambda e: e.dma_start(out=wpool_bf[:], in_=w_pool.rearrange("g (c p) d -> p g c d", p=128)),
                  writes=["wpool"], key="wpool")
            s.op("dve", lambda e: e.tensor_copy(out=gqk[:, 0:16, :], in_=qg[:].unsqueeze(1).to_broadcast([128, 16, HD])),
                 reads=["qg"], writes=["gqk"])
            s.op("dve", lambda e: e.tensor_copy(out=gqk[:, 16:20, :], in_=kg[:].unsqueeze(1).to_broadcast([128, 4, HD])),
                 reads=["kg", "gqk"], writes=["gqk"])
            s.op("act", lambda e: e.activation(out=esink[:], in_=esink[:], func=AF.Exp), reads=["esink"], writes=["esink"])
            for i in range(3):
                s.op("pool", lambda e, i=i: e.memset(vv[i][:], 1.0), writes=["vv%d" % i])

            def F1(i):
                p = i % 2
                XT, HN = xt[p], hn[p]
                nxt, nhn = "xtA%d" % p, "hnA%d" % p
                s.dma("sp", lambda e: e.dma_start(out=XT[:], in_=x[i * 128:(i + 1) * 128, :]), writes=[nxt], key=nxt)
                s.op("act", lambda e: e.activation(out=junk[:], in_=XT[:], func=AF.Square, accum_out=ss[:]),
                     reads=[nxt], writes=["junk", "ss"])
                s.op("act", lambda e: e.activation(out=std[:], in_=ss[:], func=AF.Ln, scale=1.0 / D, bias=epsb[:, 0:1]),
                     reads=["ss", "epsb"], writes=["std"])
                s.op("act", lambda e: e.activation(out=rstd[:], in_=std[:], func=AF.Exp, scale=-0.5), reads=["std"], writes=["rstd"])
                s.op("dve", lambda e: e.scalar_tensor_tensor(out=HN[:], in0=XT[:], scalar=rstd[:, 0:1], in1=gmix[:],
                                                             op0=ALU.mult, op1=ALU.mult),
                     reads=[nxt, "rstd", "gmix"], writes=[nhn])

            def F2(i, part):
                p = i % 2
                HN = hn[p]
                nhn = "hnA%d" % p
                VV, UU, QK = vv[i % 3], uu[i % 3], qk32[p]
                nvv, nuu, nqk = "vv%d" % (i % 3), "uu%d" % (i % 3), "qk32_%d" % p
                if part == 0:
                    for r in range(2):
                        bank, nb_ = (pt[:], "pt") if r == 0 else (pt2, "sc0")
                        for b in range(8):
                            s.op("pe", lambda e, r=r, b=b, bank=bank: e.transpose(out=bank[:, b, :], in_=HN[:, (r * 8 + b) * 128:(r * 8 + b + 1) * 128],
                                                                                  identity=ident),
                                 reads=[nhn, "cst"], writes=[nb_])
                        s.op("act", lambda e, r=r, bank=bank: e.copy(out=hnT[:, r * 8:(r + 1) * 8, :], in_=bank), reads=[],
                             writes=[nb_, "hnT%d" % r])
                    return
                def chunk(c):
                    for k in range(KD):
                        s.op("pe", lambda e, k=k: e.matmul(out=pj[:, c, :], lhsT=hnT[:, k, :], rhs=w_in_bf[:, k, c * 512:(c + 1) * 512],
                                                           start=(k == 0), stop=(k == KD - 1)),
                             reads=["hnT%d" % (k // 8), "win%d" % c], writes=["pj%d" % c])

                def evac_qkv():
                    s.op("act", lambda e: e.copy(out=QK[:], in_=pjf[:, 0:1280]), reads=[], writes=["pj0", "pj1", "pj2", nqk])
                    s.op("act", lambda e: e.copy(out=VV[:, :, 0:HD], in_=pjf[:, 1280:1536].rearrange("p (h d) -> p h d", d=HD)),
                         reads=[], writes=["pj2", nvv])

                def evac_u():
                    s.op("act", lambda e: e.copy(out=UU[:], in_=pjf[:, 1536:2560]), reads=[], writes=["pj3", "pj4", nuu])

                return [lambda: chunk(0), lambda: chunk(1), lambda: (chunk(2), evac_qkv()), lambda: (chunk(3), chunk(4), evac_u())]

            def QKC(i):
                p = i % 2
                QK = qk32[p]
                nqk = "qk32_%d" % p
                s.op("act", lambda e: e.activation(out=sq32[:], in_=QK[:], func=AF.Square), reads=[nqk], writes=["sq32"])
                s.op("pool", lambda e: e.tensor_tensor(out=qg32[:], in0=QK[:].rearrange("p (h d) -> p h d", d=HD), in1=gqk[:], op=ALU.mult),
                     reads=[nqk, "gqk"], writes=["qg32"])
                s.op("dve", lambda e: e.tensor_reduce(out=ssq[:], in_=sq32[:].rearrange("p (h d) -> p h d", d=HD), axis=AX.X, op=ALU.add),
                     reads=["sq32"], writes=["ssq"])
                s.op("act", lambda e: e.activation(out=stq[:], in_=ssq[:], func=AF.Ln, scale=1.0 / HD, bias=epsb[:, 0:1]),
                     reads=["ssq", "epsb"], writes=["stq"])
                s.op("act", lambda e: e.activation(out=rq[:], in_=stq[:], func=AF.Exp, scale=-0.5), reads=["stq"], writes=["rq"])
                s.op("dve", lambda e: e.tensor_tensor(out=qn32[:], in0=qg32[:],
                                                       in1=rq[:].unsqueeze(2).to_broadcast([128, 20, HD]), op=ALU.mult),
                     reads=["qg32", "rq"], writes=["qn32"])
                cosb = cs[:, i, 0:8].unsqueeze(1).to_broadcast([128, 20, 8])
                sinb = cs[:, i, 8:16].unsqueeze(1).to_broadcast([128, 20, 8])
                s.op("dve", lambda e: e.tensor_tensor(out=tr[0][:], in0=qn32[:, :, 0:8], in1=cosb, op=ALU.mult),
                     reads=["qn32", "cs"], writes=["tr0"])
                s.op("dve", lambda e: e.tensor_tensor(out=tr[1][:], in0=qn32[:, :, 8:16], in1=sinb, op=ALU.mult),
                     reads=["qn32", "cs"], writes=["tr1"])
                s.op("dve", lambda e: e.tensor_tensor(out=tr[2][:], in0=qn32[:, :, 8:16], in1=cosb, op=ALU.mult),
                     reads=["qn32", "cs"], writes=["tr2"])
                s.op("dve", lambda e: e.tensor_tensor(out=tr[3][:], in0=qn32[:, :, 0:8], in1=sinb, op=ALU.mult),
                     reads=["qn32", "cs"], writes=["tr3"])
                s.op("dve", lambda e: e.tensor_tensor(out=qkb[:, :, 0:8], in0=tr[0][:], in1=tr[1][:], op=ALU.subtract),
                     reads=["tr0", "tr1"], writes=["qkb_a"])
                s.op("dve", lambda e: e.tensor_tensor(out=qkb[:, :, 8:16], in0=tr[2][:], in1=tr[3][:], op=ALU.add),
                     reads=["tr2", "tr3"], writes=["qkb_b"])
                s.op("pool", lambda e: e.tensor_copy(out=qkb[:, :, 16:HD], in_=qn32[:, :, 16:HD]), reads=["qn32"], writes=["qkb_c"])
                s.op("dve", lambda e: e.tensor_copy(out=kd[:], in_=qkb[:, 16:20, :].unsqueeze(2).to_broadcast([128, 4, 2, HD])),
                     reads=["qkb_a", "qkb_b", "qkb_c"], writes=["kd"])

            def BK(i):
                p, pp = i % 2, 1 - (i % 2)
                KT, KTp, MX = kT[p], kT[pp], mixed[p]
                nkt, nktp, nmx = "kT%d" % p, "kT%d" % pp, "mixed%d" % p
                VV, VVp, UU, UUp = vv[i % 3], vv[(i - 1) % 3], uu[i % 3], uu[(i - 1) % 3]
                nvv, nvvp, nuu, nuup = "vv%d" % (i % 3), "vv%d" % ((i - 1) % 3), "uu%d" % (i % 3), "uu%d" % ((i - 1) % 3)
                kbs = [1] if i == 0 else [0, 1]
                nkb = len(kbs)
                qkbf = qkb[:].rearrange("p h d -> p (h d)")
                kdf = kd[:].rearrange("p g t d -> p (g t d)")

                def tq():
                    for b in range(8):
                        s.op("pe", lambda e, b=b: e.transpose(out=pt[:, b, :], in_=qkbf[:, b * 128:(b + 1) * 128], identity=ident),
                             reads=["qkb_a", "qkb_b", "qkb_c", "cst"], writes=["pt"])
                    s.op("dve", lambda e: e.tensor_copy(out=qT[:], in_=pt[:]), reads=[], writes=["pt", "qT"])
                    for g in range(4):
                        s.op("pe", lambda e, g=g: e.transpose(out=pt2[:, g, :], in_=kdf[:, g * 128:(g + 1) * 128], identity=ident),
                             reads=["kd", "cst"], writes=["sc0"])
                    s.op("act", lambda e: e.copy(out=KT[:], in_=pt2[:, 0:4, :]), reads=[], writes=["sc0", nkt])

                def scores(g):
                    for kb in kbs:
                        ktile, nk = (KTp, nktp) if kb == 0 else (KT, nkt)
                        for hf in range(2):
                            s.op("pe", lambda e, kb=kb, hf=hf, ktile=ktile: e.matmul(
                                out=sc[hf][:, kb, :, :], lhsT=ktile[hf * 64:(hf + 1) * 64, g, :],
                                rhs=qT[hf * 64:(hf + 1) * 64, 2 * g:2 * g + 2, :], start=True, stop=True),
                                reads=[nk, "qT"], writes=["sc%d" % hf])
                    k0 = kbs[0]
                    for hf in range(2):
                        P = PT[g % 2][hf]
                        nP = "PT%d%d" % (g % 2, hf)
                        s.op("act", lambda e, hf=hf, P=P: e.activation(out=P[:, k0:2], in_=sc[hf][:, k0:2], func=AF.Exp, scale=HD ** -0.5),
                             reads=[], writes=["sc%d" % hf, nP])
                        s.op("pool" if hf == 1 else "dve", lambda e, P=P: e.tensor_tensor(
                            out=P[:, k0:2], in0=P[:, k0:2], in1=mask2[:, k0:2].unsqueeze(2).to_broadcast([128, 2 - k0, 2, 128]), op=ALU.mult),
                            reads=[nP, "cst"], writes=[nP])

                PVB = (0, 1, 2)

                def pv(g):
                    for blk in range(2):
                        for hf in range(2):
                            h = 4 * g + 2 * blk + hf
                            bnk, j = PVB[h // 7], h % 7
                            o = pj[:, bnk, j * 65:(j + 1) * 65]
                            P = PT[g % 2][hf]
                            for n, kb in enumerate(kbs):
                                vt, nv = (VVp, nvvp) if kb == 0 else (VV, nvv)
                                s.op("pe", lambda e, o=o, P=P, kb=kb, blk=blk, vt=vt, n=n: e.matmul(
                                    out=o, lhsT=P[:, kb, blk, :], rhs=vt[:, g, :], start=(n == 0), stop=(n == nkb - 1)),
                                    reads=["PT%d%d" % (g % 2, hf), nv], writes=["pj%d" % bnk])

                def pv_evac(rnd):
                    bnk = PVB[rnd]
                    h0 = rnd * 7
                    nh = min(7, NH - h0)
                    v = pj[:, bnk, 0:nh * 65].rearrange("p (h d) -> p h d", d=65)
                    s.op("dve", lambda e: e.tensor_tensor(out=den[:, 0:nh], in0=v[:, :, 64], in1=esink[:, h0:h0 + nh], op=ALU.add),
                         reads=["esink"], writes=["pj%d" % bnk, "den"])
                    s.op("dve", lambda e: e.reciprocal(out=rden[:, 0:nh], in_=den[:, 0:nh]), reads=["den"], writes=["rden"])
                    s.op("dve", lambda e: e.tensor_tensor(
                        out=MX[:, h0 * HD:(h0 + nh) * HD].rearrange("p (h d) -> p h d", d=HD), in0=v[:, :, 0:HD],
                        in1=rden[:, 0:nh].unsqueeze(2).to_broadcast([128, nh, HD]), op=ALU.mult),
                        reads=["rden"], writes=["pj%d" % bnk, nmx + "_a%d" % rnd])

                def pool_dT():
                    for cc in range(8):
                        g = cc // 2
                        o = pj[:, 3 + cc // 4, (cc % 4) * 128:(cc % 4 + 1) * 128]
                        if i > 0:
                            s.op("pe", lambda e, o=o, cc=cc, g=g: e.matmul(out=o, lhsT=UUp[:, cc * 128:(cc + 1) * 128], rhs=poolm(1, g),
                                                                           start=True, stop=False),
                                 reads=[nuup, "cst"], writes=["pj%d" % (3 + cc // 4)])
                        s.op("pe", lambda e, o=o, cc=cc, g=g: e.matmul(out=o, lhsT=UU[:, cc * 128:(cc + 1) * 128],
                                                                       rhs=poolm(2 if i == 0 else 0, g), start=(i == 0), stop=True),
                             reads=[nuu, "cst"], writes=["pj%d" % (3 + cc // 4)])
                    s.op("act", lambda e: e.copy(out=dTs[:].rearrange("p a b -> p (a b)"), in_=pjf[:, 1536:2560]),
                         reads=[], writes=["pj3", "pj4", "dTs"])

                def pool_y():
                    for g in range(4):
                        for c2 in range(2):
                            s.op("pe", lambda e, g=g, c2=c2: e.matmul(out=pjf[:, 1536 + g * 256:1536 + (g + 1) * 256], lhsT=dTs[:, 2 * g + c2, :],
                                                                      rhs=wpool_bf[:, g, c2, :], start=(c2 == 0), stop=(c2 == 1)),
                                 reads=["dTs", "wpool"], writes=["pj%d" % (3 + g // 2)])
                    s.op("dve", lambda e: e.tensor_tensor(out=MX[:, 1024:2048], in0=pjf[:, 1536:2560], in1=pscale[:], op=ALU.mult),
                         reads=["pscale"], writes=["pj3", "pj4", nmx + "_p"])

                def sec0():
                    tq()

                def sec1():
                    scores(0)
                    pool_dT()

                def sec2():
                    scores(1)
                    pool_y()

                def sec3():
                    pv(0)
                    scores(2)
                    pv(1)
                    pv_evac(0)

                def sec4():
                    scores(3)
                    pv(2)
                    pv(3)
                    pv_evac(1)
                    pv_evac(2)

                return [sec0, sec1, sec2, sec3, sec4]

            def storeA(i):
                MX = mixed[i % 2]
                nmx = "mixed%d" % (i % 2)
                s.dma("sp", lambda e: e.dma_start(out=MIX[i * 128:(i + 1) * 128, :], in_=MX[:]),
                      reads=[nmx + "_a0", nmx + "_a1", nmx + "_a2", nmx + "_p"], writes=["MIX%d" % i], key="st_" + nmx)

            F1(0)
            if NT > 1:
                F1(1)
            F2(0, 0)
            for f in F2(0, 1):
                f()
            for st in range(NT):
                if st + 1 < NT:
                    F2(st + 1, 0)
                if st + 2 < NT:
                    F1(st + 2)
                if st >= 1:
                    storeA(st - 1)
                QKC(st)
                if st + 1 < NT:
                    for f in F2(st + 1, 1):
                        f()
                for f in BK(st):
                    f()
            storeA(NT - 1)
            s.barrier()
            pa.close()

        def phase_B():
            pb = ExitStack()
            w_out_bf = sbt(pb, "w_out_bf", [128, KD, D], BF16)
            gffn = sbt(pb, "gffn", [128, D], F32)
            wr_bf = sbt(pb, "wr_bf", [128, KD, 36], BF16)
            brb = sbt(pb, "brb", [128, 36], F32)
            xt = [sbt(pb, "xtB%d" % i, [128, D], F32) for i in range(3)]
            mx = [sbt(pb, "mxB%d" % i, [128, D], BF16) for i in range(3)]
            mxT = sbt(pb, "mxT", [128, KD, 128], BF16)
            junk = sbt(pb, "junkB", [128, D], BF16)
            ss = sbt(pb, "ssB", [128, 1], F32)
            std = sbt(pb, "stdB", [128, 1], F32)
            rstd = sbt(pb, "rstdB", [128, 1], F32)
            hn2 = [sbt(pb, "hn2_%d" % i, [128, D], BF16) for i in range(3)]
            hn2T = sbt(pb, "hn2T", [128, KD, 128], BF16)
            lg = sbt(pb, "lg", [128, 36], F32)
            cmax = sbt(pb, "cmax", [128, 1], F32)
            ncmax = sbt(pb, "ncmax", [128, 1], F32)
            ohg = sbt(pb, "ohg", [128, 4], F32)
            ec = sbt(pb, "ec", [128, 4], F32)
            sumc = sbt(pb, "sumc", [128, 1], F32)
            pgrp = sbt(pb, "pgrp", [128, 1], F32)
            tmp48 = sbt(pb, "tmp48", [128, 4, 8], F32)
            fsel = sbt(pb, "fsel", [128, 8], F32)
            m1 = sbt(pb, "m1", [128, 1], F32)
            oh1 = sbt(pb, "oh1", [128, 8], F32)
            msk = sbt(pb, "msk", [128, 8], F32)
            m2 = sbt(pb, "m2", [128, 1], F32)
            oh2 = sbt(pb, "oh2", [128, 8], F32)
            d21 = sbt(pb, "d21", [128, 1], F32)
            e21 = sbt(pb, "e21", [128, 1], F32)
            rr = sbt(pb, "rr", [128, 1], F32)
            E1 = sbt(pb, "E1", [128, 4, 8], F32)
            E2 = sbt(pb, "E2", [128, 4, 8], F32)
            Abf = sbt(pb, "Abf", [128, E], BF16)
            posv = sbt(pb, "posv", [128, E], F32)
            ov = sbt(pb, "ov", [128, E], F32)
            tmp32 = sbt(pb, "tmp32", [128, E], F32)
            dd = sbt(pb, "dd", [128, 2], F32)
            po = pst(pb, "po", [128, 4, 512], F32)
            ptB = pst(pb, "ptB", [128, 8, 128], BF16)
            ptB2 = pst(pb, "ptB2", [128, 8, 128], BF16)
            plg = pst(pb, "plg", [128, 512], F32)
            prk = pst(pb, "prk", [128, 512], F32)
            pof = po[:].rearrange("p a b -> p (a b)")

            for c in range(4):
                s.dma("pool", lambda e, c=c: e.dma_start(
                    out=w_out_bf[:, :, c * 512:(c + 1) * 512],
                    in_=w_out[:, c * 512:(c + 1) * 512].rearrange("(k p) n -> p k n", p=128)),
                    writes=["wout%d" % c], key="wout%d" % c)
            s.dma("pool", lambda e: e.dma_start(out=wr_bf[:], in_=w_r.rearrange("(k p) n -> p k n", p=128)), writes=["wr"], key="wr")
            s.dma("sp", lambda e: e.dma_start(out=gffn[:], in_=norm_ffn.partition_broadcast(128)), writes=["gffn"], key="gffn")
            s.dma("sp", lambda e: e.dma_start(out=brb[:], in_=b_r.partition_broadcast(128)), writes=["brb"], key="brb")

            def prefetch_e0():
                s.dma("pool", lambda e: e.dma_start(out=pre["wg"][:], in_=w_gate[0].rearrange("(k p) n -> p k n", p=128)),
                      writes=["wg0"], key="wg0")
                s.dma("pool", lambda e: e.dma_start(out=pre["wu"][:], in_=w_up[0].rearrange("(k p) n -> p k n", p=128)),
                      writes=["wu0"], key="wu0")
                for hh_ in range(2):
                    s.dma("pool", lambda e, hh_=hh_: e.dma_start(
                        out=pre["wd"][:, :, hh_ * 1024:(hh_ + 1) * 1024],
                        in_=w_down[0][:, hh_ * 1024:(hh_ + 1) * 1024].rearrange("(j p) n -> p j n", p=128)),
                        writes=["wd0_%d" % hh_], key="wd0_%d" % hh_)

            ptBs = [ptB, ptB2]
            tcnt = [0]

            def transposeB(src, nsrc, dst, ndst):
                for r in range(2):
                    bk = tcnt[0] % 2
                    tcnt[0] += 1
                    PTb = ptBs[bk]
                    for b in range(8):
                        s.op("pe", lambda e, r=r, b=b, PTb=PTb: e.transpose(out=PTb[:, b, :], in_=src[:, (r * 8 + b) * 128:(r * 8 + b + 1) * 128],
                                                                            identity=ident),
                             reads=[nsrc, "cst"], writes=["ptB%d" % bk])
                    s.op("act", lambda e, r=r, PTb=PTb: e.copy(out=dst[:, r * 8:(r + 1) * 8, :], in_=PTb[:]), reads=["ptB%d" % bk],
                         writes=[ndst + "%d" % r])

            V = lambda fn, r, w: s.op("dve", fn, reads=r, writes=w)
            E1f = E1[:].rearrange("p g e -> p (g e)")
            E2f = E2[:].rearrange("p g e -> p (g e)")

            def loadsB(i):
                p = i % 3
                XT, MXb = xt[p], mx[p]
                nxt, nmx = "xtB%d" % p, "mxB%d" % p
                s.dma("sp", lambda e: e.dma_start(out=XT[:], in_=x[i * 128:(i + 1) * 128, :]), writes=[nxt], key=nxt)
                s.dma("sp", lambda e: e.dma_start(out=MXb[:], in_=MIX[i * 128:(i + 1) * 128, :]), writes=[nmx], key=nmx)

            def storeB(i):
                p = i % 3
                XT = xt[p]
                nxt = "xtB%d" % p
                s.dma("sp", lambda e: e.dma_start(out=out[i * 128:(i + 1) * 128, :], in_=XT[:]), reads=[nxt],
                      writes=["out%d" % i], key="st_" + nxt)

            def G1(i, part):
                p = i % 3
                XT, MXb = xt[p], mx[p]
                nxt, nmx = "xtB%d" % p, "mxB%d" % p
                if part == 0:
                    transposeB(MXb, nmx, mxT, "mxT")
                    return
                for c in range(4):
                    for k in range(KD):
                        s.op("pe", lambda e, c=c, k=k: e.matmul(out=po[:, c, :], lhsT=mxT[:, k, :], rhs=w_out_bf[:, k, c * 512:(c + 1) * 512],
                                                                start=(k == 0), stop=(k == KD - 1)),
                             reads=["mxT%d" % (k // 8), "wout%d" % c], writes=["po%d" % c])
                s.op("dve", lambda e: e.tensor_tensor(out=XT[:], in0=pof, in1=XT[:], op=ALU.add),
                     reads=["po0", "po1", "po2", "po3", nxt], writes=[nxt])

            def G2a(i):
                p = i % 3
                XT, H2 = xt[i % 3], hn2[p]
                nxt, nh2 = "xtB%d" % (i % 3), "hn2_%d" % p
                s.op("act", lambda e: e.activation(out=junk[:], in_=XT[:], func=AF.Square, accum_out=ss[:]),
                     reads=[nxt], writes=["junk", "ss"])
                s.op("act", lambda e: e.activation(out=std[:], in_=ss[:], func=AF.Ln, scale=1.0 / D, bias=epsb[:, 0:1]),
                     reads=["ss", "epsb"], writes=["std"])
                s.op("act", lambda e: e.activation(out=rstd[:], in_=std[:], func=AF.Exp, scale=-0.5), reads=["std"], writes=["rstd"])
                s.op("dve", lambda e: e.scalar_tensor_tensor(out=H2[:], in0=XT[:], scalar=rstd[:, 0:1], in1=gffn[:],
                                                             op0=ALU.mult, op1=ALU.mult),
                     reads=[nxt, "rstd", "gffn"], writes=[nh2])

            def G2b(i):
                p = i % 3
                H2 = hn2[p]
                nh2 = "hn2_%d" % p
                transposeB(H2, nh2, hn2T, "hn2T")
                for k in range(KD):
                    s.op("pe", lambda e, k=k: e.matmul(out=plg[:, 0:36], lhsT=hn2T[:, k, :], rhs=wr_bf[:, k, :],
                                                       start=(k == 0), stop=(k == KD - 1)),
                         reads=["hn2T%d" % (k // 8), "wr"], writes=["plg"])
                V(lambda e: e.tensor_tensor(out=lg[:], in0=plg[:, 0:36], in1=brb[:], op=ALU.add), ["plg", "brb"], ["lg"])

            def G3a(i):
                V(lambda e: e.tensor_reduce(out=cmax[:], in_=lg[:, 0:4], axis=AX.X, op=ALU.max), ["lg"], ["cmax"])
                V(lambda e: e.tensor_scalar(out=ohg[:], in0=lg[:, 0:4], scalar1=cmax[:, 0:1], scalar2=None, op0=ALU.is_ge), ["lg", "cmax"], ["ohg"])
                V(lambda e: e.tensor_scalar(out=ncmax[:], in0=cmax[:], scalar1=-1.0, scalar2=None, op0=ALU.mult), ["cmax"], ["ncmax"])
                s.op("act", lambda e: e.activation(out=ec[:], in_=lg[:, 0:4], func=AF.Exp, bias=ncmax[:, 0:1], accum_out=sumc[:]),
                     reads=["lg", "ncmax"], writes=["ec", "sumc"])
                V(lambda e: e.tensor_tensor(out=tmp48[:], in0=lg[:, 4:36].rearrange("p (g e) -> p g e", g=4),
                                            in1=ohg[:].unsqueeze(2).to_broadcast([128, 4, 8]), op=ALU.mult), ["lg", "ohg"], ["tmp48"])
                V(lambda e: e.tensor_reduce(out=fsel[:], in_=tmp48[:].rearrange("p g e -> p e g"), axis=AX.X, op=ALU.add), ["tmp48"], ["fsel"])
                V(lambda e: e.tensor_reduce(out=m1[:], in_=fsel[:], axis=AX.X, op=ALU.max), ["fsel"], ["m1"])
                V(lambda e: e.tensor_scalar(out=oh1[:], in0=fsel[:], scalar1=m1[:, 0:1], scalar2=None, op0=ALU.is_ge), ["fsel", "m1"], ["oh1"])
                V(lambda e: e.scalar_tensor_tensor(out=msk[:], in0=oh1[:], scalar=-1e30, in1=fsel[:], op0=ALU.mult, op1=ALU.add),
                  ["oh1", "fsel"], ["msk"])
                V(lambda e: e.tensor_reduce(out=m2[:], in_=msk[:], axis=AX.X, op=ALU.max), ["msk"], ["m2"])
                V(lambda e: e.tensor_scalar(out=oh2[:], in0=msk[:], scalar1=m2[:, 0:1], scalar2=None, op0=ALU.is_ge), ["msk", "m2"], ["oh2"])
                V(lambda e: e.tensor_tensor(out=d21[:], in0=m2[:], in1=m1[:], op=ALU.subtract), ["m1", "m2"], ["d21"])
                s.op("act", lambda e: e.activation(out=e21[:], in_=d21[:], func=AF.Exp), reads=["d21"], writes=["e21"])
                V(lambda e: e.tensor_tensor(out=E1[:], in0=ohg[:].unsqueeze(2).to_broadcast([128, 4, 8]),
                                            in1=oh1[:].unsqueeze(1).to_broadcast([128, 4, 8]), op=ALU.mult), ["ohg", "oh1"], ["E1"])
                V(lambda e: e.tensor_tensor(out=E2[:], in0=ohg[:].unsqueeze(2).to_broadcast([128, 4, 8]),
                                            in1=oh2[:].unsqueeze(1).to_broadcast([128, 4, 8]), op=ALU.mult), ["ohg", "oh2"], ["E2"])
                V(lambda e: e.tensor_tensor(out=Abf[:], in0=E1f, in1=E2f, op=ALU.add), ["E1", "E2"], ["Abf"])
                V(lambda e: e.reciprocal(out=pgrp[:], in_=sumc[:]), ["sumc"], ["pgrp"])
                V(lambda e: e.tensor_scalar(out=e21[:], in0=e21[:], scalar1=1.0, scalar2=None, op0=ALU.add), ["e21"], ["e21"])
                V(lambda e: e.reciprocal(out=rr[:], in_=e21[:]), ["e21"], ["rr"])
                V(lambda e: e.tensor_tensor(out=GATE[:, i, 0:1], in0=pgrp[:], in1=rr[:], op=ALU.mult), ["pgrp", "rr"], ["GATE"])
                V(lambda e: e.tensor_tensor(out=GATE[:, i, 1:2], in0=pgrp[:], in1=GATE[:, i, 0:1], op=ALU.subtract), ["pgrp", "GATE"], ["GATE"])

            def G3b(i):
                p = i % 3
                H2 = hn2[p]
                nh2 = "hn2_%d" % p
                s.op("pe", lambda e: e.matmul(out=prk[:, 0:E], lhsT=tri, rhs=Abf[:], start=True, stop=True), reads=["Abf", "cst"], writes=["prk"])
                s.op("pe", lambda e: e.matmul(out=prk[:, E:2 * E], lhsT=ones, rhs=Abf[:], start=True, stop=True), reads=["Abf", "cst"], writes=["prk"])
                V(lambda e: e.tensor_tensor(out=posv[:], in0=prk[:, 0:E], in1=base[:], op=ALU.add), ["prk", "base"], ["posv"])
                V(lambda e: e.tensor_tensor(out=base[:], in0=prk[:, E:2 * E], in1=base[:], op=ALU.add), ["prk", "base"], ["base"])
                V(lambda e: e.tensor_scalar(out=ov[:], in0=posv[:], scalar1=float(C), scalar2=1e9, op0=ALU.is_ge, op1=ALU.mult), ["posv"], ["ov"])
                V(lambda e: e.tensor_tensor(out=posv[:], in0=posv[:], in1=ov[:], op=ALU.add), ["posv", "ov"], ["posv"])
                V(lambda e: e.tensor_tensor(out=posv[:], in0=posv[:], in1=eoff[:], op=ALU.add), ["posv", "eoff"], ["posv"])
                V(lambda e: e.tensor_tensor(out=tmp32[:], in0=E1f, in1=posv[:], op=ALU.mult), ["E1", "posv"], ["tmp32"])
                V(lambda e: e.tensor_reduce(out=dd[:, 0:1], in_=tmp32[:], axis=AX.X, op=ALU.add), ["tmp32"], ["dd"])
                V(lambda e: e.tensor_tensor(out=tmp32[:], in0=E2f, in1=posv[:], op=ALU.mult), ["E2", "posv", "dd"], ["tmp32"])
                V(lambda e: e.tensor_reduce(out=dd[:, 1:2], in_=tmp32[:], axis=AX.X, op=ALU.add), ["tmp32"], ["dd"])
                V(lambda e: e.tensor_copy(out=DEST[:, i, :], in_=dd[:]), ["dd"], ["DEST%d" % i])
                for j in range(2):
                    s.dma("pool", lambda e, j=j: e.indirect_dma_start(
                        out=XS, out_offset=bass.IndirectOffsetOnAxis(ap=DEST[:, i, j:j + 1], axis=0),
                        in_=H2[:], in_offset=None, bounds_check=bcreg(e), oob_is_err=False),
                        reads=[nh2, "DEST%d" % i], writes=["XS_%d_%d" % (i, j)], key="sc_" + nh2)

            loadsB(0)
            for st in range(NT + 2):
                if pre and st == min(6, NT):
                    prefetch_e0()
                if st + 1 < NT:
                    loadsB(st + 1)
                if 0 <= st - 1 < NT:
                    storeB(st - 1)
                if st < NT:
                    G1(st, 0)
                if 0 <= st - 1 < NT:
                    G2a(st - 1)
                if 0 <= st - 2 < NT:
                    G3a(st - 2)
                if st < NT:
                    G1(st, 1)
                if 0 <= st - 1 < NT:
                    G2b(st - 1)
                if 0 <= st - 2 < NT:
                    G3b(st - 2)
            s.barrier()
            pb.close()

        def phase_C():
            pc = ExitStack()
            if pre:
                wg = [pre["wg"], sbt(pc, "wg1", [128, KD, DE], BF16)]
                wu = [pre["wu"], sbt(pc, "wu1", [128, KD, DE], BF16)]
                wd = [pre["wd"], sbt(pc, "wd1", [128, 4, D], BF16)]
            else:
                wg = [sbt(pc, "wg%d" % i, [128, KD, DE], BF16) for i in range(2)]
                wu = [sbt(pc, "wu%d" % i, [128, KD, DE], BF16) for i in range(2)]
                wd = [sbt(pc, "wd%d" % i, [128, 4, D], BF16) for i in range(2)]
            xr = [sbt(pc, "xr%d" % i, [128, D], BF16) for i in range(3)]
            xT = [sbt(pc, "xTC%d" % i, [128, KD, C], BF16) for i in range(2)]
            hT = [sbt(pc, "hTC%d" % i, [128, 4, C], BF16) for i in range(2)]
            sil = [sbt(pc, "sil%d" % i, [128, C], F32) for i in range(2)]
            ys = [sbt(pc, "ys%d" % i, [128, D], BF16) for i in range(4)]
            pab = [[pst(pc, "pab%d%d" % (a, b), [128, 512], F32) for b in range(2)] for a in range(2)]
            py = [pst(pc, "py%d" % i, [128, 512], F32) for i in range(2)]
            ptC = [pst(pc, "ptC%d" % i, [128, 8, 128], BF16) for i in range(2)]
            cnt = dict(row=0, py=0, half=0, cp=0)

            def load_w(ex):
                p = ex % 2
                WG, WU, WD = wg[p], wu[p], wd[p]
                s.dma("pool", lambda e: e.dma_start(out=WG[:], in_=w_gate[ex].rearrange("(k p) n -> p k n", p=128)),
                      writes=["wg%d" % p], key="wg%d" % p)
                s.dma("pool", lambda e: e.dma_start(out=WU[:], in_=w_up[ex].rearrange("(k p) n -> p k n", p=128)),
                      writes=["wu%d" % p], key="wu%d" % p)
                for hh_ in range(2):
                    s.dma("pool", lambda e, hh_=hh_: e.dma_start(
                        out=WD[:, :, hh_ * 1024:(hh_ + 1) * 1024],
                        in_=w_down[ex][:, hh_ * 1024:(hh_ + 1) * 1024].rearrange("(j p) n -> p j n", p=128)),
                        writes=["wd%d_%d" % (p, hh_)], key="wd%d_%d" % (p, hh_))

            def load_x(ex):
                for r in range(CT):
                    q = (ex * CT + r) % 3
                    XR = xr[q]
                    s.dma("sp", lambda e, XR=XR, r=r: e.dma_start(out=XR[0:rws(r), :], in_=XS[ex * C + r * 128:ex * C + r * 128 + rws(r), :]),
                          writes=["xr%d" % q], key="xr%d" % q)

            def transposes(ex):
                p = ex % 2
                XT = xT[p]
                for r in range(CT):
                    q = (ex * CT + r) % 3
                    XR = xr[q]
                    for rr_ in range(2):
                        hf = cnt["half"] % 2
                        cnt["half"] += 1
                        for b in range(8):
                            kk = rr_ * 8 + b
                            s.op("pe", lambda e, XR=XR, kk=kk, hf=hf, b=b, r=r: e.transpose(
                                out=ptC[hf][:, b, 0:rws(r)], in_=XR[0:rws(r), kk * 128:(kk + 1) * 128], identity=ident[0:rws(r), 0:rws(r)]),
                                 reads=["xr%d" % q, "cst"], writes=["ptC%d" % hf])
                        if hf == 0:
                            s.op("act", lambda e, rr_=rr_, r=r, hf=hf: e.copy(out=XT[:, rr_ * 8:(rr_ + 1) * 8, r * 128:r * 128 + rws(r)],
                                                                             in_=ptC[hf][:, :, 0:rws(r)]),
                                 reads=["ptC%d" % hf], writes=["xT%d" % p])
                        else:
                            s.op("dve", lambda e, rr_=rr_, r=r, hf=hf: e.tensor_copy(out=XT[:, rr_ * 8:(rr_ + 1) * 8, r * 128:r * 128 + rws(r)],
                                                                                    in_=ptC[hf][:, :, 0:rws(r)]),
                                 reads=["ptC%d" % hf], writes=["xT%d" % p])

            def gateup(ex):
                p = ex % 2
                WG, WU, XT, HT = wg[p], wu[p], xT[p], hT[p]
                for j in range(4):
                    PA, PB = pab[j % 2]
                    na, nb = "pab%d0" % (j % 2), "pab%d1" % (j % 2)
                    for k in range(KD):
                        s.op("pe", lambda e, PA=PA, j=j, k=k: e.matmul(out=PA[:, 0:C], lhsT=WG[:, k, j * 128:(j + 1) * 128], rhs=XT[:, k, :],
                                                                       start=(k == 0), stop=(k == KD - 1)),
                             reads=["wg%d" % p, "xT%d" % p], writes=[na])
                    for k in range(KD):
                        s.op("pe", lambda e, PB=PB, j=j, k=k: e.matmul(out=PB[:, 0:C], lhsT=WU[:, k, j * 128:(j + 1) * 128], rhs=XT[:, k, :],
                                                                       start=(k == 0), stop=(k == KD - 1)),
                             reads=["wu%d" % p, "xT%d" % p], writes=[nb])
                    SL = sil[j % 2]
                    s.op("act", lambda e, PA=PA, SL=SL: e.activation(out=SL[:], in_=PA[:, 0:C], func=AF.Silu), reads=[na], writes=["sil%d" % (j % 2)])
                    s.op("dve", lambda e, PB=PB, SL=SL, j=j: e.tensor_tensor(out=HT[:, j, :], in0=PB[:, 0:C], in1=SL[:], op=ALU.mult),
                         reads=[nb, "sil%d" % (j % 2)], writes=["hT%d" % p])

            def down(ex):
                p = ex % 2
                WD, HT = wd[p], hT[p]
                for r in range(CT):
                    q = (ex * CT + r) % 4
                    YSb = ys[q]
                    for c in range(4):
                        PY = py[cnt["py"] % 2]
                        npn = "py%d" % (cnt["py"] % 2)
                        for j in range(4):
                            s.op("pe", lambda e, PY=PY, r=r, c=c, j=j: e.matmul(out=PY[0:rws(r), :], lhsT=HT[:, j, r * 128:r * 128 + rws(r)],
                                                                                rhs=WD[:, j, c * 512:(c + 1) * 512], start=(j == 0), stop=(j == 3)),
                                 reads=["hT%d" % p, "wd%d_%d" % (p, c // 2)], writes=[npn])
                        if cnt["py"] % 2 == 0:
                            s.op("act", lambda e, PY=PY, YSb=YSb, c=c, r=r: e.copy(out=YSb[0:rws(r), c * 512:(c + 1) * 512], in_=PY[0:rws(r), :]),
                                 reads=[npn], writes=["ys%d" % q])
                        else:
                            s.op("dve", lambda e, PY=PY, YSb=YSb, c=c, r=r: e.tensor_copy(out=YSb[0:rws(r), c * 512:(c + 1) * 512], in_=PY[0:rws(r), :]),
                                 reads=[npn], writes=["ys%d" % q])
                        cnt["py"] += 1
                    s.dma("sp", lambda e, YSb=YSb, r=r: e.dma_start(out=YS[ex * C + r * 128:ex * C + r * 128 + rws(r), :], in_=YSb[0:rws(r), :]),
                          reads=["ys%d" % q], writes=["YS_%d_%d" % (ex, r)], key="st_ys%d" % q)

            if not pre:
                load_w(0)
            load_x(0)
            transposes(0)
            for ex in range(E):
                if ex + 1 < E:
                    load_w(ex + 1)
                    load_x(ex + 1)
                gateup(ex)
                if ex + 1 < E:
                    transposes(ex + 1)
                down(ex)
            s.barrier()
            pc.close()

        def phase_D():
            pd = ExitStack()
            NS = 4
            ya = [sbt(pd, "ya%d" % i, [128, D], BF16) for i in range(NS)]
            yb = [sbt(pd, "yb%d" % i, [128, D], BF16) for i in range(NS)]
            hh = [sbt(pd, "hh%d" % i, [128, D], F32) for i in range(NS)]
            for i in range(NS):
                s.op("dve", lambda e, i=i: e.memset(ya[i][:], 0.0), writes=["ya%d" % i])
                s.op("pool", lambda e, i=i: e.memset(yb[i][:], 0.0), writes=["yb%d" % i])

            def loadsD(i):
                p = i % NS
                YA, YB, HH = ya[p], yb[p], hh[p]
                s.dma("pool", lambda e: e.indirect_dma_start(
                    out=YA[:], out_offset=None, in_=YS, in_offset=bass.IndirectOffsetOnAxis(ap=DEST[:, i, 0:1], axis=0),
                    bounds_check=bcreg(e), oob_is_err=False), writes=["ya%d" % p], key="ya%d" % p)
                s.dma("pool", lambda e: e.indirect_dma_start(
                    out=YB[:], out_offset=None, in_=YS, in_offset=bass.IndirectOffsetOnAxis(ap=DEST[:, i, 1:2], axis=0),
                    bounds_check=bcreg(e), oob_is_err=False), writes=["yb%d" % p], key="yb%d" % p)
                s.dma("sp", lambda e: e.dma_start(out=HH[:], in_=out[i * 128:(i + 1) * 128, :]), writes=["hh%d" % p], key="hh%d" % p)

            def combineD(i):
                p = i % NS
                YA, YB, HH = ya[p], yb[p], hh[p]
                s.op("dve", lambda e: e.scalar_tensor_tensor(out=HH[:], in0=YA[:], scalar=GATE[:, i, 0:1], in1=HH[:],
                                                             op0=ALU.mult, op1=ALU.add),
                     reads=["ya%d" % p, "hh%d" % p], writes=["hh%d" % p])
                s.op("dve", lambda e: e.scalar_tensor_tensor(out=HH[:], in0=YB[:], scalar=GATE[:, i, 1:2], in1=HH[:],
                                                             op0=ALU.mult, op1=ALU.add),
                     reads=["yb%d" % p, "hh%d" % p], writes=["hh%d" % p])
                s.dma("sp", lambda e: e.dma_start(out=out[i * 128:(i + 1) * 128, :], in_=HH[:]), reads=["hh%d" % p],
                      writes=["outD%d" % i], key="st_hh%d" % p)

            for i in range(NT + 2):
                if i < NT:
                    loadsD(i)
                if i - 2 >= 0:
                    combineD(i - 2)
            pd.close()

        pre = {}
        for ph, fn in (("A", phase_A), ("B", phase_B), ("C", phase_C), ("D", phase_D)):
            if ph == "B" and "B" in phases and "C" in phases:
                pbc = ExitStack()
                pre["wg"] = sbt(pbc, "wg0", [128, KD, DE], BF16)
                pre["wu"] = sbt(pbc, "wu0", [128, KD, DE], BF16)
                pre["wd"] = sbt(pbc, "wd0", [128, 4, D], BF16)
            if ph in phases:
                fn()
            if ph == "C" and pre:
                pbc.close()
        if debug:
            s.barrier()
            s.dma("sp", lambda e: e.dma_start(out=DBGD, in_=DEST[:]), key="dbgd")
            s.dma("sp", lambda e: e.dma_start(out=DBGG, in_=GATE[:]), key="dbgg")
        s.finish()
        info = run_schedule(nc, s, es)
    return nc, info


CAP = 352
_PROG_CACHE = {}


def make_in_map(xb, prm, consts):
    cst, cs, eoff = consts
    m = dict(prm)
    m["x"] = np.ascontiguousarray(xb)
    m["cst"] = cst
    m["cs"] = cs
    m["eoff"] = eoff
    return m


def prep_params(norm_mix, w_in, q_norm, k_norm, sinks, w_pool, pool_scale, w_out, norm_ffn,
                w_coarse, b_coarse, w_fine, b_fine, w_gate, w_up, w_down):
    f = lambda a: np.ascontiguousarray(np.asarray(a, dtype=np.float32))
    return dict(
        norm_mix=f(norm_mix).reshape(1, D), w_in=f(w_in).reshape(D, INW), q_norm=f(q_norm).reshape(1, HD),
        k_norm=f(k_norm).reshape(1, HD), sinks=f(sinks).reshape(1, NH), w_pool=f(w_pool).reshape(4, 256, 256),
        pool_scale=f(pool_scale).reshape(1, 1024), w_out=f(w_out).reshape(D, D), norm_ffn=f(norm_ffn).reshape(1, D),
        w_r=np.ascontiguousarray(np.concatenate([f(w_coarse).reshape(D, 4), f(w_fine).reshape(D, E)], axis=1)),
        b_r=np.ascontiguousarray(np.concatenate([f(b_coarse).reshape(1, 4), f(b_fine).reshape(1, E)], axis=1)),
        w_gate=f(w_gate).reshape(E, D, DE), w_up=f(w_up).reshape(E, D, DE), w_down=f(w_down).reshape(E, DE, D))


def kernel(x, norm_mix, w_in, q_norm, k_norm, sinks, w_pool, pool_scale, w_out, norm_ffn,
           w_coarse, b_coarse, w_fine, b_fine, w_gate, w_up, w_down):
    x = np.asarray(x, dtype=np.float32)
    B, S, _ = x.shape
    assert B == N_CORES
    prm = prep_params(norm_mix, w_in, q_norm, k_norm, sinks, w_pool, pool_scale, w_out, norm_ffn,
                      w_coarse, b_coarse, w_fine, b_fine, w_gate, w_up, w_down)
    key = (S, CAP)
    if key not in _PROG_CACHE:
        _PROG_CACHE[key] = build_program(S, CAP)[0]
    nc = _PROG_CACHE[key]
    consts = build_consts(S, CAP)
    in_maps = [make_in_map(x[b], prm, consts) for b in range(B)]
    res = run_bass_kernel_spmd(nc, in_maps, core_ids=list(range(B)))
    return np.stack([np.asarray(r["out"], dtype=np.float32) for r in res.results], axis=0)
```
